# Optimizing a Trainium2 kernel written in Bass

```python
import jax
import jax.numpy as jnp
from jax import lax
import numpy as np

D_MODEL = 1024
BATCH = 2
SEQ = 16384
DEPTH = 4

GRID_W = 64
CTX_LEN = 256
N_MIXERS = 2
N_LAYERS_A = (DEPTH + N_MIXERS - 1) // N_MIXERS
N_LAYERS_B = DEPTH // N_MIXERS
N_MOD = 6
NORM_EPS = 1e-6
CHUNK = 128
GMLP_WIDTH = 2 * D_MODEL
GMLP_GROUP_CH = 128
GMLP_GROUPS = GMLP_WIDTH // GMLP_GROUP_CH
LN_EPS = 1e-5
RWKV_HEAD = 64
RWKV_HEADS = D_MODEL // RWKV_HEAD
DECAY_LORA = 64
AAA_LORA = 64
GATE_LORA = 160
GN_EPS = 64e-5
N_DIR = 2
N_GROUPS = 4
EXPERTS_PER_GROUP = 8
N_EXPERTS = N_GROUPS * EXPERTS_PER_GROUP
EXPERT_HIDDEN = 512
TOP_K = 2
MOE_BLOCK = 128

kernel_name = 'hybrid_gmlp_rwkv7_hmoe_dit'


def rmsnorm(x, g):
    xf = x.astype(jnp.float32)
    y = xf * lax.rsqrt(jnp.mean(xf * xf, axis=-1, keepdims=True) + NORM_EPS)
    return y.astype(x.dtype) * g


def layernorm(x, g, b, eps):
    xf = x.astype(jnp.float32)
    mu = jnp.mean(xf, axis=-1, keepdims=True)
    var = jnp.mean(jnp.square(xf - mu), axis=-1, keepdims=True)
    return ((xf - mu) * lax.rsqrt(var + eps)).astype(x.dtype) * g + b


def gmlp_mixer(h, w_in, b_in, ln_g, ln_b, w_s, b_s, w_out, b_out):
    bsz, n, _ = h.shape
    z = jax.nn.gelu(h @ w_in + b_in, approximate=False)
    u, v = jnp.split(z, 2, axis=-1)
    v = layernorm(v, ln_g, ln_b, LN_EPS)
    v = v.reshape(bsz, n // CHUNK, CHUNK, GMLP_GROUPS, GMLP_GROUP_CH)
    v = jnp.einsum('gpq,bcqgd->bcpgd', w_s, v) + b_s.T[:, :, None]
    return (u * v.reshape(bsz, n, GMLP_WIDTH)) @ w_out + b_out


def qshift_grid(h, rows):
    bsz, n, d = h.shape
    q = d // 4
    g = h.reshape(bsz, rows, GRID_W, d)
    left = jnp.pad(g[:, :, :-1, :q], ((0, 0), (0, 0), (1, 0), (0, 0)))
    right = jnp.pad(g[:, :, 1:, q:2 * q], ((0, 0), (0, 0), (0, 1), (0, 0)))
    up = jnp.pad(g[:, :-1, :, 2 * q:3 * q], ((0, 0), (1, 0), (0, 0), (0, 0)))
    down = jnp.pad(g[:, 1:, :, 3 * q:], ((0, 0), (0, 1), (0, 0), (0, 0)))
    return jnp.concatenate([left, right, up, down], axis=-1).reshape(bsz, n, d)


def bishift_1d(h):
    half = h.shape[-1] // 2
    prev = jnp.pad(h[:, :-1, :half], ((0, 0), (1, 0), (0, 0)))
    nxt = jnp.pad(h[:, 1:, half:], ((0, 0), (0, 1), (0, 0)))
    return jnp.concatenate([prev, nxt], axis=-1)


def _heads(z):
    return z.reshape(*z.shape[:-1], RWKV_HEADS, RWKV_HEAD)


def _both_dirs(z):
    return jnp.stack([z, z[:, ::-1]])


def _rev_dir1(z):
    return jnp.stack([z[0], z[1][:, ::-1]])


def _scan_layout(z):
    return jnp.moveaxis(_heads(z), 2, 0).astype(jnp.float32)


def rwkv_prepare(h, h_shift, mu, wr, wk, wv, w0, w1, w2, a0, a1, a2, k_k, k_a):
    xx = h_shift - h
    xr, xw, xk, xv, xa = [h + xx * mu[j] for j in range(5)]
    r = xr @ wr
    k = xk @ wk
    v = xv @ wv
    lw = jnp.einsum('zbtr,zrd->zbtd', jnp.tanh(jnp.einsum('btd,zdr->zbtr', xw, w1)), w2)
    w_log = -jax.nn.softplus(-(w0[:, None, None, :] + lw).astype(jnp.float32)) - 0.5
    decay = jnp.exp(-jnp.exp(w_log))
    la = jnp.einsum('zbtr,zrd->zbtd', jnp.einsum('btd,zdr->zbtr', xa, a1), a2)
    a = jax.nn.sigmoid((a0[:, None, None, :] + la).astype(jnp.float32))
    kk = _heads(k * k_k).astype(jnp.float32)
    kk = (kk * lax.rsqrt(jnp.sum(kk * kk, axis=-1, keepdims=True) + 1e-12)).reshape(k.shape)
    k_mod = k[None] * (1 + (a - 1) * k_a)
    scan_in = (_scan_layout(_both_dirs(r)), _scan_layout(_rev_dir1(decay)), _scan_layout(_rev_dir1(k_mod)),
               _scan_layout(_both_dirs(v)), _scan_layout(_both_dirs(-kk)), _scan_layout(_rev_dir1(kk[None] * a)))
    return scan_in, r, k_mod, v, xx


def _wkv_step(S, inp):
    r_t, w_t, k_t, v_t, a_t, b_t = inp
    sa = jnp.einsum('zbhvk,zbhk->zbhv', S, a_t)
    S = S * w_t[..., None, :] + sa[..., :, None] * b_t[..., None, :] + v_t[..., :, None] * k_t[..., None, :]
    return S, jnp.einsum('zbhvk,zbhk->zbhv', S, r_t)


def wkv_bidir(state0, scan_in):
    state, ys = lax.scan(_wkv_step, state0, scan_in)
    ys = jnp.moveaxis(ys, 0, 2)
    return state, ys[0] + ys[1][:, ::-1]


def rwkv_out(y, h, xx, r, k_mod, v, mu, ln_g, ln_b, r_k, g1, g2, wo):
    bsz, n, d = r.shape
    y = layernorm(y, ln_g.reshape(RWKV_HEADS, RWKV_HEAD), ln_b.reshape(RWKV_HEADS, RWKV_HEAD), GN_EPS)
    bonus = jnp.einsum('bthk,zbthk->bth', _heads(r) * r_k, _heads(k_mod))
    y = y.astype(r.dtype) + bonus[..., None] * _heads(v)
    xg = h + xx * mu[5]
    gate = jax.nn.sigmoid(xg @ g1) @ g2
    return (y.reshape(bsz, n, d) * gate) @ wo


def rwkv_mixer(hc, hl, rows, need_ctx_out, mu, wr, wk, wv, wo, w0, w1, w2, a0, a1, a2, g1, g2,
               k_k, k_a, r_k, ln_g, ln_b):
    proj = (mu, wr, wk, wv, w0, w1, w2, a0, a1, a2, k_k, k_a)
    scan_c, r_c, km_c, v_c, xx_c = rwkv_prepare(hc, bishift_1d(hc), *proj)
    scan_l, r_l, km_l, v_l, xx_l = rwkv_prepare(hl, qshift_grid(hl, rows), *proj)
    state0 = jnp.zeros((N_DIR, hc.shape[0], RWKV_HEADS, RWKV_HEAD, RWKV_HEAD), jnp.float32)
    state_c, y_c = wkv_bidir(state0, scan_c)
    _, y_l = wkv_bidir(state_c, scan_l)
    outp = (mu, ln_g, ln_b, r_k, g1, g2, wo)
    out_l = rwkv_out(y_l, hl, xx_l, r_l, km_l, v_l, *outp)
    out_c = rwkv_out(y_c, hc, xx_c, r_c, km_c, v_c, *outp) if need_ctx_out else None
    return out_c, out_l


def hier_moe(h, w_grp, b_grp, w_exp, b_exp, w_gate, w_up, w_down):
    n_tok, d = h.shape
    hf = h.astype(jnp.float32)
    grp_logits = hf @ w_grp.astype(jnp.float32) + b_grp.astype(jnp.float32)
    grp_prob = jax.nn.softmax(grp_logits, axis=-1)
    grp = jnp.argmax(grp_logits, axis=-1)
    p_grp = jnp.take_along_axis(grp_prob, grp[:, None], axis=-1)
    exp_logits = (hf @ w_exp.astype(jnp.float32) + b_exp.astype(jnp.float32)).reshape(n_tok, N_GROUPS, EXPERTS_PER_GROUP)
    exp_logits = jnp.take_along_axis(exp_logits, grp[:, None, None], axis=1)[:, 0]
    top_logit, top_idx = lax.top_k(exp_logits, TOP_K)
    weights = (p_grp * jax.nn.softmax(top_logit, axis=-1)).astype(h.dtype)
    expert_id = (grp[:, None] * EXPERTS_PER_GROUP + top_idx).reshape(-1)
    n_assign = n_tok * TOP_K
    order = jnp.argsort(expert_id)
    sorted_e = expert_id[order]
    token_of = order // TOP_K
    counts = jnp.bincount(expert_id, length=N_EXPERTS)
    padded = ((counts + MOE_BLOCK - 1) // MOE_BLOCK) * MOE_BLOCK
    pad_end = jnp.cumsum(padded)
    pad_start = pad_end - padded
    raw_start = jnp.cumsum(counts) - counts
    dest = pad_start[sorted_e] + (jnp.arange(n_assign) - raw_start[sorted_e])
    n_blocks = (n_assign + MOE_BLOCK - 1) // MOE_BLOCK + N_EXPERTS
    cap = n_blocks * MOE_BLOCK
    buf = jnp.zeros((cap, d), h.dtype).at[dest].set(h[token_of])
    block_expert = jnp.minimum(jnp.searchsorted(pad_end, jnp.arange(n_blocks) * MOE_BLOCK, side='right'),
                               N_EXPERTS - 1)

    def expert_block(args):
        xb, e = args
        return (jax.nn.silu(xb @ w_gate[e]) * (xb @ w_up[e])) @ w_down[e]

    out = lax.map(expert_block, (buf.reshape(n_blocks, MOE_BLOCK, d), block_expert)).reshape(cap, d)
    y = jnp.zeros((n_assign, d), h.dtype).at[order].set(out[dest]).reshape(n_tok, TOP_K, d)
    return jnp.einsum('tk,tkd->td', weights, y)


def setup_inputs(seed: int = 0) -> dict:
    key = jax.random.key(seed)
    keys = iter(jax.random.split(key, 48))
    f32 = jnp.float32

    def nrm(shape, scale):
        return scale * jax.random.normal(next(keys), shape, f32)

    def gain(shape):
        return 1.0 + nrm(shape, 0.1)

    def unif(shape, lo, hi):
        return jax.random.uniform(next(keys), shape, f32, lo, hi)

    D, W, F = D_MODEL, GMLP_WIDTH, EXPERT_HIDDEN
    NA, NB = N_LAYERS_A, N_LAYERS_B
    return {
        'x': nrm((BATCH, SEQ, D), 1.0),
        'c': nrm((BATCH, D), 1.0),
        'ctx': nrm((BATCH, CTX_LEN, D), 1.0),
        'c_ctx': nrm((D,), 1.0),
        'ada_w': nrm((DEPTH, D, N_MOD * D), 0.5 * D ** -0.5),
        'ada_b': nrm((DEPTH, N_MOD * D), 0.05),
        'norm1_g': gain((DEPTH, D)),
        'norm2_g': gain((DEPTH, D)),
        'final_g': gain((D,)),
        'ga_w_in': nrm((NA, D, 2 * W), D ** -0.5),
        'ga_b_in': nrm((NA, 2 * W), 0.02),
        'ga_ln_g': gain((NA, W)),
        'ga_ln_b': nrm((NA, W), 0.02),
        'ga_w_s': nrm((NA, GMLP_GROUPS, CHUNK, CHUNK), 0.5 * CHUNK ** -0.5),
        'ga_b_s': gain((NA, GMLP_GROUPS, CHUNK)),
        'ga_w_out': nrm((NA, W, D), W ** -0.5),
        'ga_b_out': nrm((NA, D), 0.02),
        'rw_mu': unif((NB, N_MOD, D), 0.0, 1.0),
        'rw_wr': nrm((NB, D, D), D ** -0.5),
        'rw_wk': nrm((NB, D, D), D ** -0.5),
        'rw_wv': nrm((NB, D, D), D ** -0.5),
        'rw_wo': nrm((NB, D, D), D ** -0.5),
        'rw_w0': unif((NB, N_DIR, D), -6.0, 0.0),
        'rw_w1': nrm((NB, N_DIR, D, DECAY_LORA), 0.5 * D ** -0.5),
        'rw_w2': nrm((NB, N_DIR, DECAY_LORA, D), 0.5 * DECAY_LORA ** -0.5),
        'rw_a0': nrm((NB, N_DIR, D), 0.5),
        'rw_a1': nrm((NB, N_DIR, D, AAA_LORA), 0.5 * D ** -0.5),
        'rw_a2': nrm((NB, N_DIR, AAA_LORA, D), 0.5 * AAA_LORA ** -0.5),
        'rw_g1': nrm((NB, D, GATE_LORA), D ** -0.5),
        'rw_g2': nrm((NB, GATE_LORA, D), GATE_LORA ** -0.5),
        'rw_k_k': 0.85 + nrm((NB, D), 0.05),
        'rw_k_a': gain((NB, D)),
        'rw_r_k': nrm((NB, RWKV_HEADS, RWKV_HEAD), 0.1),
        'rw_ln_g': gain((NB, D)),
        'rw_ln_b': nrm((NB, D), 0.02),
        'moe_w_grp': nrm((DEPTH, D, N_GROUPS), D ** -0.5),
        'moe_b_grp': nrm((DEPTH, N_GROUPS), 0.01),
        'moe_w_exp': nrm((DEPTH, D, N_EXPERTS), D ** -0.5),
        'moe_b_exp': nrm((DEPTH, N_EXPERTS), 0.01),
        'moe_w_gate': nrm((DEPTH, N_EXPERTS, D, F), D ** -0.5),
        'moe_w_up': nrm((DEPTH, N_EXPERTS, D, F), D ** -0.5),
        'moe_w_down': nrm((DEPTH, N_EXPERTS, F, D), F ** -0.5),
    }


def reference(x, c, ctx, c_ctx, ada_w, ada_b, norm1_g, norm2_g, final_g,
              ga_w_in, ga_b_in, ga_ln_g, ga_ln_b, ga_w_s, ga_b_s, ga_w_out, ga_b_out,
              rw_mu, rw_wr, rw_wk, rw_wv, rw_wo, rw_w0, rw_w1, rw_w2, rw_a0, rw_a1, rw_a2,
              rw_g1, rw_g2, rw_k_k, rw_k_a, rw_r_k, rw_ln_g, rw_ln_b,
              moe_w_grp, moe_b_grp, moe_w_exp, moe_b_exp, moe_w_gate, moe_w_up, moe_w_down):
    bsz, n, d = x.shape
    rows = n // GRID_W
    xc = ctx
    silu_c = jax.nn.silu(c)
    silu_cc = jax.nn.silu(c_ctx)
    for i in range(DEPTH):
        last = i == DEPTH - 1
        j = i // N_MIXERS
        mod_l = silu_c @ ada_w[i] + ada_b[i]
        mod_c = silu_cc @ ada_w[i] + ada_b[i]
        sh1, sc1, gt1, sh2, sc2, gt2 = jnp.split(mod_l[:, None, :], N_MOD, axis=-1)
        csh1, csc1, cgt1, csh2, csc2, cgt2 = jnp.split(mod_c, N_MOD, axis=-1)
        hl = rmsnorm(x, norm1_g[i]) * (1 + sc1) + sh1
        hc = rmsnorm(xc, norm1_g[i]) * (1 + csc1) + csh1
        if i % N_MIXERS == 0:
            gp = (ga_w_in[j], ga_b_in[j], ga_ln_g[j], ga_ln_b[j], ga_w_s[j], ga_b_s[j], ga_w_out[j], ga_b_out[j])
            yl = gmlp_mixer(hl, *gp)
            yc = None if last else gmlp_mixer(hc, *gp)
        else:
            yc, yl = rwkv_mixer(hc, hl, rows, not last, rw_mu[j], rw_wr[j], rw_wk[j], rw_wv[j], rw_wo[j],
                                rw_w0[j], rw_w1[j], rw_w2[j], rw_a0[j], rw_a1[j], rw_a2[j], rw_g1[j], rw_g2[j],
                                rw_k_k[j], rw_k_a[j], rw_r_k[j], rw_ln_g[j], rw_ln_b[j])
        x = x + gt1 * yl
        hl2 = rmsnorm(x, norm2_g[i]) * (1 + sc2) + sh2
        mp = (moe_w_grp[i], moe_b_grp[i], moe_w_exp[i], moe_b_exp[i], moe_w_gate[i], moe_w_up[i], moe_w_down[i])
        if last:
            x = x + gt2 * hier_moe(hl2.reshape(-1, d), *mp).reshape(x.shape)
        else:
            xc = xc + cgt1 * yc
            hc2 = rmsnorm(xc, norm2_g[i]) * (1 + csc2) + csh2
            out = hier_moe(jnp.concatenate([hl2.reshape(-1, d), hc2.reshape(-1, d)], axis=0), *mp)
            x = x + gt2 * out[:bsz * n].reshape(x.shape)
            xc = xc + cgt2 * out[bsz * n:].reshape(xc.shape)
    return rmsnorm(x, final_g)
```

```python
from contextlib import ExitStack
import numpy as np
import concourse.bass as bass
import concourse.mybir as mybir
from concourse.bass_utils import run_bass_kernel_spmd

F32 = mybir.dt.float32
BF16 = mybir.dt.bfloat16
AF = mybir.ActivationFunctionType
ALU = mybir.AluOpType
AX = mybir.AxisListType

COMPUTE = ("tensor", "vector", "scalar", "gpsimd")
NDMASEM = 16
STRICT = False
NCORES = 8
D = 1024


class Buf:
    __slots__ = ("name", "lw", "rd")

    def __init__(self, name=""):
        self.name = name
        self.lw = None
        self.rd = []


class Sched:
    def __init__(self, nc):
        self.nc = nc
        self.ops = []
        self.stack = ExitStack()
        self.ndma = 0

    def sb(self, name, shape, dt):
        t = self.stack.enter_context(self.nc.sbuf_tensor("s_" + name, shape, dt))
        return t

    def ps(self, name, shape, dt):
        return self.stack.enter_context(self.nc.psum_tensor("p_" + name, shape, dt))

    def _deps(self, r, w):
        d = set()
        raw = set()
        for b in r:
            if b.lw is not None:
                d.add(b.lw)
                raw.add(b.lw)
        for b in w:
            if b.lw is not None:
                d.add(b.lw)
            d.update(b.rd)
        self._raw = set(d) if STRICT else raw
        return d

    def _commit(self, idx, r, w, eng, kind):
        for b in r:
            if kind == "c":
                b.rd = [i for i in b.rd if not (self.ops[i]["kind"] == "c" and self.ops[i]["eng"] == eng)]
            b.rd.append(idx)
        for b in w:
            b.lw = idx
            b.rd = []

    def op(self, eng, fn, r=(), w=()):
        idx = len(self.ops)
        self.ops.append(dict(eng=eng, fn=fn, deps=self._deps(r, w), kind="c", sig=False))
        self.ops[-1]["raw"] = self._raw
        self._commit(idx, r, w, eng, "c")
        return idx

    def dma(self, eng, out, in_, r=(), w=(), **kw):
        idx = len(self.ops)
        k = self.ndma
        self.ndma += 1
        self.ops.append(dict(eng=eng, fn=None, out=out, in_=in_, kw=kw, deps=self._deps(r, w),
                             kind="d", k=k, sig=True))
        self.ops[-1]["raw"] = self._raw
        self._commit(idx, r, w, eng, "d")
        return idx

    def emit(self, final_wait_ops=()):
        nc = self.nc
        ops = self.ops
        for i, o in enumerate(ops):
            for d in o["deps"]:
                od = ops[d]
                if od["kind"] == "c" and (od["eng"] != o["eng"] or o["kind"] == "d"
                                          or (d in o["raw"] and od["eng"] != "tensor")):
                    od["sig"] = True
        cnt = {e: 0 for e in COMPUTE}
        for o in ops:
            if o["kind"] == "c" and o["sig"]:
                cnt[o["eng"]] += 1
                o["sv"] = cnt[o["eng"]]
        NS = 2 * NDMASEM
        dcnt = [0] * NS
        prev_on_sem = [None] * NS
        qcount = {"sync": 0, "gpsimd": 0}
        for i, o in enumerate(ops):
            if o["kind"] == "d":
                qi = qcount[o["eng"]]
                qcount[o["eng"]] += 1
                s = (qi % NDMASEM) + (NDMASEM if o["eng"] == "gpsimd" else 0)
                dcnt[s] += 1
                o["ds"] = s
                o["dv"] = 16 * dcnt[s]
                o["prev"] = prev_on_sem[s]
                prev_on_sem[s] = i
        with ExitStack() as st:
            csem = {e: st.enter_context(nc.semaphore("cs_" + e)) for e in COMPUTE}
            dsem = [st.enter_context(nc.semaphore("ds_%d" % i)) for i in range(2 * NDMASEM)]
            block = st.enter_context(nc.Block())
            engines = ["tensor", "vector", "scalar", "gpsimd", "sync"]
            by_eng = {e: [] for e in engines}
            for i, o in enumerate(ops):
                by_eng[o["eng"]].append(i)
            finals = list(final_wait_ops)

            def make(ename):
                def body(e):
                    waited = {}

                    def wait(key, sem, val):
                        if waited.get(key, 0) >= val:
                            return
                        waited[key] = val
                        e.wait_ge(sem, val)

                    for i in by_eng[ename]:
                        o = ops[i]
                        for d in sorted(o["deps"]):
                            od = ops[d]
                            if od["kind"] == "c":
                                if od["eng"] == ename and o["kind"] == "c" and (
                                        d not in o["raw"] or ename == "tensor"):
                                    continue
                                wait(("c", od["eng"]), csem[od["eng"]], od["sv"])
                            else:
                                wait(("d", od["ds"]), dsem[od["ds"]], od["dv"])
                        if o["kind"] == "d":
                            if o["prev"] is not None:
                                op_ = ops[o["prev"]]
                                wait(("d", op_["ds"]), dsem[op_["ds"]], op_["dv"])
                            ins = e.dma_start(out=o["out"], in_=o["in_"], **o["kw"])
                            ins.then_inc(dsem[o["ds"]], 16)
                        else:
                            ins = o["fn"](e)
                            if o["sig"]:
                                ins.then_inc(csem[ename], 1)
                    if ename == "sync":
                        for i in finals:
                            od = ops[i]
                            wait(("d", od["ds"]), dsem[od["ds"]], od["dv"])
                return body

            block.tensor(make("tensor"))
            block.vector(make("vector"))
            block.scalar(make("scalar"))
            block.gpsimd(make("gpsimd"))
            block.sync(make("sync"))
        self.stack.close()


def new_nc():
    return bass.Bass("TRN2", target_bir_lowering=False)


def dram_in(nc, name, shape, dt=F32):
    return nc.dram_tensor(name, list(shape), dt, kind="ExternalInput").ap()


def dram_out(nc, name, shape, dt=F32):
    return nc.dram_tensor(name, list(shape), dt, kind="ExternalOutput").ap()


def cols(v):
    return np.ascontiguousarray(np.asarray(v, np.float32).reshape(-1, 128).T)


def bcast(v, p=128):
    v = np.asarray(v, np.float32).reshape(1, -1)
    return np.ascontiguousarray(np.broadcast_to(v, (p, v.shape[1])))


_MOD_NC = None


def build_mod():
    nc = new_nc()
    NH = 3072
    cT = dram_in(nc, "cT", [128, 8, 3])
    aw = dram_in(nc, "aw", [1024, NH])
    ab = dram_in(nc, "ab", [3, NH])
    out = dram_out(nc, "mod", [3, NH])
    S = Sched(nc)
    ct = S.sb("ct", [128, 8, 3], F32); bct = Buf()
    sg = S.sb("sg", [128, 8, 3], F32)
    wt = S.sb("wt", [128, 8, NH], F32); bwt = [Buf() for _ in range(8)]
    bt = S.sb("bt", [3, NH], F32); bbt = Buf()
    ot = S.sb("ot", [3, NH], F32); bot = Buf()
    pss = [S.ps("ps%d" % i, [128, 512], F32) for i in range(2)]; bps = [Buf() for _ in range(2)]
    S.dma("sync", ct[:], cT, w=[bct])
    S.dma("sync", bt[:], ab, w=[bbt])
    awv = aw.rearrange("(c p) n -> p c n", p=128)
    for c in range(8):
        S.dma("sync" if c % 2 == 0 else "gpsimd", wt[:, c, :], awv[:, c, :], w=[bwt[c]])
    S.op("scalar", lambda e: e.activation(sg[:], ct[:], AF.Sigmoid), r=[bct], w=[bot])
    S.op("vector", lambda e: e.tensor_tensor(ct[:], ct[:], sg[:], ALU.mult), r=[bot, bct], w=[bct])
    for n in range(NH // 512):
        p = pss[n % 2]; bp = bps[n % 2]
        for c in range(8):
            S.op("tensor", lambda e, p=p, c=c, n=n: e.matmul(p[0:3, :], ct[:, c, :], wt[:, c, n * 512:(n + 1) * 512],
                                                              start=(c == 0), stop=(c == 7)),
                 r=[bct, bwt[c]], w=[bp])
        S.op("vector", lambda e, p=p, n=n: e.tensor_tensor(ot[:, n * 512:(n + 1) * 512], p[0:3, :],
                                                           bt[:, n * 512:(n + 1) * 512], ALU.add),
             r=[bp, bbt], w=[bot])
    o = S.dma("sync", out, ot[:], r=[bot])
    S.emit([o])
    return nc


def run_mod(c, c_ctx, ada_w, ada_b):
    global _MOD_NC
    if _MOD_NC is None:
        _MOD_NC = build_mod()
    cvec = np.stack([c[0], c[1], c_ctx], 0).astype(np.float32)
    cT = np.ascontiguousarray(cvec.reshape(3, 8, 128).transpose(2, 1, 0))
    maps = []
    for k in range(NCORES):
        l, h = k // 2, k % 2
        maps.append(dict(cT=cT, aw=np.ascontiguousarray(ada_w[l][:, h * 3072:(h + 1) * 3072]),
                         ab=bcast(ada_b[l][h * 3072:(h + 1) * 3072], 3)))
    res = run_bass_kernel_spmd(_MOD_NC, maps, core_ids=list(range(NCORES)))
    mods = np.zeros((4, 3, 6144), np.float32)
    for k in range(NCORES):
        l, h = k // 2, k % 2
        mods[l, :, h * 3072:(h + 1) * 3072] = res.results[k]["mod"]
    return mods


NLAT = 4096
NCTX = 64
NTOK = NLAT + NCTX
NEXP = 32
FH = 512


def token_tiles(ntok):
    tiles = []
    o = 0
    while o < ntok:
        p = min(128, ntok - o)
        tiles.append((o, p))
        o += p
    return tiles


def build_moe(final=False, ntok=NTOK, nexp=NEXP, dbg=False):
    nc = new_nc()
    xin = dram_in(nc, "xin", [ntok, D])
    wr_d = dram_in(nc, "wr", [128, 8, 36])
    br_d = dram_in(nc, "br", [128, 36])
    mc_d = dram_in(nc, "mcols", [128, 6, 8])
    gt_d = dram_in(nc, "gtb", [128, 2, D])
    fg_d = dram_in(nc, "fgb", [128, D])
    id_d = dram_in(nc, "ident", [128, 128])
    wg_d = dram_in(nc, "wg", [nexp, D, FH])
    wu_d = dram_in(nc, "wu", [nexp, D, FH])
    wd_d = dram_in(nc, "wd", [nexp, FH, D])
    xout = dram_out(nc, "xout", [ntok, D])
    S = Sched(nc)
    tiles = token_tiles(ntok)
    nt = len(tiles)
    sts = []
    i = 0
    while i < nt:
        j = min(i + 8, nt)
        if nt - j < 4:
            j = nt
        sts.append(list(range(i, j)))
        i = j
    MAXT = max(len(s) for s in sts)
    MAXTOK = MAXT * 128

    ident = S.sb("ident", [128, 128], F32); b_id = Buf()
    wr = S.sb("wr", [128, 8, 36], F32); b_wr = Buf()
    br = S.sb("br", [128, 36], F32); b_br = Buf()
    mc = S.sb("mc", [128, 6, 8], F32); b_mc = Buf()
    gsc = S.sb("gsc", [128, 2, 8], F32); b_gsc = Buf()
    gtb = S.sb("gtb", [128, 2, D], F32); b_gt = Buf()
    fgb = S.sb("fgb", [128, D], F32); b_fg = Buf()
    hTb = S.sb("hTb", [128, 8, MAXTOK], BF16); b_hTb = [Buf() for _ in range(MAXT)]
    acc = S.sb("acc", [128, MAXT, D], F32); b_acc = [Buf() for _ in range(MAXT)]
    coef = S.sb("coef", [128, MAXT, 32], F32); b_coef = [Buf() for _ in range(MAXT)]
    HT = [S.sb("HT%d" % i, [128, 4, 512], BF16) for i in range(2)]; b_HT = [[Buf() for _ in range(4)] for _ in range(2)]
    sgt = [S.sb("sgt%d" % i, [128, 512], F32) for i in range(2)]; b_sg = [Buf() for _ in range(2)]
    wgs = [S.sb("wgs%d" % i, [128, 8, FH], BF16) for i in range(2)]; b_wg = [Buf() for _ in range(2)]
    wus = [S.sb("wus%d" % i, [128, 8, FH], BF16) for i in range(2)]; b_wu = [Buf() for _ in range(2)]
    wds = [S.sb("wds%d" % i, [128, 4, D], BF16) for i in range(2)]; b_wd = [Buf() for _ in range(2)]
    xt = [S.sb("xt%d" % i, [128, D], F32) for i in range(2)]; b_xt = [Buf() for _ in range(2)]
    xs = [S.sb("xs%d" % i, [128, D], F32) for i in range(2)]; b_xs = [Buf() for _ in range(2)]
    hTf = [S.sb("hTf%d" % i, [128, 8, 128], F32) for i in range(2)]; b_hTf = [Buf() for _ in range(2)]
    sm = [S.sb("sm%d" % i, [128, 96], F32) for i in range(2)]; b_sm = [Buf() for _ in range(2)]
    PS = [S.ps("psb%d" % i, [128, 512], F32) for i in range(8)]; b_ps = [Buf() for _ in range(8)]

    S.dma("sync", ident[:], id_d, w=[b_id])
    S.dma("sync", wr[:], wr_d, w=[b_wr])
    S.dma("sync", br[:], br_d, w=[b_br])
    S.dma("sync", mc[:], mc_d, w=[b_mc])
    S.dma("sync", gtb[:], gt_d, w=[b_gt])
    if final:
        S.dma("sync", fgb[:], fg_d, w=[b_fg])
    for v in range(2):
        S.op("vector", lambda e, v=v: e.scalar_tensor_tensor(gsc[:, v, :], mc[:, 1 + 2 * v, :], 1.0, mc[:, 0, :],
                                                              ALU.add, ALU.mult), r=[b_mc], w=[b_gsc])

    wstate = dict(next=0)
    n_items_total = len(sts) * nexp

    def load_weights(k):
        e = k % nexp
        par = k % 2
        S.dma("gpsimd", wgs[par][:], wg_d[e].rearrange("(c p) n -> p c n", p=128), w=[b_wg[par]])
        S.dma("gpsimd", wus[par][:], wu_d[e].rearrange("(c p) n -> p c n", p=128), w=[b_wu[par]])
        S.dma("gpsimd", wds[par][:], wd_d[e].rearrange("(c p) n -> p c n", p=128), w=[b_wd[par]])

    load_weights(0)
    load_weights(1)
    wk = 2

    tcount = 0
    out_ops = []
    for si, st in enumerate(sts):
        ntl = len(st)
        loff = []
        o = 0
        for t in st:
            loff.append(o)
            o += tiles[t][1]
        stok = o
        for li, t in enumerate(st):
            off, P = tiles[t]
            par = tcount % 2
            tcount += 1
            v = 1 if off >= NLAT and ntok > NLAT else 0
            S.dma("sync", xt[par][0:P, :], xin[off:off + P, :], w=[b_xt[par]])
            ss = sm[par][0:P, 0:1]; rstd = sm[par][0:P, 1:2]
            S.op("scalar", lambda e, par=par, P=P, ss=ss: e.activation(xs[par][0:P, :], xt[par][0:P, :], AF.Square, accum_out=ss),
                 r=[b_xt[par]], w=[b_xs[par], b_sm[par]])
            S.op("vector", lambda e, ss=ss, rstd=rstd: e.tensor_scalar(rstd, ss, 1.0 / D, 1e-6, ALU.mult, ALU.add),
                 r=[b_sm[par]], w=[b_sm[par]])
            S.op("scalar", lambda e, rstd=rstd: e.activation(rstd, rstd, AF.Sqrt), r=[b_sm[par]], w=[b_sm[par]])
            S.op("vector", lambda e, rstd=rstd: e.reciprocal(rstd, rstd), r=[b_sm[par]], w=[b_sm[par]])
            S.op("vector", lambda e, par=par, P=P, rstd=rstd: e.tensor_scalar(xs[par][0:P, :], xt[par][0:P, :], rstd, None, ALU.mult),
                 r=[b_xt[par], b_sm[par]], w=[b_xs[par]])
            for c in range(8):
                pb = c // 4
                S.op("tensor", lambda e, par=par, P=P, c=c, pb=pb: e.transpose(PS[pb][:, (c % 4) * 128:(c % 4) * 128 + P],
                                                                                xs[par][0:P, c * 128:(c + 1) * 128], ident[0:P, 0:P]),
                     r=[b_xs[par], b_id], w=[b_ps[pb]])
            for c in range(8):
                pb = c // 4
                S.op("scalar", lambda e, par=par, P=P, c=c, pb=pb, v=v: e.activation(
                    hTf[par][:, c, 0:P], PS[pb][:, (c % 4) * 128:(c % 4) * 128 + P], AF.Identity,
                    bias=mc[:, 2 + 2 * v, c:c + 1], scale=gsc[:, v, c:c + 1]),
                    r=[b_ps[pb], b_mc, b_gsc], w=[b_hTf[par]])
            S.op("vector", lambda e, par=par, P=P, lo=loff[li]: e.tensor_copy(hTb[:, :, lo:lo + P], hTf[par][:, :, 0:P]),
                 r=[b_hTf[par]], w=[b_hTb[li]])
            for c in range(8):
                S.op("tensor", lambda e, par=par, P=P, c=c: e.matmul(PS[2][0:P, 0:36], hTf[par][:, c, 0:P], wr[:, c, :],
                                                                     start=(c == 0), stop=(c == 7)),
                     r=[b_hTf[par], b_wr], w=[b_ps[2]])
            m = sm[par]
            lg = m[0:P, 8:44]; gl = m[0:P, 8:12]
            gmax = m[0:P, 2:3]; ngmax = m[0:P, 3:4]; gsum = m[0:P, 4:5]; pg = m[0:P, 5:6]
            goh = m[0:P, 44:48]; sel = m[0:P, 48:56]; m8 = m[0:P, 56:64]; gex = m[0:P, 64:68]
            t1 = m[0:P, 68:76]; t2 = m[0:P, 76:84]
            dd = m[0:P, 84:85]; e2 = m[0:P, 85:86]; w1 = m[0:P, 86:87]; w2 = m[0:P, 87:88]
            bsm = [b_sm[par]]

            def V(fn, r=(), w=()):
                S.op("vector", fn, r=list(r) + bsm, w=list(w) + bsm)

            V(lambda e, lg=lg, P=P: e.tensor_tensor(lg, PS[2][0:P, 0:36], br[0:P, :], ALU.add), r=[b_ps[2], b_br])
            V(lambda e, gmax=gmax, gl=gl: e.reduce_max(gmax, gl, AX.X))
            V(lambda e, ngmax=ngmax, gmax=gmax: e.tensor_scalar(ngmax, gmax, -1.0, None, ALU.mult))
            S.op("scalar", lambda e, gex=gex, gl=gl, ngmax=ngmax, gsum=gsum: e.activation(gex, gl, AF.Exp, bias=ngmax, scale=1.0, accum_out=gsum),
                 r=bsm, w=bsm)
            V(lambda e, pg=pg, gsum=gsum: e.reciprocal(pg, gsum))
            V(lambda e, goh=goh, gl=gl, gmax=gmax: e.tensor_scalar(goh, gl, gmax, None, ALU.is_ge))
            V(lambda e, sel=sel, m=m, P=P, goh=goh: e.tensor_scalar(sel, m[0:P, 12:20], goh[:, 0:1], None, ALU.mult))
            for g in range(1, 4):
                V(lambda e, sel=sel, m=m, P=P, goh=goh, g=g: e.scalar_tensor_tensor(sel, m[0:P, 12 + 8 * g:20 + 8 * g], goh[:, g:g + 1], sel,
                                                                                     ALU.mult, ALU.add))
            V(lambda e, m8=m8, sel=sel: e.max(m8, sel))
            V(lambda e, dd=dd, m8=m8: e.tensor_tensor(dd, m8[:, 1:2], m8[:, 0:1], ALU.subtract))
            S.op("scalar", lambda e, e2=e2, dd=dd: e.activation(e2, dd, AF.Exp), r=bsm, w=bsm)
            V(lambda e, w1=w1, e2=e2: e.tensor_scalar(w1, e2, 1.0, None, ALU.add))
            V(lambda e, w1=w1: e.reciprocal(w1, w1))
            V(lambda e, w2=w2, e2=e2, w1=w1: e.tensor_tensor(w2, e2, w1, ALU.mult))
            V(lambda e, w1=w1, pg=pg: e.tensor_tensor(w1, w1, pg, ALU.mult))
            V(lambda e, w2=w2, pg=pg: e.tensor_tensor(w2, w2, pg, ALU.mult))
            V(lambda e, t1=t1, sel=sel, m8=m8, w1=w1: e.tensor_scalar(t1, sel, m8[:, 0:1], w1, ALU.is_equal, ALU.mult))
            V(lambda e, t2=t2, sel=sel, m8=m8, w2=w2: e.tensor_scalar(t2, sel, m8[:, 1:2], w2, ALU.is_equal, ALU.mult))
            V(lambda e, t1=t1, t2=t2: e.tensor_tensor(t1, t1, t2, ALU.add))
            for g in range(4):
                V(lambda e, li=li, P=P, g=g, t1=t1, goh=goh: e.tensor_scalar(coef[0:P, li, 8 * g:8 * g + 8], t1, goh[:, g:g + 1], None, ALU.mult),
                  w=[b_coef[li]])
        chunks = []
        li = 0
        while li < ntl:
            lj = li
            n = 0
            while lj < ntl and n + tiles[st[lj]][1] <= 512:
                n += tiles[st[lj]][1]
                lj += 1
            chunks.append((li, lj, loff[li], n))
            li = lj
        items = [(e, q) for e in range(nexp) for q in range(len(chunks))]

        def GU(idx):
            e, q = items[idx]
            l0, l1, to, n = chunks[q]
            kk = si * nexp + e
            par = kk % 2
            hp = idx % 2
            for j in range(4):
                pg_ = PS[(2 * j) % 4]; pu_ = PS[(2 * j) % 4 + 1]
                bg_ = b_ps[(2 * j) % 4]; bu_ = b_ps[(2 * j) % 4 + 1]
                for c in range(8):
                    S.op("tensor", lambda e_, par=par, c=c, j=j, pg_=pg_, to=to, n=n: e_.matmul(
                        pg_[:, 0:n], wgs[par][:, c, j * 128:(j + 1) * 128], hTb[:, c, to:to + n], start=(c == 0), stop=(c == 7)),
                        r=[b_wg[par]] + b_hTb[l0:l1], w=[bg_])
                for c in range(8):
                    S.op("tensor", lambda e_, par=par, c=c, j=j, pu_=pu_, to=to, n=n: e_.matmul(
                        pu_[:, 0:n], wus[par][:, c, j * 128:(j + 1) * 128], hTb[:, c, to:to + n], start=(c == 0), stop=(c == 7)),
                        r=[b_wu[par]] + b_hTb[l0:l1], w=[bu_])
                sp = j % 2
                S.op("scalar", lambda e_, sp=sp, pg_=pg_, n=n: e_.activation(sgt[sp][:, 0:n], pg_[:, 0:n], AF.Silu),
                     r=[bg_], w=[b_sg[sp]])
                S.op("vector", lambda e_, sp=sp, pu_=pu_, n=n, hp=hp, j=j: e_.tensor_tensor(HT[hp][:, j, 0:n], sgt[sp][:, 0:n], pu_[:, 0:n], ALU.mult),
                     r=[b_sg[sp], bu_], w=[b_HT[hp][j]])

        dstate = dict(k=0)

        def DN(idx):
            e, q = items[idx]
            l0, l1, to, n = chunks[q]
            kk = si * nexp + e
            par = kk % 2
            hp = idx % 2
            for li in range(l0, l1):
                P = tiles[st[li]][1]
                lo = loff[li] - to
                for h in range(2):
                    pb = 4 + dstate["k"] % 4
                    dstate["k"] += 1
                    for j in range(4):
                        S.op("tensor", lambda e_, pb=pb, P=P, hp=hp, j=j, lo=lo, par=par, h=h: e_.matmul(
                            PS[pb][0:P, :], HT[hp][:, j, lo:lo + P], wds[par][:, j, h * 512:(h + 1) * 512], start=(j == 0), stop=(j == 3)),
                            r=[b_HT[hp][j], b_wd[par]], w=[b_ps[pb]])
                    if e == 0:
                        S.op("vector", lambda e_, pb=pb, P=P, li=li, h=h, e=e: e_.tensor_scalar(
                            acc[0:P, li, h * 512:(h + 1) * 512], PS[pb][0:P, :], coef[0:P, li, e:e + 1], None, ALU.mult),
                            r=[b_ps[pb], b_coef[li]], w=[b_acc[li]])
                    else:
                        S.op("vector", lambda e_, pb=pb, P=P, li=li, h=h, e=e: e_.scalar_tensor_tensor(
                            acc[0:P, li, h * 512:(h + 1) * 512], PS[pb][0:P, :], coef[0:P, li, e:e + 1],
                            acc[0:P, li, h * 512:(h + 1) * 512], ALU.mult, ALU.add),
                            r=[b_ps[pb], b_coef[li]], w=[b_acc[li]])

        nonlocal_wk = [wk]
        for idx in range(len(items)):
            GU(idx)
            if idx > 0:
                DN(idx - 1)
                e_prev, q_prev = items[idx - 1]
                if q_prev == len(chunks) - 1 and nonlocal_wk[0] < n_items_total:
                    load_weights(nonlocal_wk[0])
                    nonlocal_wk[0] += 1
        DN(len(items) - 1)
        if nonlocal_wk[0] < n_items_total:
            load_weights(nonlocal_wk[0])
            nonlocal_wk[0] += 1
        wk = nonlocal_wk[0]
        for li, t in enumerate(st):
            off, P = tiles[t]
            par = tcount % 2
            tcount += 1
            v = 1 if off >= NLAT and ntok > NLAT else 0
            S.dma("sync", xt[par][0:P, :], xin[off:off + P, :], w=[b_xt[par]])
            S.op("vector", lambda e, P=P, li=li, v=v: e.tensor_tensor(acc[0:P, li, :], acc[0:P, li, :], gtb[0:P, v, :], ALU.mult),
                 r=[b_gt], w=[b_acc[li]])
            S.op("vector", lambda e, P=P, li=li, par=par: e.tensor_tensor(acc[0:P, li, :], acc[0:P, li, :], xt[par][0:P, :], ALU.add),
                 r=[b_xt[par]], w=[b_acc[li]])
            if final:
                ss = sm[par][0:P, 0:1]; rstd = sm[par][0:P, 1:2]
                S.op("scalar", lambda e, par=par, P=P, ss=ss, li=li: e.activation(xs[par][0:P, :], acc[0:P, li, :], AF.Square, accum_out=ss),
                     r=[b_acc[li]], w=[b_xs[par], b_sm[par]])
                S.op("vector", lambda e, ss=ss, rstd=rstd: e.tensor_scalar(rstd, ss, 1.0 / D, 1e-6, ALU.mult, ALU.add),
                     r=[b_sm[par]], w=[b_sm[par]])
                S.op("scalar", lambda e, rstd=rstd: e.activation(rstd, rstd, AF.Sqrt), r=[b_sm[par]], w=[b_sm[par]])
                S.op("vector", lambda e, rstd=rstd: e.reciprocal(rstd, rstd), r=[b_sm[par]], w=[b_sm[par]])
                S.op("vector", lambda e, P=P, li=li, rstd=rstd: e.scalar_tensor_tensor(acc[0:P, li, :], acc[0:P, li, :], rstd, fgb[0:P, :],
                                                                                      ALU.mult, ALU.mult),
                     r=[b_sm[par], b_fg], w=[b_acc[li]])
            out_ops.append(S.dma("sync", xout[off:off + P, :], acc[0:P, li, :], r=[b_acc[li]], w=[]))
    if dbg:
        d_sm = dram_out(nc, "d_sm", [128, 96]); d_coef = dram_out(nc, "d_coef", [128, 32])
        d_hTf = dram_out(nc, "d_hTf", [128, 8, 128]); d_hTb = dram_out(nc, "d_hTb", [128, 8, MAXTOK], BF16)
        d_HT = dram_out(nc, "d_HT", [128, 4, 512], BF16); d_wg = dram_out(nc, "d_wg", [128, 8, FH], BF16)
        out_ops.append(S.dma("sync", d_sm, sm[0][:], r=[b_sm[0]]))
        out_ops.append(S.dma("sync", d_coef, coef[:, 0, :], r=[b_coef[0]]))
        out_ops.append(S.dma("sync", d_hTf, hTf[0][:], r=[b_hTf[0]]))
        out_ops.append(S.dma("sync", d_hTb, hTb[:], r=b_hTb))
        out_ops.append(S.dma("sync", d_HT, HT[0][:], r=b_HT[0]))
        out_ops.append(S.dma("sync", d_wg, wgs[0][:], r=[b_wg[0]]))
    S.emit(out_ops)
    return nc


_MOE_NC = {}


def moe_maps(xs_per_core, i, mods, inp, nexp=NEXP):
    wr = np.concatenate([inp["moe_w_grp"][i], inp["moe_w_exp"][i]], axis=1)
    wr = np.ascontiguousarray(wr.reshape(8, 128, 36).transpose(1, 0, 2))
    br = bcast(np.concatenate([inp["moe_b_grp"][i], inp["moe_b_exp"][i]]))
    ident = np.eye(128, dtype=np.float32)
    fgb = bcast(inp["final_g"])
    wg = np.ascontiguousarray(inp["moe_w_gate"][i][:nexp])
    wu = np.ascontiguousarray(inp["moe_w_up"][i][:nexp])
    wd = np.ascontiguousarray(inp["moe_w_down"][i][:nexp])
    maps = []
    for k in range(NCORES):
        b = k // 4
        ml = mods[i, b]
        mcx = mods[i, 2]
        mc = np.stack([cols(inp["norm2_g"][i]), cols(ml[4 * D:5 * D]), cols(ml[3 * D:4 * D]),
                       cols(mcx[4 * D:5 * D]), cols(mcx[3 * D:4 * D]), np.zeros((128, 8), np.float32)], axis=1)
        gtb = np.stack([bcast(ml[5 * D:6 * D]), bcast(mcx[5 * D:6 * D])], axis=1)
        maps.append(dict(xin=np.ascontiguousarray(xs_per_core[k]), wr=wr, br=br, mcols=np.ascontiguousarray(mc),
                         gtb=np.ascontiguousarray(gtb), fgb=fgb, ident=ident, wg=wg, wu=wu, wd=wd))
    return maps


def run_moe(xs_per_core, i, mods, inp, final=False, ntok=NTOK, nexp=NEXP, trace=False, dbg=False):
    key = (final, ntok, nexp, dbg)
    if key not in _MOE_NC:
        _MOE_NC[key] = build_moe(final=final, ntok=ntok, nexp=nexp, dbg=dbg)
    maps = moe_maps(xs_per_core, i, mods, inp, nexp)
    res = run_bass_kernel_spmd(_MOE_NC[key], maps, core_ids=list(range(NCORES)), trace=trace)
    if trace:
        print("moe exec_time_ns", res.exec_time_ns)
    if dbg:
        return res.results
    return [r["xout"] for r in res.results]


GW = 2048


def build_gmlp(nch=33, ctx_last=True):
    nc = new_nc()
    ntok = nch * 128
    xin = dram_in(nc, "xin", [ntok, D])
    mc_d = dram_in(nc, "mcols", [128, 5, 8])
    gt_d = dram_in(nc, "gtb", [128, 2, D])
    bo_d = dram_in(nc, "boutb", [128, D])
    win_d = dram_in(nc, "w_in", [D, 2 * GW])
    bu_d = dram_in(nc, "b_ucol", [128, 16])
    bv_d = dram_in(nc, "b_vrow", [1, GW])
    lg_d = dram_in(nc, "lngb", [128, GW])
    lb_d = dram_in(nc, "lnbb", [128, GW])
    ws_d = dram_in(nc, "wsT", [128, 16, 128])
    bs_d = dram_in(nc, "bsrow", [1, 16, 128])
    wo_d = dram_in(nc, "w_out", [GW, D])
    id_d = dram_in(nc, "ident", [128, 128])
    xout = dram_out(nc, "xout", [ntok, D])
    S = Sched(nc)
    GT = 256
    ident = S.sb("ident", [128, 128], F32); b_id = Buf()
    mc = S.sb("mc", [128, 5, 8], F32); b_mc = Buf()
    gsc = S.sb("gsc", [128, 2, 8], F32); b_gsc = Buf()
    gtb = S.sb("gtb", [128, 2, D], F32); b_gt = Buf()
    bob = S.sb("bob", [128, D], F32); b_bo = Buf()
    win = S.sb("win", [128, 8, 2 * GW], BF16); b_win = Buf()
    bu = S.sb("bu", [128, 16], F32); b_bu = Buf()
    bvr = S.sb("bvr", [1, GW], BF16); b_bv = Buf()
    lgb = S.sb("lgb", [128, GW], F32); b_lg = Buf()
    lbb = S.sb("lbb", [128, GW], F32); b_lb = Buf()
    wsT = S.sb("wsT", [128, 16, 128], BF16); b_ws = Buf()
    bsr = S.sb("bsr", [1, 16, 128], BF16); b_bs = Buf()
    wo = S.sb("wo", [128, 16, D], BF16); b_wo = Buf()
    ones = S.sb("ones", [1, 128], BF16); b_ones = Buf()
    hT = [S.sb("hT%d" % i, [128, 8, GT], BF16) for i in range(2)]; b_hT = [Buf() for _ in range(2)]
    uT = S.sb("uT", [128, 16, GT], BF16); b_uT = [Buf() for _ in range(16)]
    vv = [S.sb("vv%d" % i, [128, GW], F32) for i in range(2)]; b_vv = [Buf() for _ in range(2)]
    vn = [S.sb("vn%d" % i, [128, GW], BF16) for i in range(2)]; b_vn = [Buf() for _ in range(2)]
    prod = S.sb("prod", [128, 16, GT], BF16); b_prod = [[Buf() for _ in range(4)] for _ in range(2)]
    xt = [S.sb("xt%d" % i, [128, D], F32) for i in range(2)]; b_xt = [Buf() for _ in range(2)]
    _xs = S.sb("xs0", [128, D], F32); _bxs = Buf()
    xs = [_xs, _xs]; b_xs = [_bxs, _bxs]
    ot = [S.sb("ot%d" % i, [128, D], F32) for i in range(2)]; b_ot = [Buf() for _ in range(2)]
    sm = [S.sb("sm%d" % i, [128, 8], F32) for i in range(2)]; b_sm = [Buf() for _ in range(2)]
    PS = [S.ps("psb%d" % i, [128, 512], F32) for i in range(8)]; b_ps = [Buf() for _ in range(8)]

    S.dma("sync", ident[:], id_d, w=[b_id])
    S.dma("sync", mc[:], mc_d, w=[b_mc])
    S.dma("sync", gtb[:], gt_d, w=[b_gt])
    S.dma("sync", bob[:], bo_d, w=[b_bo])
    S.dma("sync", bu[:], bu_d, w=[b_bu])
    S.dma("sync", lgb[:], lg_d, w=[b_lg])
    S.dma("sync", lbb[:], lb_d, w=[b_lb])
    S.dma("gpsimd", bvr[:], bv_d, w=[b_bv])
    S.dma("gpsimd", bsr[:], bs_d, w=[b_bs])
    S.dma("gpsimd", wsT[:], ws_d, w=[b_ws])
    winv = win_d.rearrange("(c p) n -> p c n", p=128)
    for n in range(4):
        S.dma("gpsimd", win[:, :, n * 1024:(n + 1) * 1024], winv[:, :, n * 1024:(n + 1) * 1024], w=[b_win])
    S.dma("gpsimd", wo[:], wo_d.rearrange("(c p) n -> p c n", p=128), w=[b_wo])
    S.op("vector", lambda e: e.memset(ones[:], 1.0), w=[b_ones])
    for v in range(2):
        S.op("vector", lambda e, v=v: e.scalar_tensor_tensor(gsc[:, v, :], mc[:, 1 + 2 * v, :], 1.0, mc[:, 0, :],
                                                              ALU.add, ALU.mult), r=[b_mc], w=[b_gsc])
    out_ops = []
    groups = []
    ci = 0
    while ci < nch:
        g2 = min(ci + 2, nch)
        groups.append(list(range(ci, g2)))
        ci = g2
    tcount = 0
    vcount = 0
    for gi, grp in enumerate(groups):
        ng = len(grp)
        gt_ = ng * 128
        hp = gi % 2
        xpar = {}
        for li, ch in enumerate(grp):
            par = tcount % 2
            tcount += 1
            xpar[ch] = par
            v = 1 if (ctx_last and ch == nch - 1) else 0
            off = ch * 128
            S.dma("sync", xt[par][:], xin[off:off + 128, :], w=[b_xt[par]])
            ss = sm[par][:, 0:1]; rstd = sm[par][:, 1:2]
            S.op("scalar", lambda e, par=par, ss=ss: e.activation(xs[par][:], xt[par][:], AF.Square, accum_out=ss),
                 r=[b_xt[par]], w=[b_xs[par], b_sm[par]])
            S.op("vector", lambda e, ss=ss, rstd=rstd: e.tensor_scalar(rstd, ss, 1.0 / D, 1e-6, ALU.mult, ALU.add),
                 r=[b_sm[par]], w=[b_sm[par]])
            S.op("scalar", lambda e, rstd=rstd: e.activation(rstd, rstd, AF.Sqrt), r=[b_sm[par]], w=[b_sm[par]])
            S.op("vector", lambda e, rstd=rstd: e.reciprocal(rstd, rstd), r=[b_sm[par]], w=[b_sm[par]])
            S.op("vector", lambda e, par=par, rstd=rstd: e.tensor_scalar(xs[par][:], xt[par][:], rstd, None, ALU.mult),
                 r=[b_xt[par], b_sm[par]], w=[b_xs[par]])
            for c in range(8):
                pb = c // 4
                S.op("tensor", lambda e, par=par, c=c, pb=pb: e.transpose(PS[pb][:, (c % 4) * 128:(c % 4) * 128 + 128],
                                                                          xs[par][:, c * 128:(c + 1) * 128], ident[:]),
                     r=[b_xs[par], b_id], w=[b_ps[pb]])
            for c in range(8):
                pb = c // 4
                S.op("scalar", lambda e, hp=hp, c=c, pb=pb, v=v, li=li: e.activation(
                    hT[hp][:, c, li * 128:(li + 1) * 128], PS[pb][:, (c % 4) * 128:(c % 4) * 128 + 128], AF.Identity,
                    bias=mc[:, 2 + 2 * v, c:c + 1], scale=gsc[:, v, c:c + 1]),
                    r=[b_ps[pb], b_mc, b_gsc], w=[b_hT[hp]])
        for m in range(16):
            pb = 2 + m % 2
            for c in range(8):
                S.op("tensor", lambda e, pb=pb, c=c, m=m, hp=hp, gt_=gt_: e.matmul(
                    PS[pb][:, 0:gt_], win[:, c, m * 128:(m + 1) * 128], hT[hp][:, c, 0:gt_], start=(c == 0), stop=(c == 7)),
                    r=[b_win, b_hT[hp]], w=[b_ps[pb]])
            S.op("scalar", lambda e, pb=pb, m=m, gt_=gt_: e.activation(uT[:, m, 0:gt_], PS[pb][:, 0:gt_], AF.Gelu,
                                                                       bias=bu[:, m:m + 1], scale=1.0),
                 r=[b_ps[pb], b_bu], w=[b_uT[m]])
        for li, ch in enumerate(grp):
            vp = vcount % 2
            vcount += 1
            v = 1 if (ctx_last and ch == nch - 1) else 0
            par = xpar[ch]
            off = ch * 128
            tk = slice(li * 128, (li + 1) * 128)
            for n in range(4):
                pb = 4 + n % 2
                for c in range(8):
                    S.op("tensor", lambda e, pb=pb, c=c, n=n, hp=hp, tk=tk: e.matmul(
                        PS[pb][:, :], hT[hp][:, c, tk], win[:, c, GW + n * 512:GW + (n + 1) * 512], start=(c == 0), stop=False),
                        r=[b_win, b_hT[hp]], w=[b_ps[pb]])
                S.op("tensor", lambda e, pb=pb, n=n: e.matmul(PS[pb][:, :], ones[0:1, :], bvr[0:1, n * 512:(n + 1) * 512],
                                                              start=False, stop=True),
                     r=[b_ones, b_bv], w=[b_ps[pb]])
                S.op("scalar", lambda e, pb=pb, n=n, vp=vp: e.activation(vv[vp][:, n * 512:(n + 1) * 512], PS[pb][:, :], AF.Gelu),
                     r=[b_ps[pb]], w=[b_vv[vp]])
            s1 = sm[par][:, 2:3]; nm = sm[par][:, 3:4]; sq = sm[par][:, 4:5]; r2 = sm[par][:, 5:6]
            S.op("vector", lambda e, s1=s1, vp=vp: e.reduce_sum(s1, vv[vp][:], AX.X), r=[b_vv[vp]], w=[b_sm[par]])
            S.op("vector", lambda e, s1=s1, nm=nm: e.tensor_scalar(nm, s1, -1.0 / GW, None, ALU.mult), r=[b_sm[par]], w=[b_sm[par]])
            S.op("scalar", lambda e, vp=vp, nm=nm, sq=sq: e.activation(vn[vp][:], vv[vp][:], AF.Square, bias=nm, scale=1.0, accum_out=sq),
                 r=[b_vv[vp], b_sm[par]], w=[b_vn[vp], b_sm[par]])
            S.op("vector", lambda e, sq=sq, r2=r2: e.tensor_scalar(r2, sq, 1.0 / GW, 1e-5, ALU.mult, ALU.add), r=[b_sm[par]], w=[b_sm[par]])
            S.op("scalar", lambda e, r2=r2: e.activation(r2, r2, AF.Sqrt), r=[b_sm[par]], w=[b_sm[par]])
            S.op("vector", lambda e, r2=r2: e.reciprocal(r2, r2), r=[b_sm[par]], w=[b_sm[par]])
            S.op("vector", lambda e, vp=vp, nm=nm, r2=r2: e.tensor_scalar(vv[vp][:], vv[vp][:], nm, r2, ALU.add, ALU.mult),
                 r=[b_sm[par]], w=[b_vv[vp]])
            S.op("vector", lambda e, vp=vp: e.tensor_tensor(vv[vp][:], vv[vp][:], lgb[:], ALU.mult), r=[b_lg], w=[b_vv[vp]])
            S.op("gpsimd", lambda e, vp=vp: e.tensor_tensor(vn[vp][:], vv[vp][:], lbb[:], ALU.add), r=[b_vv[vp], b_lb], w=[b_vn[vp]])
            for g4 in range(4):
                pb = 6 + g4 % 2
                for gg in range(4):
                    g = g4 * 4 + gg
                    S.op("tensor", lambda e, pb=pb, gg=gg, g=g, vp=vp: e.matmul(
                        PS[pb][:, gg * 128:(gg + 1) * 128], vn[vp][:, g * 128:(g + 1) * 128], wsT[:, g, :], start=True, stop=False),
                        r=[b_vn[vp], b_ws], w=[b_ps[pb]])
                    S.op("tensor", lambda e, pb=pb, gg=gg, g=g: e.matmul(
                        PS[pb][:, gg * 128:(gg + 1) * 128], ones[0:1, :], bsr[0:1, g, :], start=False, stop=True),
                        r=[b_ones, b_bs], w=[b_ps[pb]])
                S.op("vector", lambda e, pb=pb, g4=g4, tk=tk: e.tensor_tensor(
                    prod[:, g4 * 4:(g4 + 1) * 4, tk], PS[pb][:, :].rearrange("p (g t) -> p g t", g=4), uT[:, g4 * 4:(g4 + 1) * 4, tk], ALU.mult),
                    r=[b_ps[pb]] + b_uT[g4 * 4:(g4 + 1) * 4], w=[b_prod[li][g4]])
            op_ = tcount % 2
            for n in range(2):
                pb = n
                for k in range(16):
                    S.op("tensor", lambda e, pb=pb, k=k, n=n, tk=tk: e.matmul(
                        PS[pb][:, :], prod[:, k, tk], wo[:, k, n * 512:(n + 1) * 512], start=(k == 0), stop=(k == 15)),
                        r=b_prod[li] + [b_wo], w=[b_ps[pb]])
                sl = slice(n * 512, (n + 1) * 512)
                S.op("vector", lambda e, pb=pb, par=par, sl=sl: e.tensor_tensor(ot[par][:, sl], PS[pb][:, :], bob[:, sl], ALU.add),
                     r=[b_ps[pb], b_bo], w=[b_ot[par]])
            S.op("gpsimd", lambda e, par=par, v=v: e.tensor_tensor(ot[par][:], ot[par][:], gtb[:, v, :], ALU.mult),
                 r=[b_gt], w=[b_ot[par]])
            S.op("gpsimd", lambda e, par=par: e.tensor_tensor(ot[par][:], ot[par][:], xt[par][:], ALU.add),
                 r=[b_xt[par]], w=[b_ot[par]])
            out_ops.append(S.dma("sync", xout[off:off + 128, :], ot[par][:], r=[b_ot[par]]))
    S.emit(out_ops)
    return nc


_GMLP_NC = {}


def run_gmlp(xs_per_core, i, mods, inp, nch=33, trace=False):
    j = i // 2
    key = nch
    if key not in _GMLP_NC:
        _GMLP_NC[key] = build_gmlp(nch=nch)
    b_in = inp["ga_b_in"][j]
    wsT = np.ascontiguousarray(inp["ga_w_s"][j].transpose(2, 0, 1))
    base = dict(boutb=bcast(inp["ga_b_out"][j]), w_in=np.ascontiguousarray(inp["ga_w_in"][j]),
                b_ucol=np.ascontiguousarray(b_in[:GW].reshape(16, 128).T), b_vrow=np.ascontiguousarray(b_in[GW:].reshape(1, GW)),
                lngb=bcast(inp["ga_ln_g"][j]), lnbb=bcast(inp["ga_ln_b"][j]), wsT=wsT,
                bsrow=np.ascontiguousarray(inp["ga_b_s"][j].reshape(1, 16, 128)), w_out=np.ascontiguousarray(inp["ga_w_out"][j]),
                ident=np.eye(128, dtype=np.float32))
    maps = []
    for k in range(NCORES):
        b = k // 4
        ml = mods[i, b]; mcx = mods[i, 2]
        mc = np.stack([cols(inp["norm1_g"][i]), cols(ml[1 * D:2 * D]), cols(ml[0:D]), cols(mcx[1 * D:2 * D]), cols(mcx[0:D])], axis=1)
        gtb = np.stack([bcast(ml[2 * D:3 * D]), bcast(mcx[2 * D:3 * D])], axis=1)
        m = dict(base)
        m.update(xin=np.ascontiguousarray(xs_per_core[k]), mcols=np.ascontiguousarray(mc), gtb=np.ascontiguousarray(gtb))
        maps.append(m)
    res = run_bass_kernel_spmd(_GMLP_NC[key], maps, core_ids=list(range(NCORES)), trace=trace)
    if trace:
        print("gmlp exec_time_ns", res.exec_time_ns)
    return [r["xout"] for r in res.results]


PBLK = 256
NEG_E05 = -0.6065306597126334


def build_prep(nlat=NLAT, with_ctx=True, stop=None):
    nc = new_nc()
    nrow = nlat + 128 + (256 if with_ctx else 0)
    TP = nlat + (256 if with_ctx else 0)
    xin = dram_in(nc, "xin", [nrow, D])
    edge_d = dram_in(nc, "edge", [128, 2])
    mc_d = dram_in(nc, "mcols", [128, 5, 8])
    mu_d = dram_in(nc, "mu", [128, 6, 8])
    vc_d = dram_in(nc, "vcols", [128, 8, 8])
    wr_d = dram_in(nc, "wr", [D, D]); wk_d = dram_in(nc, "wk", [D, D]); wv_d = dram_in(nc, "wv", [D, D])
    w1_d = dram_in(nc, "w1", [2, D, 64]); w2_d = dram_in(nc, "w2", [2, 64, D])
    a1_d = dram_in(nc, "a1", [2, D, 64]); a2_d = dram_in(nc, "a2", [2, 64, D])
    g1_d = dram_in(nc, "g1", [D, 160]); g2_d = dram_in(nc, "g2", [160, D])
    id_d = dram_in(nc, "ident", [128, 128]); blk_d = dram_in(nc, "blk", [128, 128]); cm_d = dram_in(nc, "cmask", [128, PBLK])
    ob = dram_out(nc, "ob", [9, 8, 128, TP], BF16)
    of = dram_out(nc, "of", [2, 8, 128, TP])
    pc = dram_out(nc, "pc", [2, 8, 128, TP // 64])
    S = Sched(nc)
    ident = S.sb("ident", [128, 128], F32); b_id = Buf()
    blk = S.sb("blk", [128, 128], BF16); b_blk = Buf()
    cmask = S.sb("cmask", [128, PBLK], F32); b_cm = Buf()
    edge = S.sb("edge", [128, 2], F32); b_edge = Buf()
    mc = S.sb("mc", [128, 5, 8], F32); b_mc = Buf()
    gsc = S.sb("gsc", [128, 2, 8], F32); b_gsc = Buf()
    mu = S.sb("mu", [128, 6, 8], F32); b_mu = Buf()
    vc = S.sb("vc", [128, 8, 8], F32); b_vc = Buf()
    epsc = S.sb("epsc", [128, 1], F32); b_eps = Buf()
    wr = S.sb("wr", [128, 8, D], BF16); wk = S.sb("wk", [128, 8, D], BF16); wv = S.sb("wv", [128, 8, D], BF16)
    b_wr = Buf(); b_wk = Buf(); b_wv = Buf()
    w1 = S.sb("w1", [128, 2, 8, 64], BF16); a1 = S.sb("a1", [128, 2, 8, 64], BF16); b_w1 = Buf(); b_a1 = Buf()
    w2 = S.sb("w2", [64, 2, D], BF16); a2 = S.sb("a2", [64, 2, D], BF16); b_w2 = Buf(); b_a2 = Buf()
    g1 = S.sb("g1", [128, 8, 160], BF16); b_g1 = Buf()
    g2a = S.sb("g2a", [128, D], BF16); g2b = S.sb("g2b", [32, D], BF16); b_g2 = Buf()
    HL = S.sb("HL", [128, 8, PBLK + 128], F32); b_HL = [Buf() for _ in range(8)]
    XX = S.sb("XX", [128, 8, PBLK], F32); b_XX = [Buf() for _ in range(8)]
    XM = [S.sb("XM%d" % j, [128, 8, PBLK], BF16) for j in range(6)]; b_XM = [[Buf() for _ in range(8)] for _ in range(6)]
    R32 = S.sb("R32", [128, 8, PBLK], F32); K32 = S.sb("K32", [128, 8, PBLK], F32); V32 = S.sb("V32", [128, 8, PBLK], F32)
    b_R = [Buf() for _ in range(8)]; b_K = [Buf() for _ in range(8)]; b_V = [Buf() for _ in range(8)]
    TW = [S.sb("TW%d" % z, [64, PBLK], BF16) for z in range(2)]; b_TW = [Buf() for _ in range(2)]
    AL = [S.sb("AL%d" % z, [64, PBLK], BF16) for z in range(2)]; b_AL = [Buf() for _ in range(2)]
    SGa = S.sb("SGa", [128, PBLK], BF16); SGb = S.sb("SGb", [32, PBLK], BF16); b_SG = Buf()
    xt = [S.sb("xt%d" % i, [128, D], F32) for i in range(2)]; b_xt = [Buf() for _ in range(2)]
    xs = S.sb("xs", [128, D], F32); b_xs = Buf()
    sm = [S.sb("sm%d" % i, [128, 4], F32) for i in range(2)]; b_sm = [Buf() for _ in range(2)]

    def tmp(name, dt=F32):
        return S.sb("t_" + name, [128, PBLK], dt), Buf()
    LW = [tmp("LW%d" % z) for z in range(2)]; AZ = [tmp("AZ%d" % z) for z in range(2)]
    FF = [tmp("FF%d" % z) for z in range(2)]; CE = [tmp("CE%d" % z) for z in range(2)]
    CC = [tmp("CC%d" % z) for z in range(2)]
    Ec = [tmp("Ec%d" % z) for z in range(2)]; Ee = [tmp("Ee%d" % z) for z in range(2)]; En = [tmp("En%d" % z) for z in range(2)]
    KR = tmp("KR"); SQ = tmp("SQ", BF16); RN = tmp("RN"); KK = tmp("KK"); T1 = tmp("T1")
    KM = [tmp("KM%d" % z) for z in range(2)]; BT = tmp("BT"); KS = tmp("KS"); PB = tmp("PB", BF16)
    GO = [tmp("GO%d" % i) for i in range(2)]; BV = [tmp("BV%d" % i) for i in range(2)]
    OB = [[tmp("OB%d_%d" % (k, i), BF16) for k in range(9)] for i in range(2)]
    PCt = [S.sb("PCt%d" % i, [128, 2, PBLK // 64], F32) for i in range(2)]; b_PC = [Buf() for _ in range(2)]
    PS = [S.ps("psb%d" % i, [128, 512], F32) for i in range(8)]
    _bb = [Buf() for _ in range(8)]
    b_ps = [[b, b] for b in _bb]

    for t_, d_, b_ in [(ident, id_d, b_id), (cmask, cm_d, b_cm), (edge, edge_d, b_edge), (mc, mc_d, b_mc), (mu, mu_d, b_mu), (vc, vc_d, b_vc)]:
        S.dma("sync", t_[:], d_, w=[b_])
    S.dma("gpsimd", blk[:], blk_d, w=[b_blk])
    for t_, d_, b_ in [(wr, wr_d, b_wr), (wk, wk_d, b_wk), (wv, wv_d, b_wv)]:
        S.dma("gpsimd", t_[:], d_.rearrange("(c p) n -> p c n", p=128), w=[b_])
    for z in range(2):
        S.dma("gpsimd", w1[:, z, :, :], w1_d[z].rearrange("(c p) n -> p c n", p=128), w=[b_w1])
        S.dma("gpsimd", a1[:, z, :, :], a1_d[z].rearrange("(c p) n -> p c n", p=128), w=[b_a1])
        S.dma("gpsimd", w2[:, z, :], w2_d[z], w=[b_w2])
        S.dma("gpsimd", a2[:, z, :], a2_d[z], w=[b_a2])
    S.dma("gpsimd", g1[:], g1_d.rearrange("(c p) n -> p c n", p=128), w=[b_g1])
    S.dma("gpsimd", g2a[:], g2_d[0:128, :], w=[b_g2])
    S.dma("gpsimd", g2b[:], g2_d[128:160, :], w=[b_g2])
    S.op("vector", lambda e: e.memset(epsc[:], 1e-12), w=[b_eps])
    for v in range(2):
        S.op("vector", lambda e, v=v: e.scalar_tensor_tensor(gsc[:, v, :], mc[:, 1 + 2 * v, :], 1.0, mc[:, 0, :],
                                                              ALU.add, ALU.mult), r=[b_mc], w=[b_gsc])
    out_ops = []
    nblk_lat = nlat // PBLK
    blocks = [("lat", bi) for bi in range(nblk_lat)] + ([("ctx", 0)] if with_ctx else [])
    tcount = 0
    ocount = 0
    C0 = 64
    for kind, bi in blocks:
        isctx = kind == "ctx"
        v = 1 if isctx else 0
        if isctx:
            row0 = nlat + 128
            tl = [(row0, C0), (row0 + 128, C0 + 128)]
            tok0 = nlat
        else:
            row0 = bi * PBLK
            tl = [(row0, 0), (row0 + 128, 128), (row0 + 256, 256)]
            tok0 = bi * PBLK
        for (rw, col) in tl:
            par = tcount % 2
            tcount += 1
            S.dma("sync", xt[par][:], xin[rw:rw + 128, :], w=[b_xt[par]])
            ss = sm[par][:, 0:1]; rstd = sm[par][:, 1:2]
            S.op("scalar", lambda e, par=par, ss=ss: e.activation(xs[:], xt[par][:], AF.Square, accum_out=ss),
                 r=[b_xt[par]], w=[b_xs, b_sm[par]])
            S.op("vector", lambda e, ss=ss, rstd=rstd: e.tensor_scalar(rstd, ss, 1.0 / D, 1e-6, ALU.mult, ALU.add),
                 r=[b_sm[par]], w=[b_sm[par]])
            S.op("scalar", lambda e, rstd=rstd: e.activation(rstd, rstd, AF.Sqrt), r=[b_sm[par]], w=[b_sm[par]])
            S.op("vector", lambda e, rstd=rstd: e.reciprocal(rstd, rstd), r=[b_sm[par]], w=[b_sm[par]])
            S.op("vector", lambda e, par=par, rstd=rstd: e.tensor_scalar(xs[:], xt[par][:], rstd, None, ALU.mult),
                 r=[b_xt[par], b_sm[par]], w=[b_xs])
            for c in range(8):
                pb = c // 4
                S.op("tensor", lambda e, c=c, pb=pb: e.transpose(PS[pb][:, (c % 4) * 128:(c % 4) * 128 + 128],
                                                                 xs[:, c * 128:(c + 1) * 128], ident[:]),
                     r=[b_xs, b_id], w=b_ps[pb])
            for c in range(8):
                pb = c // 4
                S.op("scalar", lambda e, c=c, pb=pb, v=v, col=col: e.activation(
                    HL[:, c, col:col + 128], PS[pb][:, (c % 4) * 128:(c % 4) * 128 + 128], AF.Identity,
                    bias=mc[:, 2 + 2 * v, c:c + 1], scale=gsc[:, v, c:c + 1]),
                    r=b_ps[pb] + [b_mc, b_gsc], w=[b_HL[c]])
        if stop == 'A':
            continue
        for c in range(8):
            ctr = HL[:, c, C0:C0 + PBLK]
            if not isctx:
                c4 = ctr.rearrange("p (a b) -> p a b", b=64)
                x4 = XX[:, c, :].rearrange("p (a b) -> p a b", b=64)
                if c < 2:
                    S.op("vector", lambda e, x4=x4, c4=c4: e.tensor_tensor(x4[:, :, 1:64], c4[:, :, 0:63], c4[:, :, 1:64], ALU.subtract),
                         r=[b_HL[c]], w=[b_XX[c]])
                    S.op("vector", lambda e, x4=x4, c4=c4: e.tensor_scalar(x4[:, :, 0:1], c4[:, :, 0:1], -1.0, None, ALU.mult),
                         r=[b_HL[c]], w=[b_XX[c]])
                elif c < 4:
                    S.op("vector", lambda e, x4=x4, c4=c4: e.tensor_tensor(x4[:, :, 0:63], c4[:, :, 1:64], c4[:, :, 0:63], ALU.subtract),
                         r=[b_HL[c]], w=[b_XX[c]])
                    S.op("vector", lambda e, x4=x4, c4=c4: e.tensor_scalar(x4[:, :, 63:64], c4[:, :, 63:64], -1.0, None, ALU.mult),
                         r=[b_HL[c]], w=[b_XX[c]])
                elif c < 6:
                    S.op("vector", lambda e, c=c, ctr=ctr: e.tensor_tensor(XX[:, c, :], HL[:, c, 0:PBLK], ctr, ALU.subtract),
                         r=[b_HL[c]], w=[b_XX[c]])
                    if bi == 0:
                        S.op("vector", lambda e, c=c: e.scalar_tensor_tensor(XX[:, c, 0:64], HL[:, c, 0:64], edge[:, 0:1], HL[:, c, 64:128],
                                                                             ALU.mult, ALU.subtract),
                             r=[b_HL[c], b_edge], w=[b_XX[c]])
                else:
                    S.op("vector", lambda e, c=c, ctr=ctr: e.tensor_tensor(XX[:, c, :], HL[:, c, 128:128 + PBLK], ctr, ALU.subtract),
                         r=[b_HL[c]], w=[b_XX[c]])
                    if bi == nblk_lat - 1:
                        S.op("vector", lambda e, c=c: e.scalar_tensor_tensor(XX[:, c, PBLK - 64:PBLK], HL[:, c, PBLK + 64:PBLK + 128], edge[:, 1:2],
                                                                             HL[:, c, PBLK:PBLK + 64], ALU.mult, ALU.subtract),
                             r=[b_HL[c], b_edge], w=[b_XX[c]])
            else:
                if c < 4:
                    S.op("vector", lambda e, c=c: e.tensor_tensor(XX[:, c, 1:PBLK], HL[:, c, C0:C0 + PBLK - 1], HL[:, c, C0 + 1:C0 + PBLK], ALU.subtract),
                         r=[b_HL[c]], w=[b_XX[c]])
                    S.op("vector", lambda e, c=c: e.tensor_scalar(XX[:, c, 0:1], HL[:, c, C0:C0 + 1], -1.0, None, ALU.mult),
                         r=[b_HL[c]], w=[b_XX[c]])
                else:
                    S.op("vector", lambda e, c=c: e.tensor_tensor(XX[:, c, 0:PBLK - 1], HL[:, c, C0 + 1:C0 + PBLK], HL[:, c, C0:C0 + PBLK - 1], ALU.subtract),
                         r=[b_HL[c]], w=[b_XX[c]])
                    S.op("vector", lambda e, c=c: e.tensor_scalar(XX[:, c, PBLK - 1:PBLK], HL[:, c, C0 + PBLK - 1:C0 + PBLK], -1.0, None, ALU.mult),
                         r=[b_HL[c]], w=[b_XX[c]])
        if stop == 'B':
            continue
        for j in range(6):
            eng = "vector"
            for c in range(8):
                S.op(eng, lambda e, j=j, c=c: e.scalar_tensor_tensor(XM[j][:, c, :], XX[:, c, :], mu[:, j, c:c + 1], HL[:, c, C0:C0 + PBLK],
                                                                     ALU.mult, ALU.add),
                     r=[b_XX[c], b_HL[c], b_mu], w=[b_XM[j][c]])
        if stop == 'C':
            continue
        pk = [0]

        def proj(wsb, bw, j, dst, bdst, extra=None):
            for co in range(8):
                pb = 2 + pk[0] % 2; hb = (pk[0] // 2) % 2
                pk[0] += 1
                reg = PS[pb][:, hb * 256:hb * 256 + PBLK]
                for c in range(8):
                    S.op("tensor", lambda e, reg=reg, c=c, co=co, wsb=wsb, j=j: e.matmul(
                        reg, wsb[:, c, co * 128:(co + 1) * 128], XM[j][:, c, :], start=(c == 0), stop=(c == 7)),
                        r=[bw] + b_XM[j], w=[b_ps[pb][hb]])
                S.op("scalar", lambda e, reg=reg, co=co, dst=dst: e.copy(dst[:, co, :], reg), r=[b_ps[pb][hb]], w=[bdst[co]])
        proj(wr, b_wr, 0, R32, b_R)
        proj(wk, b_wk, 2, K32, b_K)
        proj(wv, b_wv, 3, V32, b_V)
        for z in range(2):
            reg = PS[4][0:64, z * 256:z * 256 + PBLK]
            for c in range(8):
                S.op("tensor", lambda e, reg=reg, c=c, z=z: e.matmul(reg, w1[:, z, c, :], XM[1][:, c, :], start=(c == 0), stop=(c == 7)),
                     r=[b_w1] + b_XM[1], w=[b_ps[4][z]])
            S.op("scalar", lambda e, reg=reg, z=z: e.activation(TW[z][:], reg, AF.Tanh), r=[b_ps[4][z]], w=[b_TW[z]])
        for z in range(2):
            reg = PS[5][0:64, z * 256:z * 256 + PBLK]
            for c in range(8):
                S.op("tensor", lambda e, reg=reg, c=c, z=z: e.matmul(reg, a1[:, z, c, :], XM[4][:, c, :], start=(c == 0), stop=(c == 7)),
                     r=[b_a1] + b_XM[4], w=[b_ps[5][z]])
            S.op("scalar", lambda e, reg=reg, z=z: e.copy(AL[z][:], reg), r=[b_ps[5][z]], w=[b_AL[z]])
        rega = PS[6][:, 0:PBLK]; regb = PS[6][0:32, 256:256 + PBLK]
        for c in range(8):
            S.op("tensor", lambda e, c=c: e.matmul(rega, g1[:, c, 0:128], XM[5][:, c, :], start=(c == 0), stop=(c == 7)),
                 r=[b_g1] + b_XM[5], w=[b_ps[6][0]])
        for c in range(8):
            S.op("tensor", lambda e, c=c: e.matmul(regb, g1[:, c, 128:160], XM[5][:, c, :], start=(c == 0), stop=(c == 7)),
                 r=[b_g1] + b_XM[5], w=[b_ps[6][1]])
        S.op("scalar", lambda e: e.activation(SGa[:], rega, AF.Sigmoid), r=[b_ps[6][0]], w=[b_SG])
        S.op("scalar", lambda e: e.activation(SGb[:], regb, AF.Sigmoid), r=[b_ps[6][1]], w=[b_SG])
        if stop == 'D':
            continue
        for co in range(8):
            op_ = ocount % 2
            ocount += 1
            cs = slice(co * 128, (co + 1) * 128)
            for z in range(2):
                reg = PS[4][:, z * 256:z * 256 + PBLK]
                S.op("tensor", lambda e, reg=reg, z=z, cs=cs: e.matmul(reg, w2[0:64, z, cs], TW[z][0:64, :], start=True, stop=True),
                     r=[b_w2, b_TW[z]], w=[b_ps[4][z]])
                S.op("scalar", lambda e, reg=reg, z=z, co=co: e.activation(LW[z][0][:], reg, AF.Sigmoid, bias=vc[:, z, co:co + 1], scale=1.0),
                     r=[b_ps[4][z], b_vc], w=[LW[z][1]])
                S.op("vector", lambda e, z=z: e.tensor_scalar(LW[z][0][:], LW[z][0][:], NEG_E05, None, ALU.mult), r=[], w=[LW[z][1]])
                reg2 = PS[5][:, z * 256:z * 256 + PBLK]
                S.op("tensor", lambda e, reg2=reg2, z=z, cs=cs: e.matmul(reg2, a2[0:64, z, cs], AL[z][0:64, :], start=True, stop=True),
                     r=[b_a2, b_AL[z]], w=[b_ps[5][z]])
                S.op("scalar", lambda e, reg2=reg2, z=z, co=co: e.activation(AZ[z][0][:], reg2, AF.Sigmoid, bias=vc[:, 2 + z, co:co + 1], scale=1.0),
                     r=[b_ps[5][z], b_vc], w=[AZ[z][1]])
            regg = PS[6][:, 0:PBLK]
            S.op("tensor", lambda e, cs=cs: e.matmul(regg, g2a[:, cs], SGa[:], start=True, stop=False), r=[b_g2, b_SG], w=[b_ps[6][0]])
            S.op("tensor", lambda e, cs=cs: e.matmul(regg, g2b[0:32, cs], SGb[0:32, :], start=False, stop=True), r=[b_g2, b_SG], w=[b_ps[6][0]])
            S.op("scalar", lambda e, op_=op_: e.copy(GO[op_][0][:], regg), r=[b_ps[6][0]], w=[GO[op_][1]])
            out_ops.append(S.dma("sync", of[1, co, :, tok0:tok0 + PBLK], GO[op_][0][:], r=[GO[op_][1]]))
            if stop == 'E1':
                continue
            for z in range(2):
                S.op("vector", lambda e, z=z: e.tensor_tensor_scan(FF[z][0][:], cmask[:], LW[z][0][:], 0.0, ALU.mult, ALU.add),
                     r=[b_cm, LW[z][1]], w=[FF[z][1]])
            S.op("vector", lambda e: e.tensor_tensor(CE[0][0][:], FF[0][0][:], LW[0][0][:], ALU.subtract), r=[FF[0][1], LW[0][1]], w=[CE[0][1]])
            f3 = FF[1][0][:].rearrange("p (a b) -> p a b", b=64)
            S.op("vector", lambda e, f3=f3: e.tensor_tensor(CE[1][0][:].rearrange("p (a b) -> p a b", b=64),
                                                            f3[:, :, 63:64].to_broadcast([128, PBLK // 64, 64]), f3, ALU.subtract),
                 r=[FF[1][1]], w=[CE[1][1]])
            S.op("vector", lambda e: e.tensor_tensor(CC[1][0][:], CE[1][0][:], LW[1][0][:], ALU.add), r=[CE[1][1], LW[1][1]], w=[CC[1][1]])
            csrc = [FF[0], CC[1]]
            for z in range(2):
                S.op("scalar", lambda e, z=z: e.activation(Ec[z][0][:], csrc[z][0][:], AF.Exp), r=[csrc[z][1]], w=[Ec[z][1]])
                S.op("scalar", lambda e, z=z: e.activation(En[z][0][:], csrc[z][0][:], AF.Exp, scale=-1.0), r=[csrc[z][1]], w=[En[z][1]])
                S.op("scalar", lambda e, z=z: e.activation(Ee[z][0][:], CE[z][0][:], AF.Exp), r=[CE[z][1]], w=[Ee[z][1]])
            S.op("vector", lambda e, op_=op_: e.tensor_copy(PCt[op_][:, 0, :], Ec[0][0][:].rearrange("p (a b) -> p a b", b=64)[:, :, 63]),
                 r=[Ec[0][1]], w=[b_PC[op_]])
            S.op("vector", lambda e, op_=op_: e.tensor_copy(PCt[op_][:, 1, :], Ec[1][0][:].rearrange("p (a b) -> p a b", b=64)[:, :, 0]),
                 r=[Ec[1][1]], w=[b_PC[op_]])
            ch0 = tok0 // 64
            for z in range(2):
                out_ops.append(S.dma("sync", pc[z, co, :, ch0:ch0 + PBLK // 64], PCt[op_][:, z, :], r=[b_PC[op_]]))
            if stop == 'E2':
                continue
            S.op("vector", lambda e, co=co: e.tensor_scalar(KR[0][:], K32[:, co, :], vc[:, 4, co:co + 1], None, ALU.mult),
                 r=[b_K[co], b_vc], w=[KR[1]])
            S.op("scalar", lambda e: e.activation(SQ[0][:], KR[0][:], AF.Square), r=[KR[1]], w=[SQ[1]])
            regk = PS[6][:, 256:256 + PBLK]
            S.op("tensor", lambda e: e.matmul(regk, blk[:], SQ[0][:], start=True, stop=True), r=[b_blk, SQ[1]], w=[b_ps[6][1]])
            S.op("scalar", lambda e: e.activation(RN[0][:], regk, AF.Sqrt, bias=epsc[:, 0:1], scale=1.0), r=[b_ps[6][1], b_eps], w=[RN[1]])
            S.op("vector", lambda e: e.reciprocal(RN[0][:], RN[0][:]), r=[RN[1]], w=[RN[1]])
            S.op("vector", lambda e: e.tensor_tensor(KK[0][:], KR[0][:], RN[0][:], ALU.mult), r=[KR[1], RN[1]], w=[KK[1]])
            if stop == 'E3':
                continue
            for z in range(2):
                S.op("vector", lambda e, z=z, co=co: e.tensor_scalar(T1[0][:], AZ[z][0][:], -1.0, vc[:, 5, co:co + 1], ALU.add, ALU.mult),
                     r=[AZ[z][1], b_vc], w=[T1[1]])
                S.op("vector", lambda e, z=z, co=co: e.scalar_tensor_tensor(KM[z][0][:], T1[0][:], 1.0, K32[:, co, :], ALU.add, ALU.mult),
                     r=[T1[1], b_K[co]], w=[KM[z][1]])
            O = OB[op_]
            for z in range(2):
                S.op("vector", lambda e, z=z, O=O: e.scalar_tensor_tensor(O[z][0][:], KK[0][:], -1.0, Ee[z][0][:], ALU.mult, ALU.mult),
                     r=[KK[1], Ee[z][1]], w=[O[z][1]])
                S.op("gpsimd", lambda e, z=z, O=O, co=co: e.tensor_tensor(O[2 + z][0][:], R32[:, co, :], Ec[z][0][:], ALU.mult),
                     r=[b_R[co], Ec[z][1]], w=[O[2 + z][1]])
                S.op("gpsimd", lambda e, z=z: e.tensor_tensor(BT[0][:], KK[0][:], AZ[z][0][:], ALU.mult), r=[KK[1], AZ[z][1]], w=[BT[1]])
                S.op("gpsimd", lambda e, z=z, O=O: e.tensor_tensor(O[4 + z][0][:], BT[0][:], En[z][0][:], ALU.mult),
                     r=[BT[1], En[z][1]], w=[O[4 + z][1]])
                S.op("gpsimd", lambda e, z=z, O=O: e.tensor_tensor(O[6 + z][0][:], KM[z][0][:], En[z][0][:], ALU.mult),
                     r=[KM[z][1], En[z][1]], w=[O[6 + z][1]])
            S.op("scalar", lambda e, O=O, co=co: e.copy(O[8][0][:], V32[:, co, :]), r=[b_V[co]], w=[O[8][1]])
            for k in range(9):
                out_ops.append(S.dma("sync", ob[k, co, :, tok0:tok0 + PBLK], O[k][0][:], r=[O[k][1]]))
            if stop == 'E4':
                continue
            S.op("gpsimd", lambda e: e.tensor_tensor(KS[0][:], KM[0][0][:], KM[1][0][:], ALU.add), r=[KM[0][1], KM[1][1]], w=[KS[1]])
            S.op("vector", lambda e, co=co: e.scalar_tensor_tensor(PB[0][:], R32[:, co, :], vc[:, 6, co:co + 1], KS[0][:], ALU.mult, ALU.mult),
                 r=[b_R[co], b_vc, KS[1]], w=[PB[1]])
            regbv = PS[7][:, 0:PBLK]
            S.op("tensor", lambda e: e.matmul(regbv, blk[:], PB[0][:], start=True, stop=True), r=[b_blk, PB[1]], w=[b_ps[7][0]])
            S.op("vector", lambda e, op_=op_, co=co: e.tensor_tensor(BV[op_][0][:], regbv, V32[:, co, :], ALU.mult),
                 r=[b_ps[7][0], b_V[co]], w=[BV[op_][1]])
            out_ops.append(S.dma("sync", of[0, co, :, tok0:tok0 + PBLK], BV[op_][0][:], r=[BV[op_][1]]))
    S.emit(out_ops)
    return nc


_PREP_NC = {}


def prep_consts():
    blk = np.zeros((128, 128), np.float32); blk[:64, :64] = 1; blk[64:, 64:] = 1
    cm = np.ones((128, PBLK), np.float32); cm[:, ::64] = 0
    return dict(ident=np.eye(128, dtype=np.float32), blk=blk, cmask=cm)


def colsk(vs):
    return np.ascontiguousarray(np.stack([cols(v) for v in vs], axis=1))


def run_prep(x, xc, i, mods, inp, nlat=NLAT, trace=False):
    j = i // 2
    key = nlat
    if key not in _PREP_NC:
        _PREP_NC[key] = build_prep(nlat=nlat)
    P = {k: inp["rw_" + k][j] for k in ["mu", "wr", "wk", "wv", "w0", "w1", "w2", "a0", "a1", "a2", "g1", "g2", "k_k", "k_a", "r_k"]}
    base = dict(prep_consts())
    base.update(mu=colsk([P["mu"][t] for t in range(6)]),
                vcols=colsk([P["w0"][0], P["w0"][1], P["a0"][0], P["a0"][1], P["k_k"], P["k_a"], P["r_k"].reshape(-1), np.zeros(D)]),
                wr=np.ascontiguousarray(P["wr"]), wk=np.ascontiguousarray(P["wk"]), wv=np.ascontiguousarray(P["wv"]),
                w1=np.ascontiguousarray(P["w1"]), w2=np.ascontiguousarray(P["w2"]), a1=np.ascontiguousarray(P["a1"]),
                a2=np.ascontiguousarray(P["a2"]), g1=np.ascontiguousarray(P["g1"]), g2=np.ascontiguousarray(P["g2"]))
    nq = x.shape[1] // nlat
    maps = []
    for k in range(NCORES):
        b, q = k // nq, k % nq
        lo = q * nlat
        z64 = np.zeros((64, D), np.float32)
        hb = x[b, lo - 64:lo] if q > 0 else z64
        ha = x[b, lo + nlat:lo + nlat + 64] if q < nq - 1 else z64
        xin = np.concatenate([hb, x[b, lo:lo + nlat], ha, xc[b]], axis=0)
        ml = mods[i, b]; mcx = mods[i, 2]
        mc = np.stack([cols(inp["norm1_g"][i]), cols(ml[1 * D:2 * D]), cols(ml[0:D]), cols(mcx[1 * D:2 * D]), cols(mcx[0:D])], axis=1)
        edge = np.zeros((128, 2), np.float32); edge[:, 0] = 1.0 if q > 0 else 0.0; edge[:, 1] = 1.0 if q < nq - 1 else 0.0
        m = dict(base)
        m.update(xin=np.ascontiguousarray(xin, dtype=np.float32), mcols=np.ascontiguousarray(mc), edge=edge)
        maps.append(m)
    res = run_bass_kernel_spmd(_PREP_NC[key], maps, core_ids=list(range(NCORES)), trace=trace)
    if trace:
        print("prep exec_time_ns", res.exec_time_ns)
    return res.results


SGS = 4


def build_scan(nch=260, mode=None):
    nc = new_nc()
    AR_d = dram_in(nc, "AR", [64, nch, 8, 128], BF16)
    BK_d = dram_in(nc, "BK", [64, nch, 8, 128], BF16)
    BKT_d = dram_in(nc, "BKT", [128, nch, 8, 64], BF16)
    VT_d = dram_in(nc, "VT", [64, nch, 8, 64], BF16)
    PC_d = dram_in(nc, "PCB", [64, nch, 8])
    mg_d = dram_in(nc, "maskG", [128, 8, 128])
    mn_d = dram_in(nc, "maskNT", [64, 8, 64])
    id_d = dram_in(nc, "ident8", [64, 8, 64])
    Y_d = dram_out(nc, "Y", [64, nch, 8, 64])
    S = Sched(nc)
    maskG = S.sb("maskG", [128, 8, 128], F32); maskNT = S.sb("maskNT", [64, 8, 64], F32); id8 = S.sb("id8", [64, 8, 64], F32)
    b_c = Buf()
    ARs = [S.sb("AR%d" % i, [64, SGS, 8, 128], BF16) for i in range(2)]; b_AR = [Buf() for _ in range(2)]
    BKs = [S.sb("BK%d" % i, [64, SGS, 8, 128], BF16) for i in range(2)]; b_BK = [Buf() for _ in range(2)]
    BTs = [S.sb("BT%d" % i, [64, SGS, 8, 64], BF16) for i in range(2)]; b_BKT = [Buf() for _ in range(2)]
    KTs = [S.sb("KT%d" % i, [64, SGS, 8, 64], BF16) for i in range(2)]
    VTs = [S.sb("VT%d" % i, [64, SGS, 8, 64], BF16) for i in range(2)]
    UTs = [S.sb("UT%d" % i, [64, SGS, 8, 64], BF16) for i in range(2)]
    b_UVv = [Buf() for _ in range(2)]
    b_UVu = [[Buf() for _ in range(SGS)] for _ in range(2)]
    PCs = [S.sb("PC%d" % i, [64, SGS, 8], F32) for i in range(2)]; b_PC = [Buf() for _ in range(2)]
    Yst = [S.sb("Yst%d" % i, [64, SGS, 8, 64], F32) for i in range(2)]; b_Y = [Buf() for _ in range(2)]
    Gm = [S.sb("Gm%d" % i, [64, 8, 128], BF16) for i in range(2)]; b_Gm = [Buf() for _ in range(2)]
    Gk = [S.sb("Gk%d" % i, [64, 8, 128], BF16) for i in range(2)]
    NTm = [S.sb("NTm%d" % i, [64, 8, 64], BF16) for i in range(2)]; b_NTm = [Buf() for _ in range(2)]
    Pb = [S.sb("Pb%d" % i, [64, 8, 64], BF16) for i in range(2)]; b_Pb = [Buf() for _ in range(2)]
    PTb = [S.sb("PTb%d" % i, [64, 8, 64], BF16) for i in range(2)]; b_PTb = [Buf() for _ in range(2)]
    T32 = [S.sb("T32_%d" % i, [64, 8, 64], F32) for i in range(2)]; b_T32 = [Buf() for _ in range(2)]
    Tb = [S.sb("Tb%d" % i, [64, 8, 64], BF16) for i in range(2)]; b_Tb = [Buf() for _ in range(2)]
    WTb = S.sb("WTb", [64, 8, 64], BF16); b_WT = Buf()
    S32 = S.sb("S32", [64, 8, 64], F32); b_S32 = Buf()
    Sb = S.sb("Sb", [64, 8, 64], BF16); b_Sb = Buf()
    PS = [S.ps("psb%d" % i, [128, 512], F32) for i in range(8)]; b_ps = [Buf() for _ in range(8)]

    S.dma("sync", maskG[:], mg_d, w=[b_c]); S.dma("sync", maskNT[:], mn_d, w=[b_c]); S.dma("sync", id8[:], id_d, w=[b_c])
    S.op("vector", lambda e: e.memset(S32[:], 0.0), w=[b_S32])
    S.op("vector", lambda e: e.memset(Sb[:], 0.0), w=[b_Sb])
    out_ops = []
    ngrp = (nch + SGS - 1) // SGS

    def load_group(gi):
        gp = gi % 2
        s0 = gi * SGS; n = min(SGS, nch - s0)
        S.dma("sync", ARs[gp][:, 0:n], AR_d[:, s0:s0 + n], w=[b_AR[gp]])
        S.dma("sync", BKs[gp][:, 0:n], BK_d[:, s0:s0 + n], w=[b_BK[gp]])
        S.dma("gpsimd", BTs[gp][:, 0:n], BKT_d[0:64, s0:s0 + n], w=[b_BKT[gp]])
        S.dma("gpsimd", KTs[gp][:, 0:n], BKT_d[64:128, s0:s0 + n], w=[b_BKT[gp]])
        S.dma("gpsimd", VTs[gp][:, 0:n], VT_d[:, s0:s0 + n], w=[b_UVv[gp]])
        S.dma("sync", PCs[gp][:, 0:n], PC_d[:, s0:s0 + n], w=[b_PC[gp]])

    def prep_stages(j):
        gp = (j // SGS) % 2; g = j % SGS; sp = j % 2
        AR = ARs[gp]; BK = BKs[gp]
        st = []

        def s_G():
            for i in range(8):
                S.op("tensor", lambda e, i=i: e.matmul(PS[i // 4][0:64, (i % 4) * 128:(i % 4) * 128 + 128], BK[:, g, i, 0:64], AR[:, g, i, :],
                                                       start=True, stop=True), r=[b_BK[gp], b_AR[gp]], w=[b_ps[i // 4]])
            for i in range(8):
                S.op("tensor", lambda e, i=i: e.matmul(PS[3 + i // 4][0:64, (i % 4) * 128:(i % 4) * 128 + 128], BK[:, g, i, 64:128], AR[:, g, i, :],
                                                       start=True, stop=True), r=[b_BK[gp], b_AR[gp]], w=[b_ps[3 + i // 4]])
            for i in range(8):
                S.op("tensor", lambda e, i=i: e.matmul(PS[2][0:64, i * 64:(i + 1) * 64], AR[:, g, i, 0:64], BK[:, g, i, 0:64],
                                                       start=True, stop=True), r=[b_BK[gp], b_AR[gp]], w=[b_ps[2]])
            for h in range(2):
                S.op("vector", lambda e, h=h: e.tensor_tensor(Gm[sp][:, 4 * h:4 * h + 4, :], PS[h][0:64, :].rearrange("p (a b) -> p a b", a=4),
                                                              maskG[0:64, 4 * h:4 * h + 4, :], ALU.mult), r=[b_ps[h], b_c], w=[b_Gm[sp]])
                S.op("vector", lambda e, h=h: e.tensor_tensor(Gk[sp][:, 4 * h:4 * h + 4, :], PS[3 + h][0:64, :].rearrange("p (a b) -> p a b", a=4),
                                                              maskG[0:64, 4 * h:4 * h + 4, :], ALU.mult), r=[b_ps[3 + h], b_c], w=[b_Gm[sp]])
            S.op("vector", lambda e: e.tensor_tensor(NTm[sp][:], PS[2][0:64, :].rearrange("p (a b) -> p a b", a=8), maskNT[:], ALU.mult),
                 r=[b_ps[2], b_c], w=[b_NTm[sp]])
            S.op("vector", lambda e: e.tensor_tensor(T32[sp][:], Gm[sp][0:64, :, 0:64], id8[:], ALU.add), r=[b_Gm[sp], b_c], w=[b_T32[sp]])
            S.op("scalar", lambda e: e.copy(Tb[sp][:], T32[sp][:]), r=[b_T32[sp]], w=[b_Tb[sp]])
        st.append(s_G)
        for l in range(1, 6):
            def s_sq(l=l):
                if l == 1:
                    Pp = lambda i: Gm[sp][0:64, i, 0:64]; PTp = lambda i: NTm[sp][:, i, :]
                    rP = [b_Gm[sp]]; rPT = [b_NTm[sp]]
                else:
                    q = (l - 1) % 2
                    Pp = lambda i, q=q: Pb[q][:, i, :]; PTp = lambda i, q=q: PTb[q][:, i, :]
                    rP = [b_Pb[q]]; rPT = [b_PTb[q]]
                qn = l % 2
                if l < 5:
                    for i in range(8):
                        S.op("tensor", lambda e, i=i: e.matmul(PS[3][0:64, i * 64:(i + 1) * 64], PTp(i), Pp(i), start=True, stop=True),
                             r=rP + rPT, w=[b_ps[3]])
                for i in range(8):
                    S.op("tensor", lambda e, i=i: e.matmul(PS[4][0:64, i * 64:(i + 1) * 64], Pp(i), PTp(i), start=True, stop=True),
                         r=rP + rPT, w=[b_ps[4]])
                if l < 5:
                    S.op("scalar", lambda e: e.copy(Pb[qn][:], PS[3][0:64, :].rearrange("p (a b) -> p a b", a=8)), r=[b_ps[3]], w=[b_Pb[qn]])
                S.op("vector", lambda e: e.tensor_copy(PTb[qn][:], PS[4][0:64, :].rearrange("p (a b) -> p a b", a=8)), r=[b_ps[4]], w=[b_PTb[qn]])
            st.append(s_sq)

            def s_T(l=l):
                qn = l % 2
                for i in range(8):
                    S.op("tensor", lambda e, i=i: e.matmul(PS[2][0:64, i * 64:(i + 1) * 64], PTb[qn][:, i, :], Tb[sp][:, i, :], start=True, stop=True),
                         r=[b_PTb[qn], b_Tb[sp]], w=[b_ps[2]])
                S.op("vector", lambda e: e.tensor_tensor(T32[sp][:], T32[sp][:], PS[2][0:64, :].rearrange("p (a b) -> p a b", a=8), ALU.add),
                     r=[b_ps[2]], w=[b_T32[sp]])
                S.op("scalar", lambda e: e.copy(Tb[sp][:], T32[sp][:]), r=[b_T32[sp]], w=[b_Tb[sp]])
            st.append(s_T)
        return st

    def seq_stages(j):
        gp = (j // SGS) % 2; g = j % SGS; sp = j % 2
        AR = ARs[gp]; UT = UTs[gp]; VT = VTs[gp]; BT = BTs[gp]; KT = KTs[gp]
        st = []

        def s_W():
            for i in range(8):
                reg = PS[5][0:64, i * 64:(i + 1) * 64]
                S.op("tensor", lambda e, i=i, reg=reg: e.matmul(reg, AR[:, g, i, 0:64], Sb[:, i, :], start=True, stop=False),
                     r=[b_AR[gp], b_Sb], w=[b_ps[5]])
                S.op("tensor", lambda e, i=i, reg=reg: e.matmul(reg, Gk[sp][:, i, 0:64], VT[:, g, i, :], start=False, stop=True),
                     r=[b_Gm[sp], b_UVv[gp]], w=[b_ps[5]])
            S.op("scalar", lambda e: e.copy(WTb[:], PS[5][0:64, :].rearrange("p (a b) -> p a b", a=8)), r=[b_ps[5]], w=[b_WT])
        st.append(s_W)

        def s_U():
            for i in range(8):
                S.op("tensor", lambda e, i=i: e.matmul(PS[6][0:64, i * 64:(i + 1) * 64], Tb[sp][:, i, :], WTb[:, i, :], start=True, stop=True),
                     r=[b_Tb[sp], b_WT], w=[b_ps[6]])
            S.op("vector", lambda e: e.tensor_copy(UT[:, g, :, :], PS[6][0:64, :].rearrange("p (a b) -> p a b", a=8)),
                 r=[b_ps[6]], w=[b_UVu[gp][g]])
        st.append(s_U)

        def s_YS():
            for i in range(8):
                reg = PS[7][0:64, i * 64:(i + 1) * 64]
                S.op("tensor", lambda e, i=i, reg=reg: e.matmul(reg, AR[:, g, i, 64:128], Sb[:, i, :], start=True, stop=False),
                     r=[b_AR[gp], b_Sb], w=[b_ps[7]])
                S.op("tensor", lambda e, i=i, reg=reg: e.matmul(reg, Gm[sp][:, i, 64:128], UT[:, g, i, :], start=False, stop=False),
                     r=[b_Gm[sp], b_UVu[gp][g]], w=[b_ps[7]])
                S.op("tensor", lambda e, i=i, reg=reg: e.matmul(reg, Gk[sp][:, i, 64:128], VT[:, g, i, :], start=False, stop=True),
                     r=[b_Gm[sp], b_UVv[gp]], w=[b_ps[7]])
            for i in range(8):
                S.op("tensor", lambda e, i=i: e.matmul(PS[5][0:64, i * 64:(i + 1) * 64], BT[:, g, i, :], UT[:, g, i, :], start=True, stop=False),
                     r=[b_BKT[gp], b_UVu[gp][g]], w=[b_ps[5]])
                S.op("tensor", lambda e, i=i: e.matmul(PS[5][0:64, i * 64:(i + 1) * 64], KT[:, g, i, :], VT[:, g, i, :], start=False, stop=True),
                     r=[b_BKT[gp], b_UVv[gp]], w=[b_ps[5]])
            S.op("scalar", lambda e: e.copy(Yst[gp][:, g, :, :], PS[7][0:64, :].rearrange("p (a b) -> p a b", a=8)), r=[b_ps[7]], w=[b_Y[gp]])
            S.op("vector", lambda e: e.tensor_tensor(S32[:], S32[:], PS[5][0:64, :].rearrange("p (a b) -> p a b", a=8), ALU.add),
                 r=[b_ps[5]], w=[b_S32])
            S.op("vector", lambda e: e.tensor_tensor(S32[:], S32[:], PCs[gp][:, g, :].unsqueeze(2).to_broadcast([64, 8, 64]), ALU.mult),
                 r=[b_PC[gp]], w=[b_S32])
            S.op("scalar", lambda e: e.copy(Sb[:], S32[:]), r=[b_S32], w=[b_Sb])
        st.append(s_YS)
        return st

    load_group(0)
    if ngrp > 1:
        load_group(1)
    for f in prep_stages(0):
        f()
    for j in range(nch):
        seq = seq_stages(j)
        pre = prep_stages(j + 1) if j + 1 < nch else []
        pos = {1: 0, 4: 1, 7: 2}
        k = 0
        nseq = {None: 3, "prep": 0, "W": 1, "U": 2}[mode]
        for pi in range(max(len(pre), 8)):
            if pi < len(pre):
                pre[pi]()
            if pi in pos:
                if pos[pi] < nseq:
                    seq[pos[pi]]()
                k += 1
        for si in range(k, 3):
            if si < nseq:
                seq[si]()
        if (j + 1) % SGS == 0 or j == nch - 1:
            gi = j // SGS
            s0 = gi * SGS; n = min(SGS, nch - s0)
            out_ops.append(S.dma("sync", Y_d[:, s0:s0 + n], Yst[gi % 2][:, 0:n], r=[b_Y[gi % 2]]))
            if gi + 2 < ngrp:
                load_group(gi + 2)
    S.emit(out_ops)
    return nc


def scan_consts():
    s = np.arange(64)[:, None]; t = np.arange(64)[None, :]
    mg = np.zeros((128, 8, 128), np.float32); mn = np.zeros((64, 8, 64), np.float32); idm = np.zeros((64, 8, 64), np.float32)
    for i in range(8):
        z = i // 4
        strict = (s < t) if z == 0 else (s > t)
        incl = (s <= t) if z == 0 else (s >= t)
        blk = np.concatenate([strict, incl], axis=1).astype(np.float32)
        mg[:, i, :] = np.concatenate([blk, blk], axis=0)
        mn[:, i, :] = strict.T.astype(np.float32)
        idm[:, i, :] = np.eye(64, dtype=np.float32)
    return dict(maskG=mg, maskNT=mn, ident8=idm)


GN_EPS = 64e-5


def build_rout(ntok=NTOK):
    nc = new_nc()
    x_d = dram_in(nc, "xin", [ntok, D]); y0_d = dram_in(nc, "y0", [ntok, D]); y1_d = dram_in(nc, "y1", [ntok, D])
    bv_d = dram_in(nc, "bv", [ntok, D]); ga_d = dram_in(nc, "gate", [ntok, D])
    lg_d = dram_in(nc, "lngb", [128, D]); lb_d = dram_in(nc, "lnbb", [128, D]); gt_d = dram_in(nc, "gtb", [128, 2, D])
    wo_d = dram_in(nc, "wo", [D, D]); id_d = dram_in(nc, "ident", [128, 128])
    xout = dram_out(nc, "xout", [ntok, D])
    S = Sched(nc)
    ident = S.sb("ident", [128, 128], F32); b_id = Buf()
    lgb = S.sb("lgb", [128, D], F32); lbb = S.sb("lbb", [128, D], F32); gtb = S.sb("gtb", [128, 2, D], F32); b_c = Buf()
    wo = S.sb("wo", [128, 8, D], BF16); b_wo = Buf()
    names = ["x", "y0", "y1", "bv", "ga"]
    srcs = [x_d, y0_d, y1_d, bv_d, ga_d]
    tl = [[S.sb("%s%d" % (n, i), [128, D], F32) for n in names] for i in range(2)]
    b_tl = [[Buf() for _ in names] for _ in range(2)]
    yc = S.sb("yc", [128, D], F32); b_yc = Buf()
    sq = S.sb("sq", [128, D], F32); b_sq = Buf()
    zT = [S.sb("zT%d" % i, [128, 8, 128], BF16) for i in range(2)]; b_zT = [Buf() for _ in range(2)]
    ot = [S.sb("ot%d" % i, [128, D], F32) for i in range(2)]; b_ot = [Buf() for _ in range(2)]
    sm = [S.sb("sm%d" % i, [128, 4, 16], F32) for i in range(2)]; b_sm = [Buf() for _ in range(2)]
    PS = [S.ps("psb%d" % i, [128, 512], F32) for i in range(8)]; b_ps = [Buf() for _ in range(8)]
    S.dma("sync", ident[:], id_d, w=[b_id]); S.dma("sync", lgb[:], lg_d, w=[b_c]); S.dma("sync", lbb[:], lb_d, w=[b_c])
    S.dma("sync", gtb[:], gt_d, w=[b_c])
    S.dma("gpsimd", wo[:], wo_d.rearrange("(c p) n -> p c n", p=128), w=[b_wo])
    out_ops = []
    for ti, (off, P) in enumerate(token_tiles(ntok)):
        par = ti % 2
        v = 1 if off >= NLAT and ntok > NLAT else 0
        T = tl[par]; B = b_tl[par]
        for n in range(5):
            S.dma("sync" if n % 2 == 0 else "gpsimd", T[n][0:P, :], srcs[n][off:off + P, :], w=[B[n]])
        X, Y0, Y1, BVt, GA = [t[0:P, :] for t in T]
        m = sm[par]
        s1 = m[0:P, 0, :]; nm = m[0:P, 1, :]; s2 = m[0:P, 2, :]; rs = m[0:P, 3, :]
        v3 = lambda ap: ap.rearrange("p (h k) -> p h k", k=64)
        bc = lambda ap, P: ap.unsqueeze(2).to_broadcast([P, 16, 64])
        S.op("vector", lambda e, Y0=Y0, Y1=Y1: e.tensor_tensor(Y0, Y0, Y1, ALU.add), r=[B[2]], w=[B[1]])
        S.op("vector", lambda e, Y0=Y0, s1=s1: e.reduce_sum(s1, v3(Y0), AX.X), r=[B[1]], w=[b_sm[par]])
        S.op("vector", lambda e, s1=s1, nm=nm: e.tensor_scalar(nm, s1, -1.0 / 64, None, ALU.mult), r=[b_sm[par]], w=[b_sm[par]])
        S.op("vector", lambda e, Y0=Y0, nm=nm, P=P: e.tensor_tensor(v3(yc[0:P, :]), v3(Y0), bc(nm, P), ALU.add), r=[B[1], b_sm[par]], w=[b_yc])
        S.op("scalar", lambda e, P=P: e.activation(sq[0:P, :], yc[0:P, :], AF.Square), r=[b_yc], w=[b_sq])
        S.op("vector", lambda e, s2=s2, P=P: e.reduce_sum(s2, v3(sq[0:P, :]), AX.X), r=[b_sq], w=[b_sm[par]])
        S.op("vector", lambda e, s2=s2, rs=rs: e.tensor_scalar(rs, s2, 1.0 / 64, GN_EPS, ALU.mult, ALU.add), r=[b_sm[par]], w=[b_sm[par]])
        S.op("scalar", lambda e, rs=rs: e.activation(rs, rs, AF.Sqrt), r=[b_sm[par]], w=[b_sm[par]])
        S.op("vector", lambda e, rs=rs: e.reciprocal(rs, rs), r=[b_sm[par]], w=[b_sm[par]])
        S.op("vector", lambda e, rs=rs, P=P: e.tensor_tensor(v3(yc[0:P, :]), v3(yc[0:P, :]), bc(rs, P), ALU.mult), r=[b_sm[par]], w=[b_yc])
        S.op("vector", lambda e, P=P: e.tensor_tensor(yc[0:P, :], yc[0:P, :], lgb[0:P, :], ALU.mult), r=[b_c], w=[b_yc])
        S.op("gpsimd", lambda e, P=P: e.tensor_tensor(yc[0:P, :], yc[0:P, :], lbb[0:P, :], ALU.add), r=[b_c], w=[b_yc])
        S.op("gpsimd", lambda e, P=P, BVt=BVt: e.tensor_tensor(yc[0:P, :], yc[0:P, :], BVt, ALU.add), r=[B[3]], w=[b_yc])
        S.op("vector", lambda e, P=P, GA=GA: e.tensor_tensor(yc[0:P, :], yc[0:P, :], GA, ALU.mult), r=[B[4]], w=[b_yc])
        for c in range(8):
            pb = c // 4
            S.op("tensor", lambda e, P=P, c=c, pb=pb: e.transpose(PS[pb][:, (c % 4) * 128:(c % 4) * 128 + P], yc[0:P, c * 128:(c + 1) * 128], ident[0:P, 0:P]),
                 r=[b_yc, b_id], w=[b_ps[pb]])
        for pb in range(2):
            S.op("scalar", lambda e, P=P, pb=pb, par=par: e.copy(zT[par][:, 4 * pb:4 * pb + 4, 0:P],
                                                                  PS[pb][:, :].rearrange("p (a b) -> p a b", a=4)[:, :, 0:P]),
                 r=[b_ps[pb]], w=[b_zT[par]])
        for n in range(2):
            pb = 2 + n
            for c in range(8):
                S.op("tensor", lambda e, P=P, c=c, n=n, pb=pb, par=par: e.matmul(PS[pb][0:P, :], zT[par][:, c, 0:P], wo[:, c, n * 512:(n + 1) * 512],
                                                                                 start=(c == 0), stop=(c == 7)),
                     r=[b_zT[par], b_wo], w=[b_ps[pb]])
            sl = slice(n * 512, (n + 1) * 512)
            S.op("vector", lambda e, P=P, pb=pb, par=par, sl=sl, v=v: e.tensor_tensor(ot[par][0:P, sl], PS[pb][0:P, :], gtb[0:P, v, sl], ALU.mult),
                 r=[b_ps[pb], b_c], w=[b_ot[par]])
        S.op("gpsimd", lambda e, P=P, par=par, X=X: e.tensor_tensor(ot[par][0:P, :], ot[par][0:P, :], X, ALU.add), r=[B[0]], w=[b_ot[par]])
        out_ops.append(S.dma("sync", xout[off:off + P, :], ot[par][0:P, :], r=[b_ot[par]]))
    S.emit(out_ops)
    return nc


SEQ = 16384
CTX = 256
NCH_ALL = (SEQ + CTX) // 64
_SCAN_NC = {}
_ROUT_NC = {}
DEBUG_HOOK = None


def _dbg(name, arr):
    if DEBUG_HOOK is not None:
        DEBUG_HOOK(name, arr)


def run_rwkv_mixer(x, xc, i, mods, inp):
    j = i // 2
    pres = run_prep(x, xc, i, mods, inp)
    order = [np.arange(NCH_ALL), np.concatenate([np.arange(3, -1, -1), np.arange(NCH_ALL - 1, 3, -1)])]
    consts = scan_consts()
    maps = []
    for k in range(NCORES):
        b, hq = k // 4, k % 4
        if hq == 0:
            obs = [np.asarray(pres[b * 4 + q]["ob"]) for q in range(4)]
            pcs = [np.asarray(pres[b * 4 + q]["pc"]) for q in range(4)]
            F = np.concatenate([obs[0][:, :, :, NLAT:]] + [o[:, :, :, :NLAT] for o in obs], axis=3)
            F = F.reshape(9, 8, 128, NCH_ALL, 64)
            PCa = np.concatenate([pcs[0][:, :, :, NLAT // 64:]] + [p[:, :, :, :NLAT // 64] for p in pcs], axis=3)
        AR = np.empty((64, NCH_ALL, 8, 128), F.dtype); BK = np.empty((64, NCH_ALL, 8, 128), F.dtype)
        BKT = np.empty((128, NCH_ALL, 8, 64), F.dtype); VT = np.empty((64, NCH_ALL, 8, 64), F.dtype)
        PCB = np.empty((64, NCH_ALL, 8), np.float32)
        for ci in range(8):
            z, hl = ci // 4, ci % 4
            c = 2 * hq + hl // 2; p0 = (hl % 2) * 64
            od = order[z]
            A = F[0 + z, c, p0:p0 + 64][:, od]; R = F[2 + z, c, p0:p0 + 64][:, od]
            Bm = F[4 + z, c, p0:p0 + 64][:, od]; Km = F[6 + z, c, p0:p0 + 64][:, od]
            V = F[8, c, p0:p0 + 64][:, od]
            AR[:, :, ci, 0:64] = A; AR[:, :, ci, 64:128] = R
            BK[:, :, ci, 0:64] = Bm; BK[:, :, ci, 64:128] = Km
            BKT[0:64, :, ci, :] = Bm.transpose(2, 1, 0); BKT[64:128, :, ci, :] = Km.transpose(2, 1, 0)
            VT[:, :, ci, :] = V.transpose(2, 1, 0)
            PCB[:, :, ci] = PCa[z, c, p0:p0 + 64][:, od]
        m = dict(consts)
        m.update(AR=AR, BK=BK, BKT=BKT, VT=VT, PCB=PCB)
        maps.append(m)
    if NCH_ALL not in _SCAN_NC:
        _SCAN_NC[NCH_ALL] = build_scan(nch=NCH_ALL)
    sres = run_bass_kernel_spmd(_SCAN_NC[NCH_ALL], maps, core_ids=list(range(NCORES))).results
    yall = np.empty((2, 2, NCH_ALL, 64, 16, 64), np.float32)
    for k in range(NCORES):
        b, hq = k // 4, k % 4
        Y = np.asarray(sres[k]["Y"])
        for ci in range(8):
            z, hl = ci // 4, ci % 4
            yall[z, b, order[z], :, 4 * hq + hl, :] = Y[:, :, ci, :].transpose(1, 0, 2)
    yall = yall.reshape(2, 2, NCH_ALL * 64, D)
    _dbg("y_l%d" % i, yall)
    P = {kk: inp["rw_" + kk][j] for kk in ["wo", "ln_g", "ln_b"]}
    base = dict(lngb=bcast(P["ln_g"]), lnbb=bcast(P["ln_b"]), wo=np.ascontiguousarray(P["wo"]), ident=np.eye(128, dtype=np.float32))
    maps = []
    for k in range(NCORES):
        b, q = k // 4, k % 4
        lat = slice(CTX + q * NLAT, CTX + (q + 1) * NLAT); cx = slice(q * NCTX, (q + 1) * NCTX)
        of = np.asarray(pres[k]["of"])
        oft = of.transpose(0, 3, 1, 2).reshape(2, NLAT + CTX, D)
        tok = np.concatenate([np.arange(NLAT), NLAT + np.arange(q * NCTX, (q + 1) * NCTX)])
        ml = mods[i, b]; mcx = mods[i, 2]
        m = dict(base)
        m.update(xin=np.concatenate([x[b, q * NLAT:(q + 1) * NLAT], xc[b, cx]], axis=0),
                 y0=np.concatenate([yall[0, b, lat], yall[0, b, cx]], axis=0),
                 y1=np.concatenate([yall[1, b, lat], yall[1, b, cx]], axis=0),
                 bv=np.ascontiguousarray(oft[0][tok]), gate=np.ascontiguousarray(oft[1][tok]),
                 gtb=np.ascontiguousarray(np.stack([bcast(ml[2 * D:3 * D]), bcast(mcx[2 * D:3 * D])], axis=1)))
        maps.append(m)
    if NTOK not in _ROUT_NC:
        _ROUT_NC[NTOK] = build_rout(ntok=NTOK)
    ores = run_bass_kernel_spmd(_ROUT_NC[NTOK], maps, core_ids=list(range(NCORES))).results
    return [np.asarray(r["xout"]) for r in ores]


def kernel(**inp):
    inp = {k: np.asarray(v) for k, v in inp.items()}
    x = np.array(inp["x"], np.float32)
    xc = np.array(inp["ctx"], np.float32)
    mods = run_mod(inp["c"], inp["c_ctx"], inp["ada_w"], inp["ada_b"])
    for i in range(4):
        last = i == 3
        if i % 2 == 0:
            xs = []
            for k in range(NCORES):
                b, q = k // 4, k % 4
                xs.append(np.concatenate([x[b, q * NLAT:(q + 1) * NLAT], xc[b, (q // 2) * 128:(q // 2 + 1) * 128]], axis=0))
            outs = run_gmlp(xs, i, mods, inp)
            mix = []
            for k in range(NCORES):
                q = k % 4
                mix.append(np.concatenate([outs[k][:NLAT], outs[k][NLAT + (q % 2) * 64:NLAT + (q % 2) * 64 + 64]], axis=0))
        else:
            mix = run_rwkv_mixer(x, xc, i, mods, inp)
        _dbg("mix%d" % i, mix)
        outs = run_moe(mix, i, mods, inp, final=last)
        xn = np.empty_like(x); xcn = np.empty_like(xc)
        for k in range(NCORES):
            b, q = k // 4, k % 4
            xn[b, q * NLAT:(q + 1) * NLAT] = outs[k][:NLAT]
            xcn[b, q * NCTX:(q + 1) * NCTX] = outs[k][NLAT:]
        x, xc = xn, xcn
        _dbg("x%d" % i, x)
    return x
```

```python
from contextlib import ExitStack
import numpy as np
import concourse.bass as bass
import concourse.mybir as mybir
from concourse.bass_utils import run_bass_kernel_spmd

F32 = mybir.dt.float32
BF16 = mybir.dt.bfloat16
AF = mybir.ActivationFunctionType
ALU = mybir.AluOpType
AX = mybir.AxisListType

COMPUTE = ("tensor", "vector", "scalar", "gpsimd")
NDMASEM = 16
STRICT = False
NCORES = 8
D = 1024


class Buf:
    __slots__ = ("name", "lw", "rd")

    def __init__(self, name=""):
        self.name = name
        self.lw = None
        self.rd = []


class Sched:
    def __init__(self, nc):
        self.nc = nc
        self.ops = []
        self.stack = ExitStack()
        self.ndma = 0

    def sb(self, name, shape, dt):
        t = self.stack.enter_context(self.nc.sbuf_tensor("s_" + name, shape, dt))
        return t

    def ps(self, name, shape, dt):
        return self.stack.enter_context(self.nc.psum_tensor("p_" + name, shape, dt))

    def _deps(self, r, w):
        d = set()
        raw = set()
        for b in r:
            if b.lw is not None:
                d.add(b.lw)
                raw.add(b.lw)
        for b in w:
            if b.lw is not None:
                d.add(b.lw)
            d.update(b.rd)
        self._raw = set(d) if STRICT else raw
        return d

    def _commit(self, idx, r, w, eng, kind):
        for b in r:
            if kind == "c":
                b.rd = [i for i in b.rd if not (self.ops[i]["kind"] == "c" and self.ops[i]["eng"] == eng)]
            b.rd.append(idx)
        for b in w:
            b.lw = idx
            b.rd = []

    def op(self, eng, fn, r=(), w=()):
        idx = len(self.ops)
        self.ops.append(dict(eng=eng, fn=fn, deps=self._deps(r, w), kind="c", sig=False))
        self.ops[-1]["raw"] = self._raw
        self._commit(idx, r, w, eng, "c")
        return idx

    def dma(self, eng, out, in_, r=(), w=(), **kw):
        idx = len(self.ops)
        k = self.ndma
        self.ndma += 1
        self.ops.append(dict(eng=eng, fn=None, out=out, in_=in_, kw=kw, deps=self._deps(r, w),
                             kind="d", k=k, sig=True))
        self.ops[-1]["raw"] = self._raw
        self._commit(idx, r, w, eng, "d")
        return idx

    def emit(self, final_wait_ops=()):
        nc = self.nc
        ops = self.ops
        for i, o in enumerate(ops):
            for d in o["deps"]:
                od = ops[d]
                if od["kind"] == "c" and (od["eng"] != o["eng"] or o["kind"] == "d"
                                          or (d in o["raw"] and od["eng"] != "tensor")):
                    od["sig"] = True
        cnt = {e: 0 for e in COMPUTE}
        for o in ops:
            if o["kind"] == "c" and o["sig"]:
                cnt[o["eng"]] += 1
                o["sv"] = cnt[o["eng"]]
        NS = 2 * NDMASEM
        dcnt = [0] * NS
        prev_on_sem = [None] * NS
        qcount = {"sync": 0, "gpsimd": 0}
        for i, o in enumerate(ops):
            if o["kind"] == "d":
                qi = qcount[o["eng"]]
                qcount[o["eng"]] += 1
                s = (qi % NDMASEM) + (NDMASEM if o["eng"] == "gpsimd" else 0)
                dcnt[s] += 1
                o["ds"] = s
                o["dv"] = 16 * dcnt[s]
                o["prev"] = prev_on_sem[s]
                prev_on_sem[s] = i
        with ExitStack() as st:
            csem = {e: st.enter_context(nc.semaphore("cs_" + e)) for e in COMPUTE}
            dsem = [st.enter_context(nc.semaphore("ds_%d" % i)) for i in range(2 * NDMASEM)]
            block = st.enter_context(nc.Block())
            engines = ["tensor", "vector", "scalar", "gpsimd", "sync"]
            by_eng = {e: [] for e in engines}
            for i, o in enumerate(ops):
                by_eng[o["eng"]].append(i)
            finals = list(final_wait_ops)

            def make(ename):
                def body(e):
                    waited = {}

                    def wait(key, sem, val):
                        if waited.get(key, 0) >= val:
                            return
                        waited[key] = val
                        e.wait_ge(sem, val)

                    for i in by_eng[ename]:
                        o = ops[i]
                        for d in sorted(o["deps"]):
                            od = ops[d]
                            if od["kind"] == "c":
                                if od["eng"] == ename and o["kind"] == "c" and (
                                        d not in o["raw"] or ename == "tensor"):
                                    continue
                                wait(("c", od["eng"]), csem[od["eng"]], od["sv"])
                            else:
                                wait(("d", od["ds"]), dsem[od["ds"]], od["dv"])
                        if o["kind"] == "d":
                            if o["prev"] is not None:
                                op_ = ops[o["prev"]]
                                wait(("d", op_["ds"]), dsem[op_["ds"]], op_["dv"])
                            ins = e.dma_start(out=o["out"], in_=o["in_"], **o["kw"])
                            ins.then_inc(dsem[o["ds"]], 16)
                        else:
                            ins = o["fn"](e)
                            if o["sig"]:
                                ins.then_inc(csem[ename], 1)
                    if ename == "sync":
                        for i in finals:
                            od = ops[i]
                            wait(("d", od["ds"]), dsem[od["ds"]], od["dv"])
                return body

            block.tensor(make("tensor"))
            block.vector(make("vector"))
            block.scalar(make("scalar"))
            block.gpsimd(make("gpsimd"))
            block.sync(make("sync"))
        self.stack.close()


def new_nc():
    return bass.Bass("TRN2", target_bir_lowering=False)


def dram_in(nc, name, shape, dt=F32):
    return nc.dram_tensor(name, list(shape), dt, kind="ExternalInput").ap()


def dram_out(nc, name, shape, dt=F32):
    return nc.dram_tensor(name, list(shape), dt, kind="ExternalOutput").ap()


def cols(v):
    return np.ascontiguousarray(np.asarray(v, np.float32).reshape(-1, 128).T)


def bcast(v, p=128):
    v = np.asarray(v, np.float32).reshape(1, -1)
    return np.ascontiguousarray(np.broadcast_to(v, (p, v.shape[1])))


_MOD_NC = None


def build_mod():
    nc = new_nc()
    NH = 3072
    cT = dram_in(nc, "cT", [128, 8, 3])
    aw = dram_in(nc, "aw", [1024, NH])
    ab = dram_in(nc, "ab", [3, NH])
    out = dram_out(nc, "mod", [3, NH])
    S = Sched(nc)
    ct = S.sb("ct", [128, 8, 3], F32); bct = Buf()
    sg = S.sb("sg", [128, 8, 3], F32)
    wt = S.sb("wt", [128, 8, NH], F32); bwt = [Buf() for _ in range(8)]
    bt = S.sb("bt", [3, NH], F32); bbt = Buf()
    ot = S.sb("ot", [3, NH], F32); bot = Buf()
    pss = [S.ps("ps%d" % i, [128, 512], F32) for i in range(2)]; bps = [Buf() for _ in range(2)]
    S.dma("sync", ct[:], cT, w=[bct])
    S.dma("sync", bt[:], ab, w=[bbt])
    awv = aw.rearrange("(c p) n -> p c n", p=128)
    for c in range(8):
        S.dma("sync" if c % 2 == 0 else "gpsimd", wt[:, c, :], awv[:, c, :], w=[bwt[c]])
    S.op("scalar", lambda e: e.activation(sg[:], ct[:], AF.Sigmoid), r=[bct], w=[bot])
    S.op("vector", lambda e: e.tensor_tensor(ct[:], ct[:], sg[:], ALU.mult), r=[bot, bct], w=[bct])
    for n in range(NH // 512):
        p = pss[n % 2]; bp = bps[n % 2]
        for c in range(8):
            S.op("tensor", lambda e, p=p, c=c, n=n: e.matmul(p[0:3, :], ct[:, c, :], wt[:, c, n * 512:(n + 1) * 512],
                                                              start=(c == 0), stop=(c == 7)),
                 r=[bct, bwt[c]], w=[bp])
        S.op("vector", lambda e, p=p, n=n: e.tensor_tensor(ot[:, n * 512:(n + 1) * 512], p[0:3, :],
                                                           bt[:, n * 512:(n + 1) * 512], ALU.add),
             r=[bp, bbt], w=[bot])
    o = S.dma("sync", out, ot[:], r=[bot])
    S.emit([o])
    return nc


def run_mod(c, c_ctx, ada_w, ada_b):
    global _MOD_NC
    if _MOD_NC is None:
        _MOD_NC = build_mod()
    cvec = np.stack([c[0], c[1], c_ctx], 0).astype(np.float32)
    cT = np.ascontiguousarray(cvec.reshape(3, 8, 128).transpose(2, 1, 0))
    maps = []
    for k in range(NCORES):
        l, h = k // 2, k % 2
        maps.append(dict(cT=cT, aw=np.ascontiguousarray(ada_w[l][:, h * 3072:(h + 1) * 3072]),
                         ab=bcast(ada_b[l][h * 3072:(h + 1) * 3072], 3)))
    res = run_bass_kernel_spmd(_MOD_NC, maps, core_ids=list(range(NCORES)))
    mods = np.zeros((4, 3, 6144), np.float32)
    for k in range(NCORES):
        l, h = k // 2, k % 2
        mods[l, :, h * 3072:(h + 1) * 3072] = res.results[k]["mod"]
    return mods


NLAT = 4096
NCTX = 64
NTOK = NLAT + NCTX
NEXP = 32
FH = 512


def token_tiles(ntok):
    tiles = []
    o = 0
    while o < ntok:
        p = min(128, ntok - o)
        tiles.append((o, p))
        o += p
    return tiles


def build_moe(final=False, ntok=NTOK, nexp=NEXP, dbg=False):
    nc = new_nc()
    xin = dram_in(nc, "xin", [ntok, D])
    wr_d = dram_in(nc, "wr", [128, 8, 36])
    br_d = dram_in(nc, "br", [128, 36])
    mc_d = dram_in(nc, "mcols", [128, 6, 8])
    gt_d = dram_in(nc, "gtb", [128, 2, D])
    fg_d = dram_in(nc, "fgb", [128, D])
    id_d = dram_in(nc, "ident", [128, 128])
    wg_d = dram_in(nc, "wg", [nexp, D, FH])
    wu_d = dram_in(nc, "wu", [nexp, D, FH])
    wd_d = dram_in(nc, "wd", [nexp, FH, D])
    xout = dram_out(nc, "xout", [ntok, D])
    S = Sched(nc)
    tiles = token_tiles(ntok)
    nt = len(tiles)
    sts = []
    i = 0
    while i < nt:
        j = min(i + 8, nt)
        if nt - j < 4:
            j = nt
        sts.append(list(range(i, j)))
        i = j
    MAXT = max(len(s) for s in sts)
    MAXTOK = MAXT * 128

    ident = S.sb("ident", [128, 128], F32); b_id = Buf()
    wr = S.sb("wr", [128, 8, 36], F32); b_wr = Buf()
    br = S.sb("br", [128, 36], F32); b_br = Buf()
    mc = S.sb("mc", [128, 6, 8], F32); b_mc = Buf()
    gsc = S.sb("gsc", [128, 2, 8], F32); b_gsc = Buf()
    gtb = S.sb("gtb", [128, 2, D], F32); b_gt = Buf()
    fgb = S.sb("fgb", [128, D], F32); b_fg = Buf()
    hTb = S.sb("hTb", [128, 8, MAXTOK], BF16); b_hTb = [Buf() for _ in range(MAXT)]
    acc = S.sb("acc", [128, MAXT, D], F32); b_acc = [Buf() for _ in range(MAXT)]
    coef = S.sb("coef", [128, MAXT, 32], F32); b_coef = [Buf() for _ in range(MAXT)]
    HT = [S.sb("HT%d" % i, [128, 4, 512], BF16) for i in range(2)]; b_HT = [[Buf() for _ in range(4)] for _ in range(2)]
    sgt = [S.sb("sgt%d" % i, [128, 512], F32) for i in range(2)]; b_sg = [Buf() for _ in range(2)]
    wgs = [S.sb("wgs%d" % i, [128, 8, FH], BF16) for i in range(2)]; b_wg = [Buf() for _ in range(2)]
    wus = [S.sb("wus%d" % i, [128, 8, FH], BF16) for i in range(2)]; b_wu = [Buf() for _ in range(2)]
    wds = [S.sb("wds%d" % i, [128, 4, D], BF16) for i in range(2)]; b_wd = [Buf() for _ in range(2)]
    xt = [S.sb("xt%d" % i, [128, D], F32) for i in range(2)]; b_xt = [Buf() for _ in range(2)]
    xs = [S.sb("xs%d" % i, [128, D], F32) for i in range(2)]; b_xs = [Buf() for _ in range(2)]
    hTf = [S.sb("hTf%d" % i, [128, 8, 128], F32) for i in range(2)]; b_hTf = [Buf() for _ in range(2)]
    sm = [S.sb("sm%d" % i, [128, 96], F32) for i in range(2)]; b_sm = [Buf() for _ in range(2)]
    PS = [S.ps("psb%d" % i, [128, 512], F32) for i in range(8)]; b_ps = [Buf() for _ in range(8)]

    S.dma("sync", ident[:], id_d, w=[b_id])
    S.dma("sync", wr[:], wr_d, w=[b_wr])
    S.dma("sync", br[:], br_d, w=[b_br])
    S.dma("sync", mc[:], mc_d, w=[b_mc])
    S.dma("sync", gtb[:], gt_d, w=[b_gt])
    if final:
        S.dma("sync", fgb[:], fg_d, w=[b_fg])
    for v in range(2):
        S.op("vector", lambda e, v=v: e.scalar_tensor_tensor(gsc[:, v, :], mc[:, 1 + 2 * v, :], 1.0, mc[:, 0, :],
                                                              ALU.add, ALU.mult), r=[b_mc], w=[b_gsc])

    wstate = dict(next=0)
    n_items_total = len(sts) * nexp

    def load_weights(k):
        e = k % nexp
        par = k % 2
        S.dma("gpsimd", wgs[par][:], wg_d[e].rearrange("(c p) n -> p c n", p=128), w=[b_wg[par]])
        S.dma("gpsimd", wus[par][:], wu_d[e].rearrange("(c p) n -> p c n", p=128), w=[b_wu[par]])
        S.dma("gpsimd", wds[par][:], wd_d[e].rearrange("(c p) n -> p c n", p=128), w=[b_wd[par]])

    load_weights(0)
    load_weights(1)
    wk = 2

    tcount = 0
    out_ops = []
    for si, st in enumerate(sts):
        ntl = len(st)
        loff = []
        o = 0
        for t in st:
            loff.append(o)
            o += tiles[t][1]
        stok = o
        for li, t in enumerate(st):
            off, P = tiles[t]
            par = tcount % 2
            tcount += 1
            v = 1 if off >= NLAT and ntok > NLAT else 0
            S.dma("sync", xt[par][0:P, :], xin[off:off + P, :], w=[b_xt[par]])
            ss = sm[par][0:P, 0:1]; rstd = sm[par][0:P, 1:2]
            S.op("scalar", lambda e, par=par, P=P, ss=ss: e.activation(xs[par][0:P, :], xt[par][0:P, :], AF.Square, accum_out=ss),
                 r=[b_xt[par]], w=[b_xs[par], b_sm[par]])
            S.op("vector", lambda e, ss=ss, rstd=rstd: e.tensor_scalar(rstd, ss, 1.0 / D, 1e-6, ALU.mult, ALU.add),
                 r=[b_sm[par]], w=[b_sm[par]])
            S.op("scalar", lambda e, rstd=rstd: e.activation(rstd, rstd, AF.Sqrt), r=[b_sm[par]], w=[b_sm[par]])
            S.op("vector", lambda e, rstd=rstd: e.reciprocal(rstd, rstd), r=[b_sm[par]], w=[b_sm[par]])
            S.op("vector", lambda e, par=par, P=P, rstd=rstd: e.tensor_scalar(xs[par][0:P, :], xt[par][0:P, :], rstd, None, ALU.mult),
                 r=[b_xt[par], b_sm[par]], w=[b_xs[par]])
            for c in range(8):
                pb = c // 4
                S.op("tensor", lambda e, par=par, P=P, c=c, pb=pb: e.transpose(PS[pb][:, (c % 4) * 128:(c % 4) * 128 + P],
                                                                                xs[par][0:P, c * 128:(c + 1) * 128], ident[0:P, 0:P]),
                     r=[b_xs[par], b_id], w=[b_ps[pb]])
            for c in range(8):
                pb = c // 4
                S.op("scalar", lambda e, par=par, P=P, c=c, pb=pb, v=v: e.activation(
                    hTf[par][:, c, 0:P], PS[pb][:, (c % 4) * 128:(c % 4) * 128 + P], AF.Identity,
                    bias=mc[:, 2 + 2 * v, c:c + 1], scale=gsc[:, v, c:c + 1]),
                    r=[b_ps[pb], b_mc, b_gsc], w=[b_hTf[par]])
            S.op("vector", lambda e, par=par, P=P, lo=loff[li]: e.tensor_copy(hTb[:, :, lo:lo + P], hTf[par][:, :, 0:P]),
                 r=[b_hTf[par]], w=[b_hTb[li]])
            for c in range(8):
                S.op("tensor", lambda e, par=par, P=P, c=c: e.matmul(PS[2][0:P, 0:36], hTf[par][:, c, 0:P], wr[:, c, :],
                                                                     start=(c == 0), stop=(c == 7)),
                     r=[b_hTf[par], b_wr], w=[b_ps[2]])
            m = sm[par]
            lg = m[0:P, 8:44]; gl = m[0:P, 8:12]
            gmax = m[0:P, 2:3]; ngmax = m[0:P, 3:4]; gsum = m[0:P, 4:5]; pg = m[0:P, 5:6]
            goh = m[0:P, 44:48]; sel = m[0:P, 48:56]; m8 = m[0:P, 56:64]; gex = m[0:P, 64:68]
            t1 = m[0:P, 68:76]; t2 = m[0:P, 76:84]
            dd = m[0:P, 84:85]; e2 = m[0:P, 85:86]; w1 = m[0:P, 86:87]; w2 = m[0:P, 87:88]
            bsm = [b_sm[par]]

            def V(fn, r=(), w=()):
                S.op("vector", fn, r=list(r) + bsm, w=list(w) + bsm)

            V(lambda e, lg=lg, P=P: e.tensor_tensor(lg, PS[2][0:P, 0:36], br[0:P, :], ALU.add), r=[b_ps[2], b_br])
            V(lambda e, gmax=gmax, gl=gl: e.reduce_max(gmax, gl, AX.X))
            V(lambda e, ngmax=ngmax, gmax=gmax: e.tensor_scalar(ngmax, gmax, -1.0, None, ALU.mult))
            S.op("scalar", lambda e, gex=gex, gl=gl, ngmax=ngmax, gsum=gsum: e.activation(gex, gl, AF.Exp, bias=ngmax, scale=1.0, accum_out=gsum),
                 r=bsm, w=bsm)
            V(lambda e, pg=pg, gsum=gsum: e.reciprocal(pg, gsum))
            V(lambda e, goh=goh, gl=gl, gmax=gmax: e.tensor_scalar(goh, gl, gmax, None, ALU.is_ge))
            V(lambda e, sel=sel, m=m, P=P, goh=goh: e.tensor_scalar(sel, m[0:P, 12:20], goh[:, 0:1], None, ALU.mult))
            for g in range(1, 4):
                V(lambda e, sel=sel, m=m, P=P, goh=goh, g=g: e.scalar_tensor_tensor(sel, m[0:P, 12 + 8 * g:20 + 8 * g], goh[:, g:g + 1], sel,
                                                                                     ALU.mult, ALU.add))
            V(lambda e, m8=m8, sel=sel: e.max(m8, sel))
            V(lambda e, dd=dd, m8=m8: e.tensor_tensor(dd, m8[:, 1:2], m8[:, 0:1], ALU.subtract))
            S.op("scalar", lambda e, e2=e2, dd=dd: e.activation(e2, dd, AF.Exp), r=bsm, w=bsm)
            V(lambda e, w1=w1, e2=e2: e.tensor_scalar(w1, e2, 1.0, None, ALU.add))
            V(lambda e, w1=w1: e.reciprocal(w1, w1))
            V(lambda e, w2=w2, e2=e2, w1=w1: e.tensor_tensor(w2, e2, w1, ALU.mult))
            V(lambda e, w1=w1, pg=pg: e.tensor_tensor(w1, w1, pg, ALU.mult))
            V(lambda e, w2=w2, pg=pg: e.tensor_tensor(w2, w2, pg, ALU.mult))
            V(lambda e, t1=t1, sel=sel, m8=m8, w1=w1: e.tensor_scalar(t1, sel, m8[:, 0:1], w1, ALU.is_equal, ALU.mult))
            V(lambda e, t2=t2, sel=sel, m8=m8, w2=w2: e.tensor_scalar(t2, sel, m8[:, 1:2], w2, ALU.is_equal, ALU.mult))
            V(lambda e, t1=t1, t2=t2: e.tensor_tensor(t1, t1, t2, ALU.add))
            for g in range(4):
                V(lambda e, li=li, P=P, g=g, t1=t1, goh=goh: e.tensor_scalar(coef[0:P, li, 8 * g:8 * g + 8], t1, goh[:, g:g + 1], None, ALU.mult),
                  w=[b_coef[li]])
        chunks = []
        li = 0
        while li < ntl:
            lj = li
            n = 0
            while lj < ntl and n + tiles[st[lj]][1] <= 512:
                n += tiles[st[lj]][1]
                lj += 1
            chunks.append((li, lj, loff[li], n))
            li = lj
        items = [(e, q) for e in range(nexp) for q in range(len(chunks))]

        def GU(idx):
            e, q = items[idx]
            l0, l1, to, n = chunks[q]
            kk = si * nexp + e
            par = kk % 2
            hp = idx % 2
            for j in range(4):
                pg_ = PS[(2 * j) % 4]; pu_ = PS[(2 * j) % 4 + 1]
                bg_ = b_ps[(2 * j) % 4]; bu_ = b_ps[(2 * j) % 4 + 1]
                for c in range(8):
                    S.op("tensor", lambda e_, par=par, c=c, j=j, pg_=pg_, to=to, n=n: e_.matmul(
                        pg_[:, 0:n], wgs[par][:, c, j * 128:(j + 1) * 128], hTb[:, c, to:to + n], start=(c == 0), stop=(c == 7)),
                        r=[b_wg[par]] + b_hTb[l0:l1], w=[bg_])
                for c in range(8):
                    S.op("tensor", lambda e_, par=par, c=c, j=j, pu_=pu_, to=to, n=n: e_.matmul(
                        pu_[:, 0:n], wus[par][:, c, j * 128:(j + 1) * 128], hTb[:, c, to:to + n], start=(c == 0), stop=(c == 7)),
                        r=[b_wu[par]] + b_hTb[l0:l1], w=[bu_])
                sp = j % 2
                S.op("scalar", lambda e_, sp=sp, pg_=pg_, n=n: e_.activation(sgt[sp][:, 0:n], pg_[:, 0:n], AF.Silu),
                     r=[bg_], w=[b_sg[sp]])
                S.op("vector", lambda e_, sp=sp, pu_=pu_, n=n, hp=hp, j=j: e_.tensor_tensor(HT[hp][:, j, 0:n], sgt[sp][:, 0:n], pu_[:, 0:n], ALU.mult),
                     r=[b_sg[sp], bu_], w=[b_HT[hp][j]])

        dstate = dict(k=0)

        def DN(idx):
            e, q = items[idx]
            l0, l1, to, n = chunks[q]
            kk = si * nexp + e
            par = kk % 2
            hp = idx % 2
            for li in range(l0, l1):
                P = tiles[st[li]][1]
                lo = loff[li] - to
                for h in range(2):
                    pb = 4 + dstate["k"] % 4
                    dstate["k"] += 1
                    for j in range(4):
                        S.op("tensor", lambda e_, pb=pb, P=P, hp=hp, j=j, lo=lo, par=par, h=h: e_.matmul(
                            PS[pb][0:P, :], HT[hp][:, j, lo:lo + P], wds[par][:, j, h * 512:(h + 1) * 512], start=(j == 0), stop=(j == 3)),
                            r=[b_HT[hp][j], b_wd[par]], w=[b_ps[pb]])
                    if e == 0:
                        S.op("vector", lambda e_, pb=pb, P=P, li=li, h=h, e=e: e_.tensor_scalar(
                            acc[0:P, li, h * 512:(h + 1) * 512], PS[pb][0:P, :], coef[0:P, li, e:e + 1], None, ALU.mult),
                            r=[b_ps[pb], b_coef[li]], w=[b_acc[li]])
                    else:
                        S.op("vector", lambda e_, pb=pb, P=P, li=li, h=h, e=e: e_.scalar_tensor_tensor(
                            acc[0:P, li, h * 512:(h + 1) * 512], PS[pb][0:P, :], coef[0:P, li, e:e + 1],
                            acc[0:P, li, h * 512:(h + 1) * 512], ALU.mult, ALU.add),
                            r=[b_ps[pb], b_coef[li]], w=[b_acc[li]])

        nonlocal_wk = [wk]
        for idx in range(len(items)):
            GU(idx)
            if idx > 0:
                DN(idx - 1)
                e_prev, q_prev = items[idx - 1]
                if q_prev == len(chunks) - 1 and nonlocal_wk[0] < n_items_total:
                    load_weights(nonlocal_wk[0])
                    nonlocal_wk[0] += 1
        DN(len(items) - 1)
        if nonlocal_wk[0] < n_items_total:
            load_weights(nonlocal_wk[0])
            nonlocal_wk[0] += 1
        wk = nonlocal_wk[0]
        for li, t in enumerate(st):
            off, P = tiles[t]
            par = tcount % 2
            tcount += 1
            v = 1 if off >= NLAT and ntok > NLAT else 0
            S.dma("sync", xt[par][0:P, :], xin[off:off + P, :], w=[b_xt[par]])
            S.op("vector", lambda e, P=P, li=li, v=v: e.tensor_tensor(acc[0:P, li, :], acc[0:P, li, :], gtb[0:P, v, :], ALU.mult),
                 r=[b_gt], w=[b_acc[li]])
            S.op("vector", lambda e, P=P, li=li, par=par: e.tensor_tensor(acc[0:P, li, :], acc[0:P, li, :], xt[par][0:P, :], ALU.add),
                 r=[b_xt[par]], w=[b_acc[li]])
            if final:
                ss = sm[par][0:P, 0:1]; rstd = sm[par][0:P, 1:2]
                S.op("scalar", lambda e, par=par, P=P, ss=ss, li=li: e.activation(xs[par][0:P, :], acc[0:P, li, :], AF.Square, accum_out=ss),
                     r=[b_acc[li]], w=[b_xs[par], b_sm[par]])
                S.op("vector", lambda e, ss=ss, rstd=rstd: e.tensor_scalar(rstd, ss, 1.0 / D, 1e-6, ALU.mult, ALU.add),
                     r=[b_sm[par]], w=[b_sm[par]])
                S.op("scalar", lambda e, rstd=rstd: e.activation(rstd, rstd, AF.Sqrt), r=[b_sm[par]], w=[b_sm[par]])
                S.op("vector", lambda e, rstd=rstd: e.reciprocal(rstd, rstd), r=[b_sm[par]], w=[b_sm[par]])
                S.op("vector", lambda e, P=P, li=li, rstd=rstd: e.scalar_tensor_tensor(acc[0:P, li, :], acc[0:P, li, :], rstd, fgb[0:P, :],
                                                                                      ALU.mult, ALU.mult),
                     r=[b_sm[par], b_fg], w=[b_acc[li]])
            out_ops.append(S.dma("sync", xout[off:off + P, :], acc[0:P, li, :], r=[b_acc[li]], w=[]))
    if dbg:
        d_sm = dram_out(nc, "d_sm", [128, 96]); d_coef = dram_out(nc, "d_coef", [128, 32])
        d_hTf = dram_out(nc, "d_hTf", [128, 8, 128]); d_hTb = dram_out(nc, "d_hTb", [128, 8, MAXTOK], BF16)
        d_HT = dram_out(nc, "d_HT", [128, 4, 512], BF16); d_wg = dram_out(nc, "d_wg", [128, 8, FH], BF16)
        out_ops.append(S.dma("sync", d_sm, sm[0][:], r=[b_sm[0]]))
        out_ops.append(S.dma("sync", d_coef, coef[:, 0, :], r=[b_coef[0]]))
        out_ops.append(S.dma("sync", d_hTf, hTf[0][:], r=[b_hTf[0]]))
        out_ops.append(S.dma("sync", d_hTb, hTb[:], r=b_hTb))
        out_ops.append(S.dma("sync", d_HT, HT[0][:], r=b_HT[0]))
        out_ops.append(S.dma("sync", d_wg, wgs[0][:], r=[b_wg[0]]))
    S.emit(out_ops)
    return nc


_MOE_NC = {}


def moe_maps(xs_per_core, i, mods, inp, nexp=NEXP):
    wr = np.concatenate([inp["moe_w_grp"][i], inp["moe_w_exp"][i]], axis=1)
    wr = np.ascontiguousarray(wr.reshape(8, 128, 36).transpose(1, 0, 2))
    br = bcast(np.concatenate([inp["moe_b_grp"][i], inp["moe_b_exp"][i]]))
    ident = np.eye(128, dtype=np.float32)
    fgb = bcast(inp["final_g"])
    wg = np.ascontiguousarray(inp["moe_w_gate"][i][:nexp])
    wu = np.ascontiguousarray(inp["moe_w_up"][i][:nexp])
    wd = np.ascontiguousarray(inp["moe_w_down"][i][:nexp])
    maps = []
    for k in range(NCORES):
        b = k // 4
        ml = mods[i, b]
        mcx = mods[i, 2]
        mc = np.stack([cols(inp["norm2_g"][i]), cols(ml[4 * D:5 * D]), cols(ml[3 * D:4 * D]),
                       cols(mcx[4 * D:5 * D]), cols(mcx[3 * D:4 * D]), np.zeros((128, 8), np.float32)], axis=1)
        gtb = np.stack([bcast(ml[5 * D:6 * D]), bcast(mcx[5 * D:6 * D])], axis=1)
        maps.append(dict(xin=np.ascontiguousarray(xs_per_core[k]), wr=wr, br=br, mcols=np.ascontiguousarray(mc),
                         gtb=np.ascontiguousarray(gtb), fgb=fgb, ident=ident, wg=wg, wu=wu, wd=wd))
    return maps


def run_moe(xs_per_core, i, mods, inp, final=False, ntok=NTOK, nexp=NEXP, trace=False, dbg=False):
    key = (final, ntok, nexp, dbg)
    if key not in _MOE_NC:
        _MOE_NC[key] = build_moe(final=final, ntok=ntok, nexp=nexp, dbg=dbg)
    maps = moe_maps(xs_per_core, i, mods, inp, nexp)
    res = run_bass_kernel_spmd(_MOE_NC[key], maps, core_ids=list(range(NCORES)), trace=trace)
    if trace:
        print("moe exec_time_ns", res.exec_time_ns)
    if dbg:
        return res.results
    return [r["xout"] for r in res.results]


GW = 2048


def build_gmlp(nch=33, ctx_last=True):
    nc = new_nc()
    ntok = nch * 128
    xin = dram_in(nc, "xin", [ntok, D])
    mc_d = dram_in(nc, "mcols", [128, 5, 8])
    gt_d = dram_in(nc, "gtb", [128, 2, D])
    bo_d = dram_in(nc, "boutb", [128, D])
    win_d = dram_in(nc, "w_in", [D, 2 * GW])
    bu_d = dram_in(nc, "b_ucol", [128, 16])
    bv_d = dram_in(nc, "b_vrow", [1, GW])
    lg_d = dram_in(nc, "lngb", [128, GW])
    lb_d = dram_in(nc, "lnbb", [128, GW])
    ws_d = dram_in(nc, "wsT", [128, 16, 128])
    bs_d = dram_in(nc, "bsrow", [1, 16, 128])
    wo_d = dram_in(nc, "w_out", [GW, D])
    id_d = dram_in(nc, "ident", [128, 128])
    xout = dram_out(nc, "xout", [ntok, D])
    S = Sched(nc)
    GT = 256
    ident = S.sb("ident", [128, 128], F32); b_id = Buf()
    mc = S.sb("mc", [128, 5, 8], F32); b_mc = Buf()
    gsc = S.sb("gsc", [128, 2, 8], F32); b_gsc = Buf()
    gtb = S.sb("gtb", [128, 2, D], F32); b_gt = Buf()
    bob = S.sb("bob", [128, D], F32); b_bo = Buf()
    win = S.sb("win", [128, 8, 2 * GW], BF16); b_win = Buf()
    bu = S.sb("bu", [128, 16], F32); b_bu = Buf()
    bvr = S.sb("bvr", [1, GW], BF16); b_bv = Buf()
    lgb = S.sb("lgb", [128, GW], F32); b_lg = Buf()
    lbb = S.sb("lbb", [128, GW], F32); b_lb = Buf()
    wsT = S.sb("wsT", [128, 16, 128], BF16); b_ws = Buf()
    bsr = S.sb("bsr", [1, 16, 128], BF16); b_bs = Buf()
    wo = S.sb("wo", [128, 16, D], BF16); b_wo = Buf()
    ones = S.sb("ones", [1, 128], BF16); b_ones = Buf()
    hT = [S.sb("hT%d" % i, [128, 8, GT], BF16) for i in range(2)]; b_hT = [Buf() for _ in range(2)]
    uT = S.sb("uT", [128, 16, GT], BF16); b_uT = [Buf() for _ in range(16)]
    vv = [S.sb("vv%d" % i, [128, GW], F32) for i in range(2)]; b_vv = [Buf() for _ in range(2)]
    vn = [S.sb("vn%d" % i, [128, GW], BF16) for i in range(2)]; b_vn = [Buf() for _ in range(2)]
    prod = S.sb("prod", [128, 16, GT], BF16); b_prod = [[Buf() for _ in range(4)] for _ in range(2)]
    xt = [S.sb("xt%d" % i, [128, D], F32) for i in range(2)]; b_xt = [Buf() for _ in range(2)]
    _xs = S.sb("xs0", [128, D], F32); _bxs = Buf()
    xs = [_xs, _xs]; b_xs = [_bxs, _bxs]
    ot = [S.sb("ot%d" % i, [128, D], F32) for i in range(2)]; b_ot = [Buf() for _ in range(2)]
    sm = [S.sb("sm%d" % i, [128, 8], F32) for i in range(2)]; b_sm = [Buf() for _ in range(2)]
    PS = [S.ps("psb%d" % i, [128, 512], F32) for i in range(8)]; b_ps = [Buf() for _ in range(8)]

    S.dma("sync", ident[:], id_d, w=[b_id])
    S.dma("sync", mc[:], mc_d, w=[b_mc])
    S.dma("sync", gtb[:], gt_d, w=[b_gt])
    S.dma("sync", bob[:], bo_d, w=[b_bo])
    S.dma("sync", bu[:], bu_d, w=[b_bu])
    S.dma("sync", lgb[:], lg_d, w=[b_lg])
    S.dma("sync", lbb[:], lb_d, w=[b_lb])
    S.dma("gpsimd", bvr[:], bv_d, w=[b_bv])
    S.dma("gpsimd", bsr[:], bs_d, w=[b_bs])
    S.dma("gpsimd", wsT[:], ws_d, w=[b_ws])
    winv = win_d.rearrange("(c p) n -> p c n", p=128)
    for n in range(4):
        S.dma("gpsimd", win[:, :, n * 1024:(n + 1) * 1024], winv[:, :, n * 1024:(n + 1) * 1024], w=[b_win])
    S.dma("gpsimd", wo[:], wo_d.rearrange("(c p) n -> p c n", p=128), w=[b_wo])
    S.op("vector", lambda e: e.memset(ones[:], 1.0), w=[b_ones])
    for v in range(2):
        S.op("vector", lambda e, v=v: e.scalar_tensor_tensor(gsc[:, v, :], mc[:, 1 + 2 * v, :], 1.0, mc[:, 0, :],
                                                              ALU.add, ALU.mult), r=[b_mc], w=[b_gsc])
    out_ops = []
    groups = []
    ci = 0
    while ci < nch:
        g2 = min(ci + 2, nch)
        groups.append(list(range(ci, g2)))
        ci = g2
    tcount = 0
    vcount = 0
    for gi, grp in enumerate(groups):
        ng = len(grp)
        gt_ = ng * 128
        hp = gi % 2
        xpar = {}
        for li, ch in enumerate(grp):
            par = tcount % 2
            tcount += 1
            xpar[ch] = par
            v = 1 if (ctx_last and ch == nch - 1) else 0
            off = ch * 128
            S.dma("sync", xt[par][:], xin[off:off + 128, :], w=[b_xt[par]])
            ss = sm[par][:, 0:1]; rstd = sm[par][:, 1:2]
            S.op("scalar", lambda e, par=par, ss=ss: e.activation(xs[par][:], xt[par][:], AF.Square, accum_out=ss),
                 r=[b_xt[par]], w=[b_xs[par], b_sm[par]])
            S.op("vector", lambda e, ss=ss, rstd=rstd: e.tensor_scalar(rstd, ss, 1.0 / D, 1e-6, ALU.mult, ALU.add),
                 r=[b_sm[par]], w=[b_sm[par]])
            S.op("scalar", lambda e, rstd=rstd: e.activation(rstd, rstd, AF.Sqrt), r=[b_sm[par]], w=[b_sm[par]])
            S.op("vector", lambda e, rstd=rstd: e.reciprocal(rstd, rstd), r=[b_sm[par]], w=[b_sm[par]])
            S.op("vector", lambda e, par=par, rstd=rstd: e.tensor_scalar(xs[par][:], xt[par][:], rstd, None, ALU.mult),
                 r=[b_xt[par], b_sm[par]], w=[b_xs[par]])
            for c in range(8):
                pb = c // 4
                S.op("tensor", lambda e, par=par, c=c, pb=pb: e.transpose(PS[pb][:, (c % 4) * 128:(c % 4) * 128 + 128],
                                                                          xs[par][:, c * 128:(c + 1) * 128], ident[:]),
                     r=[b_xs[par], b_id], w=[b_ps[pb]])
            for c in range(8):
                pb = c // 4
                S.op("scalar", lambda e, hp=hp, c=c, pb=pb, v=v, li=li: e.activation(
                    hT[hp][:, c, li * 128:(li + 1) * 128], PS[pb][:, (c % 4) * 128:(c % 4) * 128 + 128], AF.Identity,
                    bias=mc[:, 2 + 2 * v, c:c + 1], scale=gsc[:, v, c:c + 1]),
                    r=[b_ps[pb], b_mc, b_gsc], w=[b_hT[hp]])
        for m in range(16):
            pb = 2 + m % 2
            for c in range(8):
                S.op("tensor", lambda e, pb=pb, c=c, m=m, hp=hp, gt_=gt_: e.matmul(
                    PS[pb][:, 0:gt_], win[:, c, m * 128:(m + 1) * 128], hT[hp][:, c, 0:gt_], start=(c == 0), stop=(c == 7)),
                    r=[b_win, b_hT[hp]], w=[b_ps[pb]])
            S.op("scalar", lambda e, pb=pb, m=m, gt_=gt_: e.activation(uT[:, m, 0:gt_], PS[pb][:, 0:gt_], AF.Gelu,
                                                                       bias=bu[:, m:m + 1], scale=1.0),
                 r=[b_ps[pb], b_bu], w=[b_uT[m]])
        vps = {}
        for li, ch in enumerate(grp):
            vp = vcount % 2
            vcount += 1
            vps[ch] = vp
            v = 1 if (ctx_last and ch == nch - 1) else 0
            par = xpar[ch]
            off = ch * 128
            tk = slice(li * 128, (li + 1) * 128)
            for n in range(4):
                pb = 4 + n % 2
                for c in range(8):
                    S.op("tensor", lambda e, pb=pb, c=c, n=n, hp=hp, tk=tk: e.matmul(
                        PS[pb][:, :], hT[hp][:, c, tk], win[:, c, GW + n * 512:GW + (n + 1) * 512], start=(c == 0), stop=False),
                        r=[b_win, b_hT[hp]], w=[b_ps[pb]])
                S.op("tensor", lambda e, pb=pb, n=n: e.matmul(PS[pb][:, :], ones[0:1, :], bvr[0:1, n * 512:(n + 1) * 512],
                                                              start=False, stop=True),
                     r=[b_ones, b_bv], w=[b_ps[pb]])
                S.op("scalar", lambda e, pb=pb, n=n, vp=vp: e.activation(vv[vp][:, n * 512:(n + 1) * 512], PS[pb][:, :], AF.Gelu),
                     r=[b_ps[pb]], w=[b_vv[vp]])
            s1 = sm[par][:, 2:3]; nm = sm[par][:, 3:4]; sq = sm[par][:, 4:5]; r2 = sm[par][:, 5:6]
            S.op("vector", lambda e, s1=s1, vp=vp: e.reduce_sum(s1, vv[vp][:], AX.X), r=[b_vv[vp]], w=[b_sm[par]])
            S.op("vector", lambda e, s1=s1, nm=nm: e.tensor_scalar(nm, s1, -1.0 / GW, None, ALU.mult), r=[b_sm[par]], w=[b_sm[par]])
            S.op("scalar", lambda e, vp=vp, nm=nm, sq=sq: e.activation(vn[vp][:], vv[vp][:], AF.Square, bias=nm, scale=1.0, accum_out=sq),
                 r=[b_vv[vp], b_sm[par]], w=[b_vn[vp], b_sm[par]])
            S.op("vector", lambda e, sq=sq, r2=r2: e.tensor_scalar(r2, sq, 1.0 / GW, 1e-5, ALU.mult, ALU.add), r=[b_sm[par]], w=[b_sm[par]])
            S.op("scalar", lambda e, r2=r2: e.activation(r2, r2, AF.Sqrt), r=[b_sm[par]], w=[b_sm[par]])
            S.op("vector", lambda e, r2=r2: e.reciprocal(r2, r2), r=[b_sm[par]], w=[b_sm[par]])
            S.op("vector", lambda e, vp=vp, nm=nm, r2=r2: e.tensor_scalar(vv[vp][:], vv[vp][:], nm, r2, ALU.add, ALU.mult),
                 r=[b_sm[par]], w=[b_vv[vp]])
            S.op("vector", lambda e, vp=vp: e.tensor_tensor(vv[vp][:], vv[vp][:], lgb[:], ALU.mult), r=[b_lg], w=[b_vv[vp]])
            S.op("gpsimd", lambda e, vp=vp: e.tensor_tensor(vn[vp][:], vv[vp][:], lbb[:], ALU.add), r=[b_vv[vp], b_lb], w=[b_vn[vp]])
        for li, ch in enumerate(grp):
            vp = vps[ch]
            v = 1 if (ctx_last and ch == nch - 1) else 0
            par = xpar[ch]
            off = ch * 128
            tk = slice(li * 128, (li + 1) * 128)
            for g4 in range(4):
                pb = 6 + g4 % 2
                for gg in range(4):
                    g = g4 * 4 + gg
                    S.op("tensor", lambda e, pb=pb, gg=gg, g=g, vp=vp: e.matmul(
                        PS[pb][:, gg * 128:(gg + 1) * 128], vn[vp][:, g * 128:(g + 1) * 128], wsT[:, g, :], start=True, stop=False),
                        r=[b_vn[vp], b_ws], w=[b_ps[pb]])
                    S.op("tensor", lambda e, pb=pb, gg=gg, g=g: e.matmul(
                        PS[pb][:, gg * 128:(gg + 1) * 128], ones[0:1, :], bsr[0:1, g, :], start=False, stop=True),
                        r=[b_ones, b_bs], w=[b_ps[pb]])
                S.op("vector", lambda e, pb=pb, g4=g4, tk=tk: e.tensor_tensor(
                    prod[:, g4 * 4:(g4 + 1) * 4, tk], PS[pb][:, :].rearrange("p (g t) -> p g t", g=4), uT[:, g4 * 4:(g4 + 1) * 4, tk], ALU.mult),
                    r=[b_ps[pb]] + b_uT[g4 * 4:(g4 + 1) * 4], w=[b_prod[li][g4]])
            op_ = tcount % 2
            for n in range(2):
                pb = n
                for k in range(16):
                    S.op("tensor", lambda e, pb=pb, k=k, n=n, tk=tk: e.matmul(
                        PS[pb][:, :], prod[:, k, tk], wo[:, k, n * 512:(n + 1) * 512], start=(k == 0), stop=(k == 15)),
                        r=b_prod[li] + [b_wo], w=[b_ps[pb]])
                sl = slice(n * 512, (n + 1) * 512)
                S.op("vector", lambda e, pb=pb, par=par, sl=sl: e.tensor_tensor(ot[par][:, sl], PS[pb][:, :], bob[:, sl], ALU.add),
                     r=[b_ps[pb], b_bo], w=[b_ot[par]])
            S.op("gpsimd", lambda e, par=par, v=v: e.tensor_tensor(ot[par][:], ot[par][:], gtb[:, v, :], ALU.mult),
                 r=[b_gt], w=[b_ot[par]])
            S.op("gpsimd", lambda e, par=par: e.tensor_tensor(ot[par][:], ot[par][:], xt[par][:], ALU.add),
                 r=[b_xt[par]], w=[b_ot[par]])
            out_ops.append(S.dma("sync", xout[off:off + 128, :], ot[par][:], r=[b_ot[par]]))
    S.emit(out_ops)
    return nc


_GMLP_NC = {}


def run_gmlp(xs_per_core, i, mods, inp, nch=33, trace=False):
    j = i // 2
    key = nch
    if key not in _GMLP_NC:
        _GMLP_NC[key] = build_gmlp(nch=nch)
    b_in = inp["ga_b_in"][j]
    wsT = np.ascontiguousarray(inp["ga_w_s"][j].transpose(2, 0, 1))
    base = dict(boutb=bcast(inp["ga_b_out"][j]), w_in=np.ascontiguousarray(inp["ga_w_in"][j]),
                b_ucol=np.ascontiguousarray(b_in[:GW].reshape(16, 128).T), b_vrow=np.ascontiguousarray(b_in[GW:].reshape(1, GW)),
                lngb=bcast(inp["ga_ln_g"][j]), lnbb=bcast(inp["ga_ln_b"][j]), wsT=wsT,
                bsrow=np.ascontiguousarray(inp["ga_b_s"][j].reshape(1, 16, 128)), w_out=np.ascontiguousarray(inp["ga_w_out"][j]),
                ident=np.eye(128, dtype=np.float32))
    maps = []
    for k in range(NCORES):
        b = k // 4
        ml = mods[i, b]; mcx = mods[i, 2]
        mc = np.stack([cols(inp["norm1_g"][i]), cols(ml[1 * D:2 * D]), cols(ml[0:D]), cols(mcx[1 * D:2 * D]), cols(mcx[0:D])], axis=1)
        gtb = np.stack([bcast(ml[2 * D:3 * D]), bcast(mcx[2 * D:3 * D])], axis=1)
        m = dict(base)
        m.update(xin=np.ascontiguousarray(xs_per_core[k]), mcols=np.ascontiguousarray(mc), gtb=np.ascontiguousarray(gtb))
        maps.append(m)
    res = run_bass_kernel_spmd(_GMLP_NC[key], maps, core_ids=list(range(NCORES)), trace=trace)
    if trace:
        print("gmlp exec_time_ns", res.exec_time_ns)
    return [r["xout"] for r in res.results]


PBLK = 256
NEG_E05 = -0.6065306597126334


def build_prep(nlat=NLAT, with_ctx=True, stop=None):
    nc = new_nc()
    nrow = nlat + 128 + (256 if with_ctx else 0)
    TP = nlat + (256 if with_ctx else 0)
    xin = dram_in(nc, "xin", [nrow, D])
    edge_d = dram_in(nc, "edge", [128, 2])
    mc_d = dram_in(nc, "mcols", [128, 5, 8])
    mu_d = dram_in(nc, "mu", [128, 6, 8])
    vc_d = dram_in(nc, "vcols", [128, 8, 8])
    wr_d = dram_in(nc, "wr", [D, D]); wk_d = dram_in(nc, "wk", [D, D]); wv_d = dram_in(nc, "wv", [D, D])
    w1_d = dram_in(nc, "w1", [2, D, 64]); w2_d = dram_in(nc, "w2", [2, 64, D])
    a1_d = dram_in(nc, "a1", [2, D, 64]); a2_d = dram_in(nc, "a2", [2, 64, D])
    g1_d = dram_in(nc, "g1", [D, 160]); g2_d = dram_in(nc, "g2", [160, D])
    id_d = dram_in(nc, "ident", [128, 128]); blk_d = dram_in(nc, "blk", [128, 128]); cm_d = dram_in(nc, "cmask", [128, PBLK])
    ob = dram_out(nc, "ob", [9, 8, 128, TP], BF16)
    of = dram_out(nc, "of", [2, 8, 128, TP])
    pc = dram_out(nc, "pc", [2, 8, 128, TP // 64])
    S = Sched(nc)
    ident = S.sb("ident", [128, 128], F32); b_id = Buf()
    blk = S.sb("blk", [128, 128], BF16); b_blk = Buf()
    cmask = S.sb("cmask", [128, PBLK], F32); b_cm = Buf()
    edge = S.sb("edge", [128, 2], F32); b_edge = Buf()
    mc = S.sb("mc", [128, 5, 8], F32); b_mc = Buf()
    gsc = S.sb("gsc", [128, 2, 8], F32); b_gsc = Buf()
    mu = S.sb("mu", [128, 6, 8], F32); b_mu = Buf()
    vc = S.sb("vc", [128, 8, 8], F32); b_vc = Buf()
    epsc = S.sb("epsc", [128, 1], F32); b_eps = Buf()
    wr = S.sb("wr", [128, 8, D], BF16); wk = S.sb("wk", [128, 8, D], BF16); wv = S.sb("wv", [128, 8, D], BF16)
    b_wr = Buf(); b_wk = Buf(); b_wv = Buf()
    w1 = S.sb("w1", [128, 2, 8, 64], BF16); a1 = S.sb("a1", [128, 2, 8, 64], BF16); b_w1 = Buf(); b_a1 = Buf()
    w2 = S.sb("w2", [64, 2, D], BF16); a2 = S.sb("a2", [64, 2, D], BF16); b_w2 = Buf(); b_a2 = Buf()
    g1 = S.sb("g1", [128, 8, 160], BF16); b_g1 = Buf()
    g2a = S.sb("g2a", [128, D], BF16); g2b = S.sb("g2b", [32, D], BF16); b_g2 = Buf()
    HL = S.sb("HL", [128, 8, PBLK + 128], F32); b_HL = [Buf() for _ in range(8)]
    XX = S.sb("XX", [128, 8, PBLK], F32); b_XX = [Buf() for _ in range(8)]
    XM = [S.sb("XM%d" % j, [128, 8, PBLK], BF16) for j in range(6)]; b_XM = [[Buf() for _ in range(8)] for _ in range(6)]
    R32 = S.sb("R32", [128, 8, PBLK], F32); K32 = S.sb("K32", [128, 8, PBLK], F32); V32 = S.sb("V32", [128, 8, PBLK], F32)
    b_R = [Buf() for _ in range(8)]; b_K = [Buf() for _ in range(8)]; b_V = [Buf() for _ in range(8)]
    TW = [S.sb("TW%d" % z, [64, PBLK], BF16) for z in range(2)]; b_TW = [Buf() for _ in range(2)]
    AL = [S.sb("AL%d" % z, [64, PBLK], BF16) for z in range(2)]; b_AL = [Buf() for _ in range(2)]
    SGa = S.sb("SGa", [128, PBLK], BF16); SGb = S.sb("SGb", [32, PBLK], BF16); b_SG = Buf()
    xt = [S.sb("xt%d" % i, [128, D], F32) for i in range(2)]; b_xt = [Buf() for _ in range(2)]
    xs = S.sb("xs", [128, D], F32); b_xs = Buf()
    sm = [S.sb("sm%d" % i, [128, 4], F32) for i in range(2)]; b_sm = [Buf() for _ in range(2)]

    def tmp(name, dt=F32):
        return S.sb("t_" + name, [128, PBLK], dt), Buf()
    LW2 = [[tmp("LW%d_%d" % (z, i)) for z in range(2)] for i in range(2)]; AZ2 = [[tmp("AZ%d_%d" % (z, i)) for z in range(2)] for i in range(2)]
    FF = [tmp("FF%d" % z) for z in range(2)]; CE = [tmp("CE%d" % z) for z in range(2)]
    CC = [tmp("CC%d" % z) for z in range(2)]
    Ec2 = [[tmp("Ec%d_%d" % (z, i)) for z in range(2)] for i in range(2)]; Ee2 = [[tmp("Ee%d_%d" % (z, i)) for z in range(2)] for i in range(2)]
    En2 = [[tmp("En%d_%d" % (z, i)) for z in range(2)] for i in range(2)]
    KR = tmp("KR"); SQ = tmp("SQ", BF16); RN = tmp("RN"); KK2 = [tmp("KK_%d" % i) for i in range(2)]; T1 = tmp("T1")
    KM2 = [[tmp("KM%d_%d" % (z, i)) for z in range(2)] for i in range(2)]; BT = tmp("BT"); KS = tmp("KS"); PB = tmp("PB", BF16)
    GO = [tmp("GO%d" % i) for i in range(2)]; BV = [tmp("BV%d" % i) for i in range(2)]
    OB = [[tmp("OB%d_%d" % (k, i), BF16) for k in range(9)] for i in range(2)]
    PCt = [S.sb("PCt%d" % i, [128, 2, PBLK // 64], F32) for i in range(2)]; b_PC = [Buf() for _ in range(2)]
    PS = [S.ps("psb%d" % i, [128, 512], F32) for i in range(8)]
    _bb = [Buf() for _ in range(8)]
    b_ps = [[b, b] for b in _bb]

    for t_, d_, b_ in [(ident, id_d, b_id), (cmask, cm_d, b_cm), (edge, edge_d, b_edge), (mc, mc_d, b_mc), (mu, mu_d, b_mu), (vc, vc_d, b_vc)]:
        S.dma("sync", t_[:], d_, w=[b_])
    S.dma("gpsimd", blk[:], blk_d, w=[b_blk])
    for t_, d_, b_ in [(wr, wr_d, b_wr), (wk, wk_d, b_wk), (wv, wv_d, b_wv)]:
        S.dma("gpsimd", t_[:], d_.rearrange("(c p) n -> p c n", p=128), w=[b_])
    for z in range(2):
        S.dma("gpsimd", w1[:, z, :, :], w1_d[z].rearrange("(c p) n -> p c n", p=128), w=[b_w1])
        S.dma("gpsimd", a1[:, z, :, :], a1_d[z].rearrange("(c p) n -> p c n", p=128), w=[b_a1])
        S.dma("gpsimd", w2[:, z, :], w2_d[z], w=[b_w2])
        S.dma("gpsimd", a2[:, z, :], a2_d[z], w=[b_a2])
    S.dma("gpsimd", g1[:], g1_d.rearrange("(c p) n -> p c n", p=128), w=[b_g1])
    S.dma("gpsimd", g2a[:], g2_d[0:128, :], w=[b_g2])
    S.dma("gpsimd", g2b[:], g2_d[128:160, :], w=[b_g2])
    S.op("vector", lambda e: e.memset(epsc[:], 1e-12), w=[b_eps])
    for v in range(2):
        S.op("vector", lambda e, v=v: e.scalar_tensor_tensor(gsc[:, v, :], mc[:, 1 + 2 * v, :], 1.0, mc[:, 0, :],
                                                              ALU.add, ALU.mult), r=[b_mc], w=[b_gsc])
    out_ops = []
    def E_body(co, op_, tok0, isctx, LW, AZ, Ec, Ee, En, KM, KK):
        cs = slice(co * 128, (co + 1) * 128)
        for z in range(2):
            reg = PS[4][:, z * 256:z * 256 + PBLK]
            S.op("tensor", lambda e, reg=reg, z=z, cs=cs: e.matmul(reg, w2[0:64, z, cs], TW[z][0:64, :], start=True, stop=True),
                 r=[b_w2, b_TW[z]], w=[b_ps[4][z]])
            S.op("scalar", lambda e, reg=reg, z=z, co=co: e.activation(LW[z][0][:], reg, AF.Sigmoid, bias=vc[:, z, co:co + 1], scale=1.0),
                 r=[b_ps[4][z], b_vc], w=[LW[z][1]])
            S.op("vector", lambda e, z=z: e.tensor_scalar(LW[z][0][:], LW[z][0][:], NEG_E05, None, ALU.mult), r=[], w=[LW[z][1]])
            reg2 = PS[5][:, z * 256:z * 256 + PBLK]
            S.op("tensor", lambda e, reg2=reg2, z=z, cs=cs: e.matmul(reg2, a2[0:64, z, cs], AL[z][0:64, :], start=True, stop=True),
                 r=[b_a2, b_AL[z]], w=[b_ps[5][z]])
            S.op("scalar", lambda e, reg2=reg2, z=z, co=co: e.activation(AZ[z][0][:], reg2, AF.Sigmoid, bias=vc[:, 2 + z, co:co + 1], scale=1.0),
                 r=[b_ps[5][z], b_vc], w=[AZ[z][1]])
        regg = PS[6][:, 0:PBLK]
        S.op("tensor", lambda e, cs=cs: e.matmul(regg, g2a[:, cs], SGa[:], start=True, stop=False), r=[b_g2, b_SG], w=[b_ps[6][0]])
        S.op("tensor", lambda e, cs=cs: e.matmul(regg, g2b[0:32, cs], SGb[0:32, :], start=False, stop=True), r=[b_g2, b_SG], w=[b_ps[6][0]])
        S.op("scalar", lambda e, op_=op_: e.copy(GO[op_][0][:], regg), r=[b_ps[6][0]], w=[GO[op_][1]])
        out_ops.append(S.dma("sync", of[1, co, :, tok0:tok0 + PBLK], GO[op_][0][:], r=[GO[op_][1]]))
        if stop == 'E1':
            return
        for z in range(2):
            S.op("vector", lambda e, z=z: e.tensor_tensor_scan(FF[z][0][:], cmask[:], LW[z][0][:], 0.0, ALU.mult, ALU.add),
                 r=[b_cm, LW[z][1]], w=[FF[z][1]])
        S.op("vector", lambda e: e.tensor_tensor(CE[0][0][:], FF[0][0][:], LW[0][0][:], ALU.subtract), r=[FF[0][1], LW[0][1]], w=[CE[0][1]])
        f3 = FF[1][0][:].rearrange("p (a b) -> p a b", b=64)
        S.op("vector", lambda e, f3=f3: e.tensor_tensor(CE[1][0][:].rearrange("p (a b) -> p a b", b=64),
                                                        f3[:, :, 63:64].to_broadcast([128, PBLK // 64, 64]), f3, ALU.subtract),
             r=[FF[1][1]], w=[CE[1][1]])
        S.op("vector", lambda e: e.tensor_tensor(CC[1][0][:], CE[1][0][:], LW[1][0][:], ALU.add), r=[CE[1][1], LW[1][1]], w=[CC[1][1]])
        csrc = [FF[0], CC[1]]
        for z in range(2):
            S.op("scalar", lambda e, z=z: e.activation(Ec[z][0][:], csrc[z][0][:], AF.Exp), r=[csrc[z][1]], w=[Ec[z][1]])
            S.op("scalar", lambda e, z=z: e.activation(En[z][0][:], csrc[z][0][:], AF.Exp, scale=-1.0), r=[csrc[z][1]], w=[En[z][1]])
            S.op("scalar", lambda e, z=z: e.activation(Ee[z][0][:], CE[z][0][:], AF.Exp), r=[CE[z][1]], w=[Ee[z][1]])
        S.op("vector", lambda e, op_=op_: e.tensor_copy(PCt[op_][:, 0, :], Ec[0][0][:].rearrange("p (a b) -> p a b", b=64)[:, :, 63]),
             r=[Ec[0][1]], w=[b_PC[op_]])
        S.op("vector", lambda e, op_=op_: e.tensor_copy(PCt[op_][:, 1, :], Ec[1][0][:].rearrange("p (a b) -> p a b", b=64)[:, :, 0]),
             r=[Ec[1][1]], w=[b_PC[op_]])
        ch0 = tok0 // 64
        for z in range(2):
            out_ops.append(S.dma("sync", pc[z, co, :, ch0:ch0 + PBLK // 64], PCt[op_][:, z, :], r=[b_PC[op_]]))
        if stop == 'E2':
            return
        S.op("vector", lambda e, co=co: e.tensor_scalar(KR[0][:], K32[:, co, :], vc[:, 4, co:co + 1], None, ALU.mult),
             r=[b_K[co], b_vc], w=[KR[1]])
        S.op("scalar", lambda e: e.activation(SQ[0][:], KR[0][:], AF.Square), r=[KR[1]], w=[SQ[1]])
        regk = PS[6][:, 256:256 + PBLK]
        S.op("tensor", lambda e: e.matmul(regk, blk[:], SQ[0][:], start=True, stop=True), r=[b_blk, SQ[1]], w=[b_ps[6][1]])
        S.op("scalar", lambda e: e.activation(RN[0][:], regk, AF.Sqrt, bias=epsc[:, 0:1], scale=1.0), r=[b_ps[6][1], b_eps], w=[RN[1]])
        S.op("vector", lambda e: e.reciprocal(RN[0][:], RN[0][:]), r=[RN[1]], w=[RN[1]])
        S.op("vector", lambda e: e.tensor_tensor(KK[0][:], KR[0][:], RN[0][:], ALU.mult), r=[KR[1], RN[1]], w=[KK[1]])
        if stop == 'E3':
            return
        for z in range(2):
            S.op("vector", lambda e, z=z, co=co: e.tensor_scalar(T1[0][:], AZ[z][0][:], -1.0, vc[:, 5, co:co + 1], ALU.add, ALU.mult),
                 r=[AZ[z][1], b_vc], w=[T1[1]])
            S.op("vector", lambda e, z=z, co=co: e.scalar_tensor_tensor(KM[z][0][:], T1[0][:], 1.0, K32[:, co, :], ALU.add, ALU.mult),
                 r=[T1[1], b_K[co]], w=[KM[z][1]])
        O = OB[op_]
        for z in range(2):
            S.op("vector", lambda e, z=z, O=O: e.scalar_tensor_tensor(O[z][0][:], KK[0][:], -1.0, Ee[z][0][:], ALU.mult, ALU.mult),
                 r=[KK[1], Ee[z][1]], w=[O[z][1]])
            S.op("gpsimd", lambda e, z=z, O=O, co=co: e.tensor_tensor(O[2 + z][0][:], R32[:, co, :], Ec[z][0][:], ALU.mult),
                 r=[b_R[co], Ec[z][1]], w=[O[2 + z][1]])
            S.op("gpsimd", lambda e, z=z: e.tensor_tensor(BT[0][:], KK[0][:], AZ[z][0][:], ALU.mult), r=[KK[1], AZ[z][1]], w=[BT[1]])
            S.op("gpsimd", lambda e, z=z, O=O: e.tensor_tensor(O[4 + z][0][:], BT[0][:], En[z][0][:], ALU.mult),
                 r=[BT[1], En[z][1]], w=[O[4 + z][1]])
            S.op("gpsimd", lambda e, z=z, O=O: e.tensor_tensor(O[6 + z][0][:], KM[z][0][:], En[z][0][:], ALU.mult),
                 r=[KM[z][1], En[z][1]], w=[O[6 + z][1]])
        S.op("scalar", lambda e, O=O, co=co: e.copy(O[8][0][:], V32[:, co, :]), r=[b_V[co]], w=[O[8][1]])
        for k in range(9):
            out_ops.append(S.dma("sync", ob[k, co, :, tok0:tok0 + PBLK], O[k][0][:], r=[O[k][1]]))
        if stop == 'E4':
            return
        S.op("gpsimd", lambda e: e.tensor_tensor(KS[0][:], KM[0][0][:], KM[1][0][:], ALU.add), r=[KM[0][1], KM[1][1]], w=[KS[1]])
        S.op("vector", lambda e, co=co: e.scalar_tensor_tensor(PB[0][:], R32[:, co, :], vc[:, 6, co:co + 1], KS[0][:], ALU.mult, ALU.mult),
             r=[b_R[co], b_vc, KS[1]], w=[PB[1]])
        regbv = PS[7][:, 0:PBLK]
        S.op("tensor", lambda e: e.matmul(regbv, blk[:], PB[0][:], start=True, stop=True), r=[b_blk, PB[1]], w=[b_ps[7][0]])
        S.op("vector", lambda e, op_=op_, co=co: e.tensor_tensor(BV[op_][0][:], regbv, V32[:, co, :], ALU.mult),
             r=[b_ps[7][0], b_V[co]], w=[BV[op_][1]])
        out_ops.append(S.dma("sync", of[0, co, :, tok0:tok0 + PBLK], BV[op_][0][:], r=[BV[op_][1]]))

    nblk_lat = nlat // PBLK
    blocks = [("lat", bi) for bi in range(nblk_lat)] + ([("ctx", 0)] if with_ctx else [])
    tcount = 0
    ocount = 0
    C0 = 64
    for kind, bi in blocks:
        isctx = kind == "ctx"
        v = 1 if isctx else 0
        if isctx:
            row0 = nlat + 128
            tl = [(row0, C0), (row0 + 128, C0 + 128)]
            tok0 = nlat
        else:
            row0 = bi * PBLK
            tl = [(row0, 0), (row0 + 128, 128), (row0 + 256, 256)]
            tok0 = bi * PBLK
        for (rw, col) in tl:
            par = tcount % 2
            tcount += 1
            S.dma("sync", xt[par][:], xin[rw:rw + 128, :], w=[b_xt[par]])
            ss = sm[par][:, 0:1]; rstd = sm[par][:, 1:2]
            S.op("scalar", lambda e, par=par, ss=ss: e.activation(xs[:], xt[par][:], AF.Square, accum_out=ss),
                 r=[b_xt[par]], w=[b_xs, b_sm[par]])
            S.op("vector", lambda e, ss=ss, rstd=rstd: e.tensor_scalar(rstd, ss, 1.0 / D, 1e-6, ALU.mult, ALU.add),
                 r=[b_sm[par]], w=[b_sm[par]])
            S.op("scalar", lambda e, rstd=rstd: e.activation(rstd, rstd, AF.Sqrt), r=[b_sm[par]], w=[b_sm[par]])
            S.op("vector", lambda e, rstd=rstd: e.reciprocal(rstd, rstd), r=[b_sm[par]], w=[b_sm[par]])
            S.op("vector", lambda e, par=par, rstd=rstd: e.tensor_scalar(xs[:], xt[par][:], rstd, None, ALU.mult),
                 r=[b_xt[par], b_sm[par]], w=[b_xs])
            for c in range(8):
                pb = c // 4
                S.op("tensor", lambda e, c=c, pb=pb: e.transpose(PS[pb][:, (c % 4) * 128:(c % 4) * 128 + 128],
                                                                 xs[:, c * 128:(c + 1) * 128], ident[:]),
                     r=[b_xs, b_id], w=b_ps[pb])
            for c in range(8):
                pb = c // 4
                S.op("scalar", lambda e, c=c, pb=pb, v=v, col=col: e.activation(
                    HL[:, c, col:col + 128], PS[pb][:, (c % 4) * 128:(c % 4) * 128 + 128], AF.Identity,
                    bias=mc[:, 2 + 2 * v, c:c + 1], scale=gsc[:, v, c:c + 1]),
                    r=b_ps[pb] + [b_mc, b_gsc], w=[b_HL[c]])
        if stop == 'A':
            continue
        for c in range(8):
            ctr = HL[:, c, C0:C0 + PBLK]
            if not isctx:
                c4 = ctr.rearrange("p (a b) -> p a b", b=64)
                x4 = XX[:, c, :].rearrange("p (a b) -> p a b", b=64)
                if c < 2:
                    S.op("vector", lambda e, x4=x4, c4=c4: e.tensor_tensor(x4[:, :, 1:64], c4[:, :, 0:63], c4[:, :, 1:64], ALU.subtract),
                         r=[b_HL[c]], w=[b_XX[c]])
                    S.op("vector", lambda e, x4=x4, c4=c4: e.tensor_scalar(x4[:, :, 0:1], c4[:, :, 0:1], -1.0, None, ALU.mult),
                         r=[b_HL[c]], w=[b_XX[c]])
                elif c < 4:
                    S.op("vector", lambda e, x4=x4, c4=c4: e.tensor_tensor(x4[:, :, 0:63], c4[:, :, 1:64], c4[:, :, 0:63], ALU.subtract),
                         r=[b_HL[c]], w=[b_XX[c]])
                    S.op("vector", lambda e, x4=x4, c4=c4: e.tensor_scalar(x4[:, :, 63:64], c4[:, :, 63:64], -1.0, None, ALU.mult),
                         r=[b_HL[c]], w=[b_XX[c]])
                elif c < 6:
                    S.op("vector", lambda e, c=c, ctr=ctr: e.tensor_tensor(XX[:, c, :], HL[:, c, 0:PBLK], ctr, ALU.subtract),
                         r=[b_HL[c]], w=[b_XX[c]])
                    if bi == 0:
                        S.op("vector", lambda e, c=c: e.scalar_tensor_tensor(XX[:, c, 0:64], HL[:, c, 0:64], edge[:, 0:1], HL[:, c, 64:128],
                                                                             ALU.mult, ALU.subtract),
                             r=[b_HL[c], b_edge], w=[b_XX[c]])
                else:
                    S.op("vector", lambda e, c=c, ctr=ctr: e.tensor_tensor(XX[:, c, :], HL[:, c, 128:128 + PBLK], ctr, ALU.subtract),
                         r=[b_HL[c]], w=[b_XX[c]])
                    if bi == nblk_lat - 1:
                        S.op("vector", lambda e, c=c: e.scalar_tensor_tensor(XX[:, c, PBLK - 64:PBLK], HL[:, c, PBLK + 64:PBLK + 128], edge[:, 1:2],
                                                                             HL[:, c, PBLK:PBLK + 64], ALU.mult, ALU.subtract),
                             r=[b_HL[c], b_edge], w=[b_XX[c]])
            else:
                if c < 4:
                    S.op("vector", lambda e, c=c: e.tensor_tensor(XX[:, c, 1:PBLK], HL[:, c, C0:C0 + PBLK - 1], HL[:, c, C0 + 1:C0 + PBLK], ALU.subtract),
                         r=[b_HL[c]], w=[b_XX[c]])
                    S.op("vector", lambda e, c=c: e.tensor_scalar(XX[:, c, 0:1], HL[:, c, C0:C0 + 1], -1.0, None, ALU.mult),
                         r=[b_HL[c]], w=[b_XX[c]])
                else:
                    S.op("vector", lambda e, c=c: e.tensor_tensor(XX[:, c, 0:PBLK - 1], HL[:, c, C0 + 1:C0 + PBLK], HL[:, c, C0:C0 + PBLK - 1], ALU.subtract),
                         r=[b_HL[c]], w=[b_XX[c]])
                    S.op("vector", lambda e, c=c: e.tensor_scalar(XX[:, c, PBLK - 1:PBLK], HL[:, c, C0 + PBLK - 1:C0 + PBLK], -1.0, None, ALU.mult),
                         r=[b_HL[c]], w=[b_XX[c]])
        if stop == 'B':
            continue
        for j in range(6):
            eng = "vector"
            for c in range(8):
                S.op(eng, lambda e, j=j, c=c: e.scalar_tensor_tensor(XM[j][:, c, :], XX[:, c, :], mu[:, j, c:c + 1], HL[:, c, C0:C0 + PBLK],
                                                                     ALU.mult, ALU.add),
                     r=[b_XX[c], b_HL[c], b_mu], w=[b_XM[j][c]])
        if stop == 'C':
            continue
        pk = [0]

        def proj(wsb, bw, j, dst, bdst, extra=None):
            for co in range(8):
                pb = 2 + pk[0] % 2; hb = (pk[0] // 2) % 2
                pk[0] += 1
                reg = PS[pb][:, hb * 256:hb * 256 + PBLK]
                for c in range(8):
                    S.op("tensor", lambda e, reg=reg, c=c, co=co, wsb=wsb, j=j: e.matmul(
                        reg, wsb[:, c, co * 128:(co + 1) * 128], XM[j][:, c, :], start=(c == 0), stop=(c == 7)),
                        r=[bw] + b_XM[j], w=[b_ps[pb][hb]])
                S.op("scalar", lambda e, reg=reg, co=co, dst=dst: e.copy(dst[:, co, :], reg), r=[b_ps[pb][hb]], w=[bdst[co]])
        proj(wr, b_wr, 0, R32, b_R)
        proj(wk, b_wk, 2, K32, b_K)
        proj(wv, b_wv, 3, V32, b_V)
        for z in range(2):
            reg = PS[4][0:64, z * 256:z * 256 + PBLK]
            for c in range(8):
                S.op("tensor", lambda e, reg=reg, c=c, z=z: e.matmul(reg, w1[:, z, c, :], XM[1][:, c, :], start=(c == 0), stop=(c == 7)),
                     r=[b_w1] + b_XM[1], w=[b_ps[4][z]])
            S.op("scalar", lambda e, reg=reg, z=z: e.activation(TW[z][:], reg, AF.Tanh), r=[b_ps[4][z]], w=[b_TW[z]])
        for z in range(2):
            reg = PS[5][0:64, z * 256:z * 256 + PBLK]
            for c in range(8):
                S.op("tensor", lambda e, reg=reg, c=c, z=z: e.matmul(reg, a1[:, z, c, :], XM[4][:, c, :], start=(c == 0), stop=(c == 7)),
                     r=[b_a1] + b_XM[4], w=[b_ps[5][z]])
            S.op("scalar", lambda e, reg=reg, z=z: e.copy(AL[z][:], reg), r=[b_ps[5][z]], w=[b_AL[z]])
        rega = PS[6][:, 0:PBLK]; regb = PS[6][0:32, 256:256 + PBLK]
        for c in range(8):
            S.op("tensor", lambda e, c=c: e.matmul(rega, g1[:, c, 0:128], XM[5][:, c, :], start=(c == 0), stop=(c == 7)),
                 r=[b_g1] + b_XM[5], w=[b_ps[6][0]])
        for c in range(8):
            S.op("tensor", lambda e, c=c: e.matmul(regb, g1[:, c, 128:160], XM[5][:, c, :], start=(c == 0), stop=(c == 7)),
                 r=[b_g1] + b_XM[5], w=[b_ps[6][1]])
        S.op("scalar", lambda e: e.activation(SGa[:], rega, AF.Sigmoid), r=[b_ps[6][0]], w=[b_SG])
        S.op("scalar", lambda e: e.activation(SGb[:], regb, AF.Sigmoid), r=[b_ps[6][1]], w=[b_SG])
        if stop == 'D':
            continue
        for co in range(8):
            op_ = ocount % 2
            ocount += 1
            E_body(co, op_, tok0, isctx, LW2[op_], AZ2[op_], Ec2[op_], Ee2[op_], En2[op_], KM2[op_], KK2[op_])
    S.emit(out_ops)
    return nc


_PREP_NC = {}


def prep_consts():
    blk = np.zeros((128, 128), np.float32); blk[:64, :64] = 1; blk[64:, 64:] = 1
    cm = np.ones((128, PBLK), np.float32); cm[:, ::64] = 0
    return dict(ident=np.eye(128, dtype=np.float32), blk=blk, cmask=cm)


def colsk(vs):
    return np.ascontiguousarray(np.stack([cols(v) for v in vs], axis=1))


def run_prep(x, xc, i, mods, inp, nlat=NLAT, trace=False):
    j = i // 2
    key = nlat
    if key not in _PREP_NC:
        _PREP_NC[key] = build_prep(nlat=nlat)
    P = {k: inp["rw_" + k][j] for k in ["mu", "wr", "wk", "wv", "w0", "w1", "w2", "a0", "a1", "a2", "g1", "g2", "k_k", "k_a", "r_k"]}
    base = dict(prep_consts())
    base.update(mu=colsk([P["mu"][t] for t in range(6)]),
                vcols=colsk([P["w0"][0], P["w0"][1], P["a0"][0], P["a0"][1], P["k_k"], P["k_a"], P["r_k"].reshape(-1), np.zeros(D)]),
                wr=np.ascontiguousarray(P["wr"]), wk=np.ascontiguousarray(P["wk"]), wv=np.ascontiguousarray(P["wv"]),
                w1=np.ascontiguousarray(P["w1"]), w2=np.ascontiguousarray(P["w2"]), a1=np.ascontiguousarray(P["a1"]),
                a2=np.ascontiguousarray(P["a2"]), g1=np.ascontiguousarray(P["g1"]), g2=np.ascontiguousarray(P["g2"]))
    nq = x.shape[1] // nlat
    maps = []
    for k in range(NCORES):
        b, q = k // nq, k % nq
        lo = q * nlat
        z64 = np.zeros((64, D), np.float32)
        hb = x[b, lo - 64:lo] if q > 0 else z64
        ha = x[b, lo + nlat:lo + nlat + 64] if q < nq - 1 else z64
        xin = np.concatenate([hb, x[b, lo:lo + nlat], ha, xc[b]], axis=0)
        ml = mods[i, b]; mcx = mods[i, 2]
        mc = np.stack([cols(inp["norm1_g"][i]), cols(ml[1 * D:2 * D]), cols(ml[0:D]), cols(mcx[1 * D:2 * D]), cols(mcx[0:D])], axis=1)
        edge = np.zeros((128, 2), np.float32); edge[:, 0] = 1.0 if q > 0 else 0.0; edge[:, 1] = 1.0 if q < nq - 1 else 0.0
        m = dict(base)
        m.update(xin=np.ascontiguousarray(xin, dtype=np.float32), mcols=np.ascontiguousarray(mc), edge=edge)
        maps.append(m)
    res = run_bass_kernel_spmd(_PREP_NC[key], maps, core_ids=list(range(NCORES)), trace=trace)
    if trace:
        print("prep exec_time_ns", res.exec_time_ns)
    return res.results


SGS = 4


def build_scan(nch=260, mode=None):
    nc = new_nc()
    AR_d = dram_in(nc, "AR", [64, nch, 8, 128], BF16)
    BK_d = dram_in(nc, "BK", [64, nch, 8, 128], BF16)
    BKT_d = dram_in(nc, "BKT", [128, nch, 8, 64], BF16)
    VT_d = dram_in(nc, "VT", [64, nch, 8, 64], BF16)
    PC_d = dram_in(nc, "PCB", [64, nch, 8])
    mg_d = dram_in(nc, "maskG", [128, 8, 128])
    mn_d = dram_in(nc, "maskNT", [64, 8, 64])
    id_d = dram_in(nc, "ident8", [64, 8, 64])
    Y_d = dram_out(nc, "Y", [64, nch, 8, 64])
    S = Sched(nc)
    maskG = S.sb("maskG", [128, 8, 128], F32); maskNT = S.sb("maskNT", [64, 8, 64], F32); id8 = S.sb("id8", [64, 8, 64], F32)
    b_c = Buf()
    ARs = [S.sb("AR%d" % i, [64, SGS, 8, 128], BF16) for i in range(2)]; b_AR = [Buf() for _ in range(2)]
    BKs = [S.sb("BK%d" % i, [64, SGS, 8, 128], BF16) for i in range(2)]; b_BK = [Buf() for _ in range(2)]
    BTs = [S.sb("BT%d" % i, [64, SGS, 8, 64], BF16) for i in range(2)]; b_BKT = [Buf() for _ in range(2)]
    KTs = [S.sb("KT%d" % i, [64, SGS, 8, 64], BF16) for i in range(2)]
    VTs = [S.sb("VT%d" % i, [64, SGS, 8, 64], BF16) for i in range(2)]
    UTs = [S.sb("UT%d" % i, [64, SGS, 8, 64], BF16) for i in range(2)]
    b_UVv = [Buf() for _ in range(2)]
    b_UVu = [[Buf() for _ in range(SGS)] for _ in range(2)]
    PCs = [S.sb("PC%d" % i, [64, SGS, 8], F32) for i in range(2)]; b_PC = [Buf() for _ in range(2)]
    Yst = [S.sb("Yst%d" % i, [64, SGS, 8, 64], F32) for i in range(2)]; b_Y = [Buf() for _ in range(2)]
    Gm = [S.sb("Gm%d" % i, [64, 8, 128], BF16) for i in range(2)]; b_Gm = [Buf() for _ in range(2)]
    Gk = [S.sb("Gk%d" % i, [64, 8, 128], BF16) for i in range(2)]
    NTm = [S.sb("NTm%d" % i, [64, 8, 64], BF16) for i in range(2)]; b_NTm = [Buf() for _ in range(2)]
    Pb = [S.sb("Pb%d" % i, [64, 8, 64], BF16) for i in range(2)]; b_Pb = [Buf() for _ in range(2)]
    PTb = [S.sb("PTb%d" % i, [64, 8, 64], BF16) for i in range(2)]; b_PTb = [Buf() for _ in range(2)]
    T32 = [S.sb("T32_%d" % i, [64, 8, 64], F32) for i in range(2)]; b_T32 = [Buf() for _ in range(2)]
    Tb = [S.sb("Tb%d" % i, [64, 8, 64], BF16) for i in range(2)]; b_Tb = [Buf() for _ in range(2)]
    WTb = S.sb("WTb", [64, 8, 64], BF16); b_WT = Buf()
    S32 = S.sb("S32", [64, 8, 64], F32); b_S32 = Buf()
    Sb = S.sb("Sb", [64, 8, 64], BF16); b_Sb = Buf()
    PS = [S.ps("psb%d" % i, [128, 512], F32) for i in range(8)]; b_ps = [Buf() for _ in range(8)]

    S.dma("sync", maskG[:], mg_d, w=[b_c]); S.dma("sync", maskNT[:], mn_d, w=[b_c]); S.dma("sync", id8[:], id_d, w=[b_c])
    S.op("vector", lambda e: e.memset(S32[:], 0.0), w=[b_S32])
    S.op("vector", lambda e: e.memset(Sb[:], 0.0), w=[b_Sb])
    out_ops = []
    ngrp = (nch + SGS - 1) // SGS

    def load_group(gi):
        gp = gi % 2
        s0 = gi * SGS; n = min(SGS, nch - s0)
        S.dma("sync", ARs[gp][:, 0:n], AR_d[:, s0:s0 + n], w=[b_AR[gp]])
        S.dma("sync", BKs[gp][:, 0:n], BK_d[:, s0:s0 + n], w=[b_BK[gp]])
        S.dma("sync", BTs[gp][:, 0:n], BKT_d[0:64, s0:s0 + n], w=[b_BKT[gp]])
        S.dma("sync", KTs[gp][:, 0:n], BKT_d[64:128, s0:s0 + n], w=[b_BKT[gp]])
        S.dma("sync", VTs[gp][:, 0:n], VT_d[:, s0:s0 + n], w=[b_UVv[gp]])
        S.dma("sync", PCs[gp][:, 0:n], PC_d[:, s0:s0 + n], w=[b_PC[gp]])

    def prep_stages(j):
        gp = (j // SGS) % 2; g = j % SGS; sp = j % 2
        AR = ARs[gp]; BK = BKs[gp]
        st = []

        def s_G():
            for i in range(8):
                S.op("tensor", lambda e, i=i: e.matmul(PS[i // 4][0:64, (i % 4) * 128:(i % 4) * 128 + 128], BK[:, g, i, 0:64], AR[:, g, i, :],
                                                       start=True, stop=True), r=[b_BK[gp], b_AR[gp]], w=[b_ps[i // 4]])
            for i in range(8):
                S.op("tensor", lambda e, i=i: e.matmul(PS[3 + i // 4][0:64, (i % 4) * 128:(i % 4) * 128 + 128], BK[:, g, i, 64:128], AR[:, g, i, :],
                                                       start=True, stop=True), r=[b_BK[gp], b_AR[gp]], w=[b_ps[3 + i // 4]])
            for i in range(8):
                S.op("tensor", lambda e, i=i: e.matmul(PS[2][0:64, i * 64:(i + 1) * 64], AR[:, g, i, 0:64], BK[:, g, i, 0:64],
                                                       start=True, stop=True), r=[b_BK[gp], b_AR[gp]], w=[b_ps[2]])
            for h in range(2):
                S.op("vector", lambda e, h=h: e.tensor_tensor(Gm[sp][:, 4 * h:4 * h + 4, :], PS[h][0:64, :].rearrange("p (a b) -> p a b", a=4),
                                                              maskG[0:64, 4 * h:4 * h + 4, :], ALU.mult), r=[b_ps[h], b_c], w=[b_Gm[sp]])
                S.op("vector", lambda e, h=h: e.tensor_tensor(Gk[sp][:, 4 * h:4 * h + 4, :], PS[3 + h][0:64, :].rearrange("p (a b) -> p a b", a=4),
                                                              maskG[0:64, 4 * h:4 * h + 4, :], ALU.mult), r=[b_ps[3 + h], b_c], w=[b_Gm[sp]])
            S.op("vector", lambda e: e.tensor_tensor(NTm[sp][:], PS[2][0:64, :].rearrange("p (a b) -> p a b", a=8), maskNT[:], ALU.mult),
                 r=[b_ps[2], b_c], w=[b_NTm[sp]])
            S.op("vector", lambda e: e.tensor_tensor(Tb[sp][:], Gm[sp][0:64, :, 0:64], id8[:], ALU.add), r=[b_Gm[sp], b_c], w=[b_Tb[sp]])
        st.append(s_G)
        for l in range(1, 6):
            def s_sq(l=l):
                if l == 1:
                    Pp = lambda i: Gm[sp][0:64, i, 0:64]; PTp = lambda i: NTm[sp][:, i, :]
                    rP = [b_Gm[sp]]; rPT = [b_NTm[sp]]
                else:
                    q = (l - 1) % 2
                    Pp = lambda i, q=q: Pb[q][:, i, :]; PTp = lambda i, q=q: PTb[q][:, i, :]
                    rP = [b_Pb[q]]; rPT = [b_PTb[q]]
                qn = l % 2
                if l < 5:
                    for i in range(8):
                        S.op("tensor", lambda e, i=i: e.matmul(PS[3][0:64, i * 64:(i + 1) * 64], PTp(i), Pp(i), start=True, stop=True),
                             r=rP + rPT, w=[b_ps[3]])
                for i in range(8):
                    S.op("tensor", lambda e, i=i: e.matmul(PS[4][0:64, i * 64:(i + 1) * 64], Pp(i), PTp(i), start=True, stop=True),
                         r=rP + rPT, w=[b_ps[4]])
                if l < 5:
                    S.op("scalar", lambda e: e.copy(Pb[qn][:], PS[3][0:64, :].rearrange("p (a b) -> p a b", a=8)), r=[b_ps[3]], w=[b_Pb[qn]])
                S.op("scalar", lambda e: e.copy(PTb[qn][:], PS[4][0:64, :].rearrange("p (a b) -> p a b", a=8)), r=[b_ps[4]], w=[b_PTb[qn]])
            st.append(s_sq)

            def s_T(l=l):
                qn = l % 2
                for i in range(8):
                    S.op("tensor", lambda e, i=i: e.matmul(PS[2][0:64, i * 64:(i + 1) * 64], PTb[qn][:, i, :], Tb[sp][:, i, :], start=True, stop=True),
                         r=[b_PTb[qn], b_Tb[sp]], w=[b_ps[2]])
                S.op("vector", lambda e: e.tensor_tensor(Tb[sp][:], Tb[sp][:], PS[2][0:64, :].rearrange("p (a b) -> p a b", a=8), ALU.add),
                     r=[b_ps[2]], w=[b_Tb[sp]])
            st.append(s_T)
        sq = [st[1 + 2 * l] for l in range(5)]; tu = [st[2 + 2 * l] for l in range(5)]
        st = [st[0], sq[0], sq[1], tu[0], sq[2], tu[1], sq[3], tu[2], sq[4], tu[3], tu[4]]
        return st

    def seq_stages(j):
        gp = (j // SGS) % 2; g = j % SGS; sp = j % 2
        AR = ARs[gp]; UT = UTs[gp]; VT = VTs[gp]; BT = BTs[gp]; KT = KTs[gp]
        st = []

        def s_W():
            for i in range(8):
                reg = PS[5][0:64, i * 64:(i + 1) * 64]
                S.op("tensor", lambda e, i=i, reg=reg: e.matmul(reg, AR[:, g, i, 0:64], Sb[:, i, :], start=True, stop=False),
                     r=[b_AR[gp], b_Sb], w=[b_ps[5]])
                S.op("tensor", lambda e, i=i, reg=reg: e.matmul(reg, Gk[sp][:, i, 0:64], VT[:, g, i, :], start=False, stop=True),
                     r=[b_Gm[sp], b_UVv[gp]], w=[b_ps[5]])
            S.op("scalar", lambda e: e.copy(WTb[:], PS[5][0:64, :].rearrange("p (a b) -> p a b", a=8)), r=[b_ps[5]], w=[b_WT])
        st.append(s_W)

        def s_U():
            for i in range(8):
                S.op("tensor", lambda e, i=i: e.matmul(PS[6][0:64, i * 64:(i + 1) * 64], Tb[sp][:, i, :], WTb[:, i, :], start=True, stop=True),
                     r=[b_Tb[sp], b_WT], w=[b_ps[6]])
            S.op("scalar", lambda e: e.copy(UT[:, g, :, :], PS[6][0:64, :].rearrange("p (a b) -> p a b", a=8)),
                 r=[b_ps[6]], w=[b_UVu[gp][g]])
        st.append(s_U)

        def s_YS():
            for i in range(8):
                reg = PS[7][0:64, i * 64:(i + 1) * 64]
                S.op("tensor", lambda e, i=i, reg=reg: e.matmul(reg, AR[:, g, i, 64:128], Sb[:, i, :], start=True, stop=False),
                     r=[b_AR[gp], b_Sb], w=[b_ps[7]])
                S.op("tensor", lambda e, i=i, reg=reg: e.matmul(reg, Gm[sp][:, i, 64:128], UT[:, g, i, :], start=False, stop=False),
                     r=[b_Gm[sp], b_UVu[gp][g]], w=[b_ps[7]])
                S.op("tensor", lambda e, i=i, reg=reg: e.matmul(reg, Gk[sp][:, i, 64:128], VT[:, g, i, :], start=False, stop=True),
                     r=[b_Gm[sp], b_UVv[gp]], w=[b_ps[7]])
            for i in range(8):
                S.op("tensor", lambda e, i=i: e.matmul(PS[5][0:64, i * 64:(i + 1) * 64], BT[:, g, i, :], UT[:, g, i, :], start=True, stop=False),
                     r=[b_BKT[gp], b_UVu[gp][g]], w=[b_ps[5]])
                S.op("tensor", lambda e, i=i: e.matmul(PS[5][0:64, i * 64:(i + 1) * 64], KT[:, g, i, :], VT[:, g, i, :], start=False, stop=True),
                     r=[b_BKT[gp], b_UVv[gp]], w=[b_ps[5]])
            S.op("scalar", lambda e: e.copy(Yst[gp][:, g, :, :], PS[7][0:64, :].rearrange("p (a b) -> p a b", a=8)), r=[b_ps[7]], w=[b_Y[gp]])
            S.op("vector", lambda e: e.tensor_tensor(S32[:], S32[:], PS[5][0:64, :].rearrange("p (a b) -> p a b", a=8), ALU.add),
                 r=[b_ps[5]], w=[b_S32])
            S.op("vector", lambda e: e.tensor_tensor(S32[:], S32[:], PCs[gp][:, g, :].unsqueeze(2).to_broadcast([64, 8, 64]), ALU.mult),
                 r=[b_PC[gp]], w=[b_S32])
            S.op("scalar", lambda e: e.copy(Sb[:], S32[:]), r=[b_S32], w=[b_Sb])
        st.append(s_YS)
        return st

    load_group(0)
    if ngrp > 1:
        load_group(1)
    for f in prep_stages(0):
        f()
    for j in range(nch):
        seq = seq_stages(j)
        pre = prep_stages(j + 1) if j + 1 < nch else []
        pos = {1: 0, 4: 1, 7: 2}
        k = 0
        nseq = {None: 3, "prep": 0, "W": 1, "U": 2}[mode]
        for pi in range(max(len(pre), 8)):
            if pi < len(pre):
                pre[pi]()
            if pi in pos:
                if pos[pi] < nseq:
                    seq[pos[pi]]()
                k += 1
        for si in range(k, 3):
            if si < nseq:
                seq[si]()
        if (j + 1) % SGS == 0 or j == nch - 1:
            gi = j // SGS
            s0 = gi * SGS; n = min(SGS, nch - s0)
            out_ops.append(S.dma("sync", Y_d[:, s0:s0 + n], Yst[gi % 2][:, 0:n], r=[b_Y[gi % 2]]))
            if gi + 2 < ngrp:
                load_group(gi + 2)
    S.emit(out_ops)
    return nc


def scan_consts():
    s = np.arange(64)[:, None]; t = np.arange(64)[None, :]
    mg = np.zeros((128, 8, 128), np.float32); mn = np.zeros((64, 8, 64), np.float32); idm = np.zeros((64, 8, 64), np.float32)
    for i in range(8):
        z = i // 4
        strict = (s < t) if z == 0 else (s > t)
        incl = (s <= t) if z == 0 else (s >= t)
        blk = np.concatenate([strict, incl], axis=1).astype(np.float32)
        mg[:, i, :] = np.concatenate([blk, blk], axis=0)
        mn[:, i, :] = strict.T.astype(np.float32)
        idm[:, i, :] = np.eye(64, dtype=np.float32)
    return dict(maskG=mg, maskNT=mn, ident8=idm)


GN_EPS = 64e-5


def build_rout(ntok=NTOK):
    nc = new_nc()
    x_d = dram_in(nc, "xin", [ntok, D]); y0_d = dram_in(nc, "y0", [ntok, D]); y1_d = dram_in(nc, "y1", [ntok, D])
    bv_d = dram_in(nc, "bv", [ntok, D]); ga_d = dram_in(nc, "gate", [ntok, D])
    lg_d = dram_in(nc, "lngb", [128, D]); lb_d = dram_in(nc, "lnbb", [128, D]); gt_d = dram_in(nc, "gtb", [128, 2, D])
    wo_d = dram_in(nc, "wo", [D, D]); id_d = dram_in(nc, "ident", [128, 128])
    xout = dram_out(nc, "xout", [ntok, D])
    S = Sched(nc)
    ident = S.sb("ident", [128, 128], F32); b_id = Buf()
    lgb = S.sb("lgb", [128, D], F32); lbb = S.sb("lbb", [128, D], F32); gtb = S.sb("gtb", [128, 2, D], F32); b_c = Buf()
    wo = S.sb("wo", [128, 8, D], BF16); b_wo = Buf()
    names = ["x", "y0", "y1", "bv", "ga"]
    srcs = [x_d, y0_d, y1_d, bv_d, ga_d]
    tl = [[S.sb("%s%d" % (n, i), [128, D], F32) for n in names] for i in range(2)]
    b_tl = [[Buf() for _ in names] for _ in range(2)]
    yc = S.sb("yc", [128, D], F32); b_yc = Buf()
    sq = S.sb("sq", [128, D], F32); b_sq = Buf()
    zT = [S.sb("zT%d" % i, [128, 8, 128], BF16) for i in range(2)]; b_zT = [Buf() for _ in range(2)]
    ot = [S.sb("ot%d" % i, [128, D], F32) for i in range(2)]; b_ot = [Buf() for _ in range(2)]
    sm = [S.sb("sm%d" % i, [128, 4, 16], F32) for i in range(2)]; b_sm = [Buf() for _ in range(2)]
    PS = [S.ps("psb%d" % i, [128, 512], F32) for i in range(8)]; b_ps = [Buf() for _ in range(8)]
    S.dma("sync", ident[:], id_d, w=[b_id]); S.dma("sync", lgb[:], lg_d, w=[b_c]); S.dma("sync", lbb[:], lb_d, w=[b_c])
    S.dma("sync", gtb[:], gt_d, w=[b_c])
    S.dma("gpsimd", wo[:], wo_d.rearrange("(c p) n -> p c n", p=128), w=[b_wo])
    out_ops = []
    for ti, (off, P) in enumerate(token_tiles(ntok)):
        par = ti % 2
        v = 1 if off >= NLAT and ntok > NLAT else 0
        T = tl[par]; B = b_tl[par]
        for n in range(5):
            S.dma("sync" if n % 2 == 0 else "gpsimd", T[n][0:P, :], srcs[n][off:off + P, :], w=[B[n]])
        X, Y0, Y1, BVt, GA = [t[0:P, :] for t in T]
        m = sm[par]
        s1 = m[0:P, 0, :]; nm = m[0:P, 1, :]; s2 = m[0:P, 2, :]; rs = m[0:P, 3, :]
        v3 = lambda ap: ap.rearrange("p (h k) -> p h k", k=64)
        bc = lambda ap, P: ap.unsqueeze(2).to_broadcast([P, 16, 64])
        S.op("vector", lambda e, Y0=Y0, Y1=Y1: e.tensor_tensor(Y0, Y0, Y1, ALU.add), r=[B[2]], w=[B[1]])
        S.op("vector", lambda e, Y0=Y0, s1=s1: e.reduce_sum(s1, v3(Y0), AX.X), r=[B[1]], w=[b_sm[par]])
        S.op("vector", lambda e, s1=s1, nm=nm: e.tensor_scalar(nm, s1, -1.0 / 64, None, ALU.mult), r=[b_sm[par]], w=[b_sm[par]])
        S.op("vector", lambda e, Y0=Y0, nm=nm, P=P: e.tensor_tensor(v3(yc[0:P, :]), v3(Y0), bc(nm, P), ALU.add), r=[B[1], b_sm[par]], w=[b_yc])
        S.op("scalar", lambda e, P=P: e.activation(sq[0:P, :], yc[0:P, :], AF.Square), r=[b_yc], w=[b_sq])
        S.op("vector", lambda e, s2=s2, P=P: e.reduce_sum(s2, v3(sq[0:P, :]), AX.X), r=[b_sq], w=[b_sm[par]])
        S.op("vector", lambda e, s2=s2, rs=rs: e.tensor_scalar(rs, s2, 1.0 / 64, GN_EPS, ALU.mult, ALU.add), r=[b_sm[par]], w=[b_sm[par]])
        S.op("scalar", lambda e, rs=rs: e.activation(rs, rs, AF.Sqrt), r=[b_sm[par]], w=[b_sm[par]])
        S.op("vector", lambda e, rs=rs: e.reciprocal(rs, rs), r=[b_sm[par]], w=[b_sm[par]])
        S.op("vector", lambda e, rs=rs, P=P: e.tensor_tensor(v3(yc[0:P, :]), v3(yc[0:P, :]), bc(rs, P), ALU.mult), r=[b_sm[par]], w=[b_yc])
        S.op("vector", lambda e, P=P: e.tensor_tensor(yc[0:P, :], yc[0:P, :], lgb[0:P, :], ALU.mult), r=[b_c], w=[b_yc])
        S.op("gpsimd", lambda e, P=P: e.tensor_tensor(yc[0:P, :], yc[0:P, :], lbb[0:P, :], ALU.add), r=[b_c], w=[b_yc])
        S.op("gpsimd", lambda e, P=P, BVt=BVt: e.tensor_tensor(yc[0:P, :], yc[0:P, :], BVt, ALU.add), r=[B[3]], w=[b_yc])
        S.op("vector", lambda e, P=P, GA=GA: e.tensor_tensor(yc[0:P, :], yc[0:P, :], GA, ALU.mult), r=[B[4]], w=[b_yc])
        for c in range(8):
            pb = c // 4
            S.op("tensor", lambda e, P=P, c=c, pb=pb: e.transpose(PS[pb][:, (c % 4) * 128:(c % 4) * 128 + P], yc[0:P, c * 128:(c + 1) * 128], ident[0:P, 0:P]),
                 r=[b_yc, b_id], w=[b_ps[pb]])
        for pb in range(2):
            S.op("scalar", lambda e, P=P, pb=pb, par=par: e.copy(zT[par][:, 4 * pb:4 * pb + 4, 0:P],
                                                                  PS[pb][:, :].rearrange("p (a b) -> p a b", a=4)[:, :, 0:P]),
                 r=[b_ps[pb]], w=[b_zT[par]])
        for n in range(2):
            pb = 2 + n
            for c in range(8):
                S.op("tensor", lambda e, P=P, c=c, n=n, pb=pb, par=par: e.matmul(PS[pb][0:P, :], zT[par][:, c, 0:P], wo[:, c, n * 512:(n + 1) * 512],
                                                                                 start=(c == 0), stop=(c == 7)),
                     r=[b_zT[par], b_wo], w=[b_ps[pb]])
            sl = slice(n * 512, (n + 1) * 512)
            S.op("vector", lambda e, P=P, pb=pb, par=par, sl=sl, v=v: e.tensor_tensor(ot[par][0:P, sl], PS[pb][0:P, :], gtb[0:P, v, sl], ALU.mult),
                 r=[b_ps[pb], b_c], w=[b_ot[par]])
        S.op("gpsimd", lambda e, P=P, par=par, X=X: e.tensor_tensor(ot[par][0:P, :], ot[par][0:P, :], X, ALU.add), r=[B[0]], w=[b_ot[par]])
        out_ops.append(S.dma("sync", xout[off:off + P, :], ot[par][0:P, :], r=[b_ot[par]]))
    S.emit(out_ops)
    return nc


SEQ = 16384
CTX = 256
NCH_ALL = (SEQ + CTX) // 64
_SCAN_NC = {}
_ROUT_NC = {}
DEBUG_HOOK = None


def _dbg(name, arr):
    if DEBUG_HOOK is not None:
        DEBUG_HOOK(name, arr)


def run_rwkv_mixer(x, xc, i, mods, inp):
    j = i // 2
    pres = run_prep(x, xc, i, mods, inp)
    order = [np.arange(NCH_ALL), np.concatenate([np.arange(3, -1, -1), np.arange(NCH_ALL - 1, 3, -1)])]
    consts = scan_consts()
    maps = []
    for k in range(NCORES):
        b, hq = k // 4, k % 4
        if hq == 0:
            obs = [np.asarray(pres[b * 4 + q]["ob"]) for q in range(4)]
            pcs = [np.asarray(pres[b * 4 + q]["pc"]) for q in range(4)]
            F = np.concatenate([obs[0][:, :, :, NLAT:]] + [o[:, :, :, :NLAT] for o in obs], axis=3)
            F = F.reshape(9, 8, 128, NCH_ALL, 64)
            PCa = np.concatenate([pcs[0][:, :, :, NLAT // 64:]] + [p[:, :, :, :NLAT // 64] for p in pcs], axis=3)
        AR = np.empty((64, NCH_ALL, 8, 128), F.dtype); BK = np.empty((64, NCH_ALL, 8, 128), F.dtype)
        BKT = np.empty((128, NCH_ALL, 8, 64), F.dtype); VT = np.empty((64, NCH_ALL, 8, 64), F.dtype)
        PCB = np.empty((64, NCH_ALL, 8), np.float32)
        for ci in range(8):
            z, hl = ci // 4, ci % 4
            c = 2 * hq + hl // 2; p0 = (hl % 2) * 64
            od = order[z]
            A = F[0 + z, c, p0:p0 + 64][:, od]; R = F[2 + z, c, p0:p0 + 64][:, od]
            Bm = F[4 + z, c, p0:p0 + 64][:, od]; Km = F[6 + z, c, p0:p0 + 64][:, od]
            V = F[8, c, p0:p0 + 64][:, od]
            AR[:, :, ci, 0:64] = A; AR[:, :, ci, 64:128] = R
            BK[:, :, ci, 0:64] = Bm; BK[:, :, ci, 64:128] = Km
            BKT[0:64, :, ci, :] = Bm.transpose(2, 1, 0); BKT[64:128, :, ci, :] = Km.transpose(2, 1, 0)
            VT[:, :, ci, :] = V.transpose(2, 1, 0)
            PCB[:, :, ci] = PCa[z, c, p0:p0 + 64][:, od]
        m = dict(consts)
        m.update(AR=AR, BK=BK, BKT=BKT, VT=VT, PCB=PCB)
        maps.append(m)
    if NCH_ALL not in _SCAN_NC:
        _SCAN_NC[NCH_ALL] = build_scan(nch=NCH_ALL)
    sres = run_bass_kernel_spmd(_SCAN_NC[NCH_ALL], maps, core_ids=list(range(NCORES))).results
    yall = np.empty((2, 2, NCH_ALL, 64, 16, 64), np.float32)
    for k in range(NCORES):
        b, hq = k // 4, k % 4
        Y = np.asarray(sres[k]["Y"])
        for ci in range(8):
            z, hl = ci // 4, ci % 4
            yall[z, b, order[z], :, 4 * hq + hl, :] = Y[:, :, ci, :].transpose(1, 0, 2)
    yall = yall.reshape(2, 2, NCH_ALL * 64, D)
    _dbg("y_l%d" % i, yall)
    P = {kk: inp["rw_" + kk][j] for kk in ["wo", "ln_g", "ln_b"]}
    base = dict(lngb=bcast(P["ln_g"]), lnbb=bcast(P["ln_b"]), wo=np.ascontiguousarray(P["wo"]), ident=np.eye(128, dtype=np.float32))
    maps = []
    for k in range(NCORES):
        b, q = k // 4, k % 4
        lat = slice(CTX + q * NLAT, CTX + (q + 1) * NLAT); cx = slice(q * NCTX, (q + 1) * NCTX)
        of = np.asarray(pres[k]["of"])
        oft = of.transpose(0, 3, 1, 2).reshape(2, NLAT + CTX, D)
        tok = np.concatenate([np.arange(NLAT), NLAT + np.arange(q * NCTX, (q + 1) * NCTX)])
        ml = mods[i, b]; mcx = mods[i, 2]
        m = dict(base)
        m.update(xin=np.concatenate([x[b, q * NLAT:(q + 1) * NLAT], xc[b, cx]], axis=0),
                 y0=np.concatenate([yall[0, b, lat], yall[0, b, cx]], axis=0),
                 y1=np.concatenate([yall[1, b, lat], yall[1, b, cx]], axis=0),
                 bv=np.ascontiguousarray(oft[0][tok]), gate=np.ascontiguousarray(oft[1][tok]),
                 gtb=np.ascontiguousarray(np.stack([bcast(ml[2 * D:3 * D]), bcast(mcx[2 * D:3 * D])], axis=1)))
        maps.append(m)
    if NTOK not in _ROUT_NC:
        _ROUT_NC[NTOK] = build_rout(ntok=NTOK)
    ores = run_bass_kernel_spmd(_ROUT_NC[NTOK], maps, core_ids=list(range(NCORES))).results
    return [np.asarray(r["xout"]) for r in ores]


def kernel(**inp):
    inp = {k: np.asarray(v) for k, v in inp.items()}
    x = np.array(inp["x"], np.float32)
    xc = np.array(inp["ctx"], np.float32)
    mods = run_mod(inp["c"], inp["c_ctx"], inp["ada_w"], inp["ada_b"])
    for i in range(4):
        last = i == 3
        if i % 2 == 0:
            xs = []
            for k in range(NCORES):
                b, q = k // 4, k % 4
                xs.append(np.concatenate([x[b, q * NLAT:(q + 1) * NLAT], xc[b, (q // 2) * 128:(q // 2 + 1) * 128]], axis=0))
            outs = run_gmlp(xs, i, mods, inp)
            mix = []
            for k in range(NCORES):
                q = k % 4
                mix.append(np.concatenate([outs[k][:NLAT], outs[k][NLAT + (q % 2) * 64:NLAT + (q % 2) * 64 + 64]], axis=0))
        else:
            mix = run_rwkv_mixer(x, xc, i, mods, inp)
        _dbg("mix%d" % i, mix)
        outs = run_moe(mix, i, mods, inp, final=last)
        xn = np.empty_like(x); xcn = np.empty_like(xc)
        for k in range(NCORES):
            b, q = k // 4, k % 4
            xn[b, q * NLAT:(q + 1) * NLAT] = outs[k][:NLAT]
            xcn[b, q * NCTX:(q + 1) * NCTX] = outs[k][NLAT:]
        x, xc = xn, xcn
        _dbg("x%d" % i, x)
    return x
```

```python
from contextlib import ExitStack
import numpy as np
import concourse.bass as bass
import concourse.mybir as mybir
from concourse.bass_utils import run_bass_kernel_spmd

F32 = mybir.dt.float32
BF16 = mybir.dt.bfloat16
AF = mybir.ActivationFunctionType
ALU = mybir.AluOpType
AX = mybir.AxisListType

COMPUTE = ("tensor", "vector", "scalar", "gpsimd")
NDMASEM = 16
STRICT = False
NCORES = 8
D = 1024


class Buf:
    __slots__ = ("name", "lw", "rd")

    def __init__(self, name=""):
        self.name = name
        self.lw = None
        self.rd = []


class Sched:
    def __init__(self, nc):
        self.nc = nc
        self.ops = []
        self.stack = ExitStack()
        self.ndma = 0

    def sb(self, name, shape, dt):
        t = self.stack.enter_context(self.nc.sbuf_tensor("s_" + name, shape, dt))
        return t

    def ps(self, name, shape, dt):
        return self.stack.enter_context(self.nc.psum_tensor("p_" + name, shape, dt))

    def _deps(self, r, w):
        d = set()
        raw = set()
        for b in r:
            if b.lw is not None:
                d.add(b.lw)
                raw.add(b.lw)
        for b in w:
            if b.lw is not None:
                d.add(b.lw)
            d.update(b.rd)
        self._raw = set(d) if STRICT else raw
        return d

    def _commit(self, idx, r, w, eng, kind):
        for b in r:
            if kind == "c":
                b.rd = [i for i in b.rd if not (self.ops[i]["kind"] == "c" and self.ops[i]["eng"] == eng)]
            b.rd.append(idx)
        for b in w:
            b.lw = idx
            b.rd = []

    def op(self, eng, fn, r=(), w=()):
        idx = len(self.ops)
        self.ops.append(dict(eng=eng, fn=fn, deps=self._deps(r, w), kind="c", sig=False))
        self.ops[-1]["raw"] = self._raw
        self._commit(idx, r, w, eng, "c")
        return idx

    def dma(self, eng, out, in_, r=(), w=(), **kw):
        idx = len(self.ops)
        k = self.ndma
        self.ndma += 1
        self.ops.append(dict(eng=eng, fn=None, out=out, in_=in_, kw=kw, deps=self._deps(r, w),
                             kind="d", k=k, sig=True))
        self.ops[-1]["raw"] = self._raw
        self._commit(idx, r, w, eng, "d")
        return idx

    def emit(self, final_wait_ops=()):
        nc = self.nc
        ops = self.ops
        for i, o in enumerate(ops):
            for d in o["deps"]:
                od = ops[d]
                if od["kind"] == "c" and (od["eng"] != o["eng"] or o["kind"] == "d"
                                          or (d in o["raw"] and od["eng"] != "tensor")):
                    od["sig"] = True
        cnt = {e: 0 for e in COMPUTE}
        for o in ops:
            if o["kind"] == "c" and o["sig"]:
                cnt[o["eng"]] += 1
                o["sv"] = cnt[o["eng"]]
        NS = 2 * NDMASEM
        dcnt = [0] * NS
        prev_on_sem = [None] * NS
        qcount = {"sync": 0, "gpsimd": 0}
        for i, o in enumerate(ops):
            if o["kind"] == "d":
                qi = qcount[o["eng"]]
                qcount[o["eng"]] += 1
                s = (qi % NDMASEM) + (NDMASEM if o["eng"] == "gpsimd" else 0)
                dcnt[s] += 1
                o["ds"] = s
                o["dv"] = 16 * dcnt[s]
                o["prev"] = prev_on_sem[s]
                prev_on_sem[s] = i
        with ExitStack() as st:
            csem = {e: st.enter_context(nc.semaphore("cs_" + e)) for e in COMPUTE}
            dsem = [st.enter_context(nc.semaphore("ds_%d" % i)) for i in range(2 * NDMASEM)]
            block = st.enter_context(nc.Block())
            engines = ["tensor", "vector", "scalar", "gpsimd", "sync"]
            by_eng = {e: [] for e in engines}
            for i, o in enumerate(ops):
                by_eng[o["eng"]].append(i)
            finals = list(final_wait_ops)

            def make(ename):
                def body(e):
                    waited = {}

                    def wait(key, sem, val):
                        if waited.get(key, 0) >= val:
                            return
                        waited[key] = val
                        e.wait_ge(sem, val)

                    for i in by_eng[ename]:
                        o = ops[i]
                        for d in sorted(o["deps"]):
                            od = ops[d]
                            if od["kind"] == "c":
                                if od["eng"] == ename and o["kind"] == "c" and (
                                        d not in o["raw"] or ename == "tensor"):
                                    continue
                                wait(("c", od["eng"]), csem[od["eng"]], od["sv"])
                            else:
                                wait(("d", od["ds"]), dsem[od["ds"]], od["dv"])
                        if o["kind"] == "d":
                            if o["prev"] is not None:
                                op_ = ops[o["prev"]]
                                wait(("d", op_["ds"]), dsem[op_["ds"]], op_["dv"])
                            ins = e.dma_start(out=o["out"], in_=o["in_"], **o["kw"])
                            ins.then_inc(dsem[o["ds"]], 16)
                        else:
                            ins = o["fn"](e)
                            if o["sig"]:
                                ins.then_inc(csem[ename], 1)
                    if ename == "sync":
                        for i in finals:
                            od = ops[i]
                            wait(("d", od["ds"]), dsem[od["ds"]], od["dv"])
                return body

            block.tensor(make("tensor"))
            block.vector(make("vector"))
            block.scalar(make("scalar"))
            block.gpsimd(make("gpsimd"))
            block.sync(make("sync"))
        self.stack.close()


def new_nc():
    return bass.Bass("TRN2", target_bir_lowering=False)


def dram_in(nc, name, shape, dt=F32):
    return nc.dram_tensor(name, list(shape), dt, kind="ExternalInput").ap()


def dram_out(nc, name, shape, dt=F32):
    return nc.dram_tensor(name, list(shape), dt, kind="ExternalOutput").ap()


def cols(v):
    return np.ascontiguousarray(np.asarray(v, np.float32).reshape(-1, 128).T)


def bcast(v, p=128):
    v = np.asarray(v, np.float32).reshape(1, -1)
    return np.ascontiguousarray(np.broadcast_to(v, (p, v.shape[1])))


_MOD_NC = None


def build_mod():
    nc = new_nc()
    NH = 3072
    cT = dram_in(nc, "cT", [128, 8, 3])
    aw = dram_in(nc, "aw", [1024, NH])
    ab = dram_in(nc, "ab", [3, NH])
    out = dram_out(nc, "mod", [3, NH])
    S = Sched(nc)
    ct = S.sb("ct", [128, 8, 3], F32); bct = Buf()
    sg = S.sb("sg", [128, 8, 3], F32)
    wt = S.sb("wt", [128, 8, NH], F32); bwt = [Buf() for _ in range(8)]
    bt = S.sb("bt", [3, NH], F32); bbt = Buf()
    ot = S.sb("ot", [3, NH], F32); bot = Buf()
    pss = [S.ps("ps%d" % i, [128, 512], F32) for i in range(2)]; bps = [Buf() for _ in range(2)]
    S.dma("sync", ct[:], cT, w=[bct])
    S.dma("sync", bt[:], ab, w=[bbt])
    awv = aw.rearrange("(c p) n -> p c n", p=128)
    for c in range(8):
        S.dma("sync" if c % 2 == 0 else "gpsimd", wt[:, c, :], awv[:, c, :], w=[bwt[c]])
    S.op("scalar", lambda e: e.activation(sg[:], ct[:], AF.Sigmoid), r=[bct], w=[bot])
    S.op("vector", lambda e: e.tensor_tensor(ct[:], ct[:], sg[:], ALU.mult), r=[bot, bct], w=[bct])
    for n in range(NH // 512):
        p = pss[n % 2]; bp = bps[n % 2]
        for c in range(8):
            S.op("tensor", lambda e, p=p, c=c, n=n: e.matmul(p[0:3, :], ct[:, c, :], wt[:, c, n * 512:(n + 1) * 512],
                                                              start=(c == 0), stop=(c == 7)),
                 r=[bct, bwt[c]], w=[bp])
        S.op("vector", lambda e, p=p, n=n: e.tensor_tensor(ot[:, n * 512:(n + 1) * 512], p[0:3, :],
                                                           bt[:, n * 512:(n + 1) * 512], ALU.add),
             r=[bp, bbt], w=[bot])
    o = S.dma("sync", out, ot[:], r=[bot])
    S.emit([o])
    return nc


def run_mod(c, c_ctx, ada_w, ada_b):
    global _MOD_NC
    if _MOD_NC is None:
        _MOD_NC = build_mod()
    cvec = np.stack([c[0], c[1], c_ctx], 0).astype(np.float32)
    cT = np.ascontiguousarray(cvec.reshape(3, 8, 128).transpose(2, 1, 0))
    maps = []
    for k in range(NCORES):
        l, h = k // 2, k % 2
        maps.append(dict(cT=cT, aw=np.ascontiguousarray(ada_w[l][:, h * 3072:(h + 1) * 3072]),
                         ab=bcast(ada_b[l][h * 3072:(h + 1) * 3072], 3)))
    res = run_bass_kernel_spmd(_MOD_NC, maps, core_ids=list(range(NCORES)))
    mods = np.zeros((4, 3, 6144), np.float32)
    for k in range(NCORES):
        l, h = k // 2, k % 2
        mods[l, :, h * 3072:(h + 1) * 3072] = res.results[k]["mod"]
    return mods


NLAT = 4096
NCTX = 64
NTOK = NLAT + NCTX
NEXP = 32
FH = 512


def token_tiles(ntok):
    tiles = []
    o = 0
    while o < ntok:
        p = min(128, ntok - o)
        tiles.append((o, p))
        o += p
    return tiles


def build_moe(final=False, ntok=NTOK, nexp=NEXP, dbg=False):
    nc = new_nc()
    xin = dram_in(nc, "xin", [ntok, D])
    wr_d = dram_in(nc, "wr", [128, 8, 36])
    br_d = dram_in(nc, "br", [128, 36])
    mc_d = dram_in(nc, "mcols", [128, 6, 8])
    gt_d = dram_in(nc, "gtb", [128, 2, D])
    fg_d = dram_in(nc, "fgb", [128, D])
    id_d = dram_in(nc, "ident", [128, 128])
    wg_d = dram_in(nc, "wg", [nexp, D, FH])
    wu_d = dram_in(nc, "wu", [nexp, D, FH])
    wd_d = dram_in(nc, "wd", [nexp, FH, D])
    xout = dram_out(nc, "xout", [ntok, D])
    S = Sched(nc)
    tiles = token_tiles(ntok)
    nt = len(tiles)
    sts = []
    i = 0
    while i < nt:
        j = min(i + 8, nt)
        if nt - j < 4:
            j = nt
        sts.append(list(range(i, j)))
        i = j
    MAXT = max(len(s) for s in sts)
    MAXTOK = MAXT * 128

    ident = S.sb("ident", [128, 128], F32); b_id = Buf()
    wr = S.sb("wr", [128, 8, 36], F32); b_wr = Buf()
    br = S.sb("br", [128, 36], F32); b_br = Buf()
    mc = S.sb("mc", [128, 6, 8], F32); b_mc = Buf()
    gsc = S.sb("gsc", [128, 2, 8], F32); b_gsc = Buf()
    gtb = S.sb("gtb", [128, 2, D], F32); b_gt = Buf()
    fgb = S.sb("fgb", [128, D], F32); b_fg = Buf()
    hTb = S.sb("hTb", [128, 8, MAXTOK], BF16); b_hTb = [Buf() for _ in range(MAXT)]
    acc = S.sb("acc", [128, MAXT, D], F32); b_acc = [Buf() for _ in range(MAXT)]
    coef = S.sb("coef", [128, MAXT, 32], F32); b_coef = [Buf() for _ in range(MAXT)]
    HT = [S.sb("HT%d" % i, [128, 4, 512], BF16) for i in range(2)]; b_HT = [[Buf() for _ in range(4)] for _ in range(2)]
    sgt = [S.sb("sgt%d" % i, [128, 512], F32) for i in range(2)]; b_sg = [Buf() for _ in range(2)]
    wgs = [S.sb("wgs%d" % i, [128, 8, FH], BF16) for i in range(2)]; b_wg = [Buf() for _ in range(2)]
    wus = [S.sb("wus%d" % i, [128, 8, FH], BF16) for i in range(2)]; b_wu = [Buf() for _ in range(2)]
    wds = [S.sb("wds%d" % i, [128, 4, D], BF16) for i in range(2)]; b_wd = [Buf() for _ in range(2)]
    xt = [S.sb("xt%d" % i, [128, D], F32) for i in range(2)]; b_xt = [Buf() for _ in range(2)]
    xs = [S.sb("xs%d" % i, [128, D], F32) for i in range(2)]; b_xs = [Buf() for _ in range(2)]
    hTf = [S.sb("hTf%d" % i, [128, 8, 128], F32) for i in range(2)]; b_hTf = [Buf() for _ in range(2)]
    sm = [S.sb("sm%d" % i, [128, 96], F32) for i in range(2)]; b_sm = [Buf() for _ in range(2)]
    PS = [S.ps("psb%d" % i, [128, 512], F32) for i in range(8)]; b_ps = [Buf() for _ in range(8)]

    S.dma("sync", ident[:], id_d, w=[b_id])
    S.dma("sync", wr[:], wr_d, w=[b_wr])
    S.dma("sync", br[:], br_d, w=[b_br])
    S.dma("sync", mc[:], mc_d, w=[b_mc])
    S.dma("sync", gtb[:], gt_d, w=[b_gt])
    if final:
        S.dma("sync", fgb[:], fg_d, w=[b_fg])
    for v in range(2):
        S.op("vector", lambda e, v=v: e.scalar_tensor_tensor(gsc[:, v, :], mc[:, 1 + 2 * v, :], 1.0, mc[:, 0, :],
                                                              ALU.add, ALU.mult), r=[b_mc], w=[b_gsc])

    wstate = dict(next=0)
    n_items_total = len(sts) * nexp

    def load_weights(k):
        e = k % nexp
        par = k % 2
        S.dma("gpsimd", wgs[par][:], wg_d[e].rearrange("(c p) n -> p c n", p=128), w=[b_wg[par]])
        S.dma("gpsimd", wus[par][:], wu_d[e].rearrange("(c p) n -> p c n", p=128), w=[b_wu[par]])
        S.dma("gpsimd", wds[par][:], wd_d[e].rearrange("(c p) n -> p c n", p=128), w=[b_wd[par]])

    load_weights(0)
    load_weights(1)
    wk = 2

    tcount = 0
    out_ops = []
    for si, st in enumerate(sts):
        ntl = len(st)
        loff = []
        o = 0
        for t in st:
            loff.append(o)
            o += tiles[t][1]
        stok = o
        for li, t in enumerate(st):
            off, P = tiles[t]
            par = tcount % 2
            tcount += 1
            v = 1 if off >= NLAT and ntok > NLAT else 0
            S.dma("sync", xt[par][0:P, :], xin[off:off + P, :], w=[b_xt[par]])
            ss = sm[par][0:P, 0:1]; rstd = sm[par][0:P, 1:2]
            S.op("scalar", lambda e, par=par, P=P, ss=ss: e.activation(xs[par][0:P, :], xt[par][0:P, :], AF.Square, accum_out=ss),
                 r=[b_xt[par]], w=[b_xs[par], b_sm[par]])
            S.op("vector", lambda e, ss=ss, rstd=rstd: e.tensor_scalar(rstd, ss, 1.0 / D, 1e-6, ALU.mult, ALU.add),
                 r=[b_sm[par]], w=[b_sm[par]])
            S.op("scalar", lambda e, rstd=rstd: e.activation(rstd, rstd, AF.Sqrt), r=[b_sm[par]], w=[b_sm[par]])
            S.op("vector", lambda e, rstd=rstd: e.reciprocal(rstd, rstd), r=[b_sm[par]], w=[b_sm[par]])
            S.op("vector", lambda e, par=par, P=P, rstd=rstd: e.tensor_scalar(xs[par][0:P, :], xt[par][0:P, :], rstd, None, ALU.mult),
                 r=[b_xt[par], b_sm[par]], w=[b_xs[par]])
            for c in range(8):
                pb = c // 4
                S.op("tensor", lambda e, par=par, P=P, c=c, pb=pb: e.transpose(PS[pb][:, (c % 4) * 128:(c % 4) * 128 + P],
                                                                                xs[par][0:P, c * 128:(c + 1) * 128], ident[0:P, 0:P]),
                     r=[b_xs[par], b_id], w=[b_ps[pb]])
            for c in range(8):
                pb = c // 4
                S.op("scalar", lambda e, par=par, P=P, c=c, pb=pb, v=v: e.activation(
                    hTf[par][:, c, 0:P], PS[pb][:, (c % 4) * 128:(c % 4) * 128 + P], AF.Identity,
                    bias=mc[:, 2 + 2 * v, c:c + 1], scale=gsc[:, v, c:c + 1]),
                    r=[b_ps[pb], b_mc, b_gsc], w=[b_hTf[par]])
            S.op("vector", lambda e, par=par, P=P, lo=loff[li]: e.tensor_copy(hTb[:, :, lo:lo + P], hTf[par][:, :, 0:P]),
                 r=[b_hTf[par]], w=[b_hTb[li]])
            for c in range(8):
                S.op("tensor", lambda e, par=par, P=P, c=c: e.matmul(PS[2][0:P, 0:36], hTf[par][:, c, 0:P], wr[:, c, :],
                                                                     start=(c == 0), stop=(c == 7)),
                     r=[b_hTf[par], b_wr], w=[b_ps[2]])
            m = sm[par]
            lg = m[0:P, 8:44]; gl = m[0:P, 8:12]
            gmax = m[0:P, 2:3]; ngmax = m[0:P, 3:4]; gsum = m[0:P, 4:5]; pg = m[0:P, 5:6]
            goh = m[0:P, 44:48]; sel = m[0:P, 48:56]; m8 = m[0:P, 56:64]; gex = m[0:P, 64:68]
            t1 = m[0:P, 68:76]; t2 = m[0:P, 76:84]
            dd = m[0:P, 84:85]; e2 = m[0:P, 85:86]; w1 = m[0:P, 86:87]; w2 = m[0:P, 87:88]
            bsm = [b_sm[par]]

            def V(fn, r=(), w=()):
                S.op("vector", fn, r=list(r) + bsm, w=list(w) + bsm)

            V(lambda e, lg=lg, P=P: e.tensor_tensor(lg, PS[2][0:P, 0:36], br[0:P, :], ALU.add), r=[b_ps[2], b_br])
            V(lambda e, gmax=gmax, gl=gl: e.reduce_max(gmax, gl, AX.X))
            V(lambda e, ngmax=ngmax, gmax=gmax: e.tensor_scalar(ngmax, gmax, -1.0, None, ALU.mult))
            S.op("scalar", lambda e, gex=gex, gl=gl, ngmax=ngmax, gsum=gsum: e.activation(gex, gl, AF.Exp, bias=ngmax, scale=1.0, accum_out=gsum),
                 r=bsm, w=bsm)
            V(lambda e, pg=pg, gsum=gsum: e.reciprocal(pg, gsum))
            V(lambda e, goh=goh, gl=gl, gmax=gmax: e.tensor_scalar(goh, gl, gmax, None, ALU.is_ge))
            V(lambda e, sel=sel, m=m, P=P, goh=goh: e.tensor_scalar(sel, m[0:P, 12:20], goh[:, 0:1], None, ALU.mult))
            for g in range(1, 4):
                V(lambda e, sel=sel, m=m, P=P, goh=goh, g=g: e.scalar_tensor_tensor(sel, m[0:P, 12 + 8 * g:20 + 8 * g], goh[:, g:g + 1], sel,
                                                                                     ALU.mult, ALU.add))
            V(lambda e, m8=m8, sel=sel: e.max(m8, sel))
            V(lambda e, dd=dd, m8=m8: e.tensor_tensor(dd, m8[:, 1:2], m8[:, 0:1], ALU.subtract))
            S.op("scalar", lambda e, e2=e2, dd=dd: e.activation(e2, dd, AF.Exp), r=bsm, w=bsm)
            V(lambda e, w1=w1, e2=e2: e.tensor_scalar(w1, e2, 1.0, None, ALU.add))
            V(lambda e, w1=w1: e.reciprocal(w1, w1))
            V(lambda e, w2=w2, e2=e2, w1=w1: e.tensor_tensor(w2, e2, w1, ALU.mult))
            V(lambda e, w1=w1, pg=pg: e.tensor_tensor(w1, w1, pg, ALU.mult))
            V(lambda e, w2=w2, pg=pg: e.tensor_tensor(w2, w2, pg, ALU.mult))
            V(lambda e, t1=t1, sel=sel, m8=m8, w1=w1: e.tensor_scalar(t1, sel, m8[:, 0:1], w1, ALU.is_equal, ALU.mult))
            V(lambda e, t2=t2, sel=sel, m8=m8, w2=w2: e.tensor_scalar(t2, sel, m8[:, 1:2], w2, ALU.is_equal, ALU.mult))
            V(lambda e, t1=t1, t2=t2: e.tensor_tensor(t1, t1, t2, ALU.add))
            for g in range(4):
                V(lambda e, li=li, P=P, g=g, t1=t1, goh=goh: e.tensor_scalar(coef[0:P, li, 8 * g:8 * g + 8], t1, goh[:, g:g + 1], None, ALU.mult),
                  w=[b_coef[li]])
        chunks = []
        li = 0
        while li < ntl:
            lj = li
            n = 0
            while lj < ntl and n + tiles[st[lj]][1] <= 512:
                n += tiles[st[lj]][1]
                lj += 1
            chunks.append((li, lj, loff[li], n))
            li = lj
        items = [(e, q) for e in range(nexp) for q in range(len(chunks))]

        def GU(idx):
            e, q = items[idx]
            l0, l1, to, n = chunks[q]
            kk = si * nexp + e
            par = kk % 2
            hp = idx % 2
            for j in range(4):
                pg_ = PS[(2 * j) % 4]; pu_ = PS[(2 * j) % 4 + 1]
                bg_ = b_ps[(2 * j) % 4]; bu_ = b_ps[(2 * j) % 4 + 1]
                for c in range(8):
                    S.op("tensor", lambda e_, par=par, c=c, j=j, pg_=pg_, to=to, n=n: e_.matmul(
                        pg_[:, 0:n], wgs[par][:, c, j * 128:(j + 1) * 128], hTb[:, c, to:to + n], start=(c == 0), stop=(c == 7)),
                        r=[b_wg[par]] + b_hTb[l0:l1], w=[bg_])
                for c in range(8):
                    S.op("tensor", lambda e_, par=par, c=c, j=j, pu_=pu_, to=to, n=n: e_.matmul(
                        pu_[:, 0:n], wus[par][:, c, j * 128:(j + 1) * 128], hTb[:, c, to:to + n], start=(c == 0), stop=(c == 7)),
                        r=[b_wu[par]] + b_hTb[l0:l1], w=[bu_])
                sp = j % 2
                S.op("scalar", lambda e_, sp=sp, pg_=pg_, n=n: e_.activation(sgt[sp][:, 0:n], pg_[:, 0:n], AF.Silu),
                     r=[bg_], w=[b_sg[sp]])
                S.op("vector", lambda e_, sp=sp, pu_=pu_, n=n, hp=hp, j=j: e_.tensor_tensor(HT[hp][:, j, 0:n], sgt[sp][:, 0:n], pu_[:, 0:n], ALU.mult),
                     r=[b_sg[sp], bu_], w=[b_HT[hp][j]])

        dstate = dict(k=0)

        def DN(idx):
            e, q = items[idx]
            l0, l1, to, n = chunks[q]
            kk = si * nexp + e
            par = kk % 2
            hp = idx % 2
            for li in range(l0, l1):
                P = tiles[st[li]][1]
                lo = loff[li] - to
                for h in range(2):
                    pb = 4 + dstate["k"] % 4
                    dstate["k"] += 1
                    for j in range(4):
                        S.op("tensor", lambda e_, pb=pb, P=P, hp=hp, j=j, lo=lo, par=par, h=h: e_.matmul(
                            PS[pb][0:P, :], HT[hp][:, j, lo:lo + P], wds[par][:, j, h * 512:(h + 1) * 512], start=(j == 0), stop=(j == 3)),
                            r=[b_HT[hp][j], b_wd[par]], w=[b_ps[pb]])
                    if e == 0:
                        S.op("vector", lambda e_, pb=pb, P=P, li=li, h=h, e=e: e_.tensor_scalar(
                            acc[0:P, li, h * 512:(h + 1) * 512], PS[pb][0:P, :], coef[0:P, li, e:e + 1], None, ALU.mult),
                            r=[b_ps[pb], b_coef[li]], w=[b_acc[li]])
                    else:
                        S.op("vector", lambda e_, pb=pb, P=P, li=li, h=h, e=e: e_.scalar_tensor_tensor(
                            acc[0:P, li, h * 512:(h + 1) * 512], PS[pb][0:P, :], coef[0:P, li, e:e + 1],
                            acc[0:P, li, h * 512:(h + 1) * 512], ALU.mult, ALU.add),
                            r=[b_ps[pb], b_coef[li]], w=[b_acc[li]])

        nonlocal_wk = [wk]
        for idx in range(len(items)):
            GU(idx)
            if idx > 0:
                DN(idx - 1)
                e_prev, q_prev = items[idx - 1]
                if q_prev == len(chunks) - 1 and nonlocal_wk[0] < n_items_total:
                    load_weights(nonlocal_wk[0])
                    nonlocal_wk[0] += 1
        DN(len(items) - 1)
        if nonlocal_wk[0] < n_items_total:
            load_weights(nonlocal_wk[0])
            nonlocal_wk[0] += 1
        wk = nonlocal_wk[0]
        for li, t in enumerate(st):
            off, P = tiles[t]
            par = tcount % 2
            tcount += 1
            v = 1 if off >= NLAT and ntok > NLAT else 0
            S.dma("sync", xt[par][0:P, :], xin[off:off + P, :], w=[b_xt[par]])
            S.op("vector", lambda e, P=P, li=li, v=v: e.tensor_tensor(acc[0:P, li, :], acc[0:P, li, :], gtb[0:P, v, :], ALU.mult),
                 r=[b_gt], w=[b_acc[li]])
            S.op("vector", lambda e, P=P, li=li, par=par: e.tensor_tensor(acc[0:P, li, :], acc[0:P, li, :], xt[par][0:P, :], ALU.add),
                 r=[b_xt[par]], w=[b_acc[li]])
            if final:
                ss = sm[par][0:P, 0:1]; rstd = sm[par][0:P, 1:2]
                S.op("scalar", lambda e, par=par, P=P, ss=ss, li=li: e.activation(xs[par][0:P, :], acc[0:P, li, :], AF.Square, accum_out=ss),
                     r=[b_acc[li]], w=[b_xs[par], b_sm[par]])
                S.op("vector", lambda e, ss=ss, rstd=rstd: e.tensor_scalar(rstd, ss, 1.0 / D, 1e-6, ALU.mult, ALU.add),
                     r=[b_sm[par]], w=[b_sm[par]])
                S.op("scalar", lambda e, rstd=rstd: e.activation(rstd, rstd, AF.Sqrt), r=[b_sm[par]], w=[b_sm[par]])
                S.op("vector", lambda e, rstd=rstd: e.reciprocal(rstd, rstd), r=[b_sm[par]], w=[b_sm[par]])
                S.op("vector", lambda e, P=P, li=li, rstd=rstd: e.scalar_tensor_tensor(acc[0:P, li, :], acc[0:P, li, :], rstd, fgb[0:P, :],
                                                                                      ALU.mult, ALU.mult),
                     r=[b_sm[par], b_fg], w=[b_acc[li]])
            out_ops.append(S.dma("sync", xout[off:off + P, :], acc[0:P, li, :], r=[b_acc[li]], w=[]))
    if dbg:
        d_sm = dram_out(nc, "d_sm", [128, 96]); d_coef = dram_out(nc, "d_coef", [128, 32])
        d_hTf = dram_out(nc, "d_hTf", [128, 8, 128]); d_hTb = dram_out(nc, "d_hTb", [128, 8, MAXTOK], BF16)
        d_HT = dram_out(nc, "d_HT", [128, 4, 512], BF16); d_wg = dram_out(nc, "d_wg", [128, 8, FH], BF16)
        out_ops.append(S.dma("sync", d_sm, sm[0][:], r=[b_sm[0]]))
        out_ops.append(S.dma("sync", d_coef, coef[:, 0, :], r=[b_coef[0]]))
        out_ops.append(S.dma("sync", d_hTf, hTf[0][:], r=[b_hTf[0]]))
        out_ops.append(S.dma("sync", d_hTb, hTb[:], r=b_hTb))
        out_ops.append(S.dma("sync", d_HT, HT[0][:], r=b_HT[0]))
        out_ops.append(S.dma("sync", d_wg, wgs[0][:], r=[b_wg[0]]))
    S.emit(out_ops)
    return nc


_MOE_NC = {}


def moe_maps(xs_per_core, i, mods, inp, nexp=NEXP):
    wr = np.concatenate([inp["moe_w_grp"][i], inp["moe_w_exp"][i]], axis=1)
    wr = np.ascontiguousarray(wr.reshape(8, 128, 36).transpose(1, 0, 2))
    br = bcast(np.concatenate([inp["moe_b_grp"][i], inp["moe_b_exp"][i]]))
    ident = np.eye(128, dtype=np.float32)
    fgb = bcast(inp["final_g"])
    wg = np.ascontiguousarray(inp["moe_w_gate"][i][:nexp])
    wu = np.ascontiguousarray(inp["moe_w_up"][i][:nexp])
    wd = np.ascontiguousarray(inp["moe_w_down"][i][:nexp])
    maps = []
    for k in range(NCORES):
        b = k // 4
        ml = mods[i, b]
        mcx = mods[i, 2]
        mc = np.stack([cols(inp["norm2_g"][i]), cols(ml[4 * D:5 * D]), cols(ml[3 * D:4 * D]),
                       cols(mcx[4 * D:5 * D]), cols(mcx[3 * D:4 * D]), np.zeros((128, 8), np.float32)], axis=1)
        gtb = np.stack([bcast(ml[5 * D:6 * D]), bcast(mcx[5 * D:6 * D])], axis=1)
        maps.append(dict(xin=np.ascontiguousarray(xs_per_core[k]), wr=wr, br=br, mcols=np.ascontiguousarray(mc),
                         gtb=np.ascontiguousarray(gtb), fgb=fgb, ident=ident, wg=wg, wu=wu, wd=wd))
    return maps


def run_moe(xs_per_core, i, mods, inp, final=False, ntok=NTOK, nexp=NEXP, trace=False, dbg=False):
    key = (final, ntok, nexp, dbg)
    if key not in _MOE_NC:
        _MOE_NC[key] = build_moe(final=final, ntok=ntok, nexp=nexp, dbg=dbg)
    maps = moe_maps(xs_per_core, i, mods, inp, nexp)
    res = run_bass_kernel_spmd(_MOE_NC[key], maps, core_ids=list(range(NCORES)), trace=trace)
    if trace:
        print("moe exec_time_ns", res.exec_time_ns)
    if dbg:
        return res.results
    return [r["xout"] for r in res.results]


GW = 2048


def build_gmlp(nch=33, ctx_last=True):
    nc = new_nc()
    ntok = nch * 128
    xin = dram_in(nc, "xin", [ntok, D])
    mc_d = dram_in(nc, "mcols", [128, 5, 8])
    gt_d = dram_in(nc, "gtb", [128, 2, D])
    bo_d = dram_in(nc, "boutb", [128, D])
    win_d = dram_in(nc, "w_in", [D, 2 * GW])
    bu_d = dram_in(nc, "b_ucol", [128, 16])
    bv_d = dram_in(nc, "b_vrow", [1, GW])
    lg_d = dram_in(nc, "lngb", [128, GW])
    lb_d = dram_in(nc, "lnbb", [128, GW])
    ws_d = dram_in(nc, "wsT", [128, 16, 128])
    bs_d = dram_in(nc, "bsrow", [1, 16, 128])
    wo_d = dram_in(nc, "w_out", [GW, D])
    id_d = dram_in(nc, "ident", [128, 128])
    xout = dram_out(nc, "xout", [ntok, D])
    S = Sched(nc)
    GT = 256
    ident = S.sb("ident", [128, 128], F32); b_id = Buf()
    mc = S.sb("mc", [128, 5, 8], F32); b_mc = Buf()
    gsc = S.sb("gsc", [128, 2, 8], F32); b_gsc = Buf()
    gtb = S.sb("gtb", [128, 2, D], F32); b_gt = Buf()
    bob = S.sb("bob", [128, D], F32); b_bo = Buf()
    win = S.sb("win", [128, 8, 2 * GW], BF16); b_win = Buf()
    bu = S.sb("bu", [128, 16], F32); b_bu = Buf()
    bvr = S.sb("bvr", [1, GW], BF16); b_bv = Buf()
    lgb = S.sb("lgb", [128, GW], F32); b_lg = Buf()
    lbb = S.sb("lbb", [128, GW], F32); b_lb = Buf()
    wsT = S.sb("wsT", [128, 16, 128], BF16); b_ws = Buf()
    bsr = S.sb("bsr", [1, 16, 128], BF16); b_bs = Buf()
    wo = S.sb("wo", [128, 16, D], BF16); b_wo = Buf()
    ones = S.sb("ones", [1, 128], BF16); b_ones = Buf()
    hT = [S.sb("hT%d" % i, [128, 8, GT], BF16) for i in range(2)]; b_hT = [Buf() for _ in range(2)]
    uT = S.sb("uT", [128, 16, GT], BF16); b_uT = [Buf() for _ in range(16)]
    vv = [S.sb("vv%d" % i, [128, GW], F32) for i in range(2)]; b_vv = [Buf() for _ in range(2)]
    vn = [S.sb("vn%d" % i, [128, GW], BF16) for i in range(2)]; b_vn = [Buf() for _ in range(2)]
    prod = S.sb("prod", [128, 16, GT], BF16); b_prod = [[Buf() for _ in range(4)] for _ in range(2)]
    xt = [S.sb("xt%d" % i, [128, D], F32) for i in range(2)]; b_xt = [Buf() for _ in range(2)]
    _xs = S.sb("xs0", [128, D], F32); _bxs = Buf()
    xs = [_xs, _xs]; b_xs = [_bxs, _bxs]
    ot = [S.sb("ot%d" % i, [128, D], F32) for i in range(2)]; b_ot = [Buf() for _ in range(2)]
    sm = [S.sb("sm%d" % i, [128, 8], F32) for i in range(2)]; b_sm = [Buf() for _ in range(2)]
    PS = [S.ps("psb%d" % i, [128, 512], F32) for i in range(8)]; b_ps = [Buf() for _ in range(8)]

    S.dma("sync", ident[:], id_d, w=[b_id])
    S.dma("sync", mc[:], mc_d, w=[b_mc])
    S.dma("sync", gtb[:], gt_d, w=[b_gt])
    S.dma("sync", bob[:], bo_d, w=[b_bo])
    S.dma("sync", bu[:], bu_d, w=[b_bu])
    S.dma("sync", lgb[:], lg_d, w=[b_lg])
    S.dma("sync", lbb[:], lb_d, w=[b_lb])
    S.dma("gpsimd", bvr[:], bv_d, w=[b_bv])
    S.dma("gpsimd", bsr[:], bs_d, w=[b_bs])
    S.dma("gpsimd", wsT[:], ws_d, w=[b_ws])
    winv = win_d.rearrange("(c p) n -> p c n", p=128)
    for n in range(4):
        S.dma("gpsimd", win[:, :, n * 1024:(n + 1) * 1024], winv[:, :, n * 1024:(n + 1) * 1024], w=[b_win])
    S.dma("gpsimd", wo[:], wo_d.rearrange("(c p) n -> p c n", p=128), w=[b_wo])
    S.op("vector", lambda e: e.memset(ones[:], 1.0), w=[b_ones])
    for v in range(2):
        S.op("vector", lambda e, v=v: e.scalar_tensor_tensor(gsc[:, v, :], mc[:, 1 + 2 * v, :], 1.0, mc[:, 0, :],
                                                              ALU.add, ALU.mult), r=[b_mc], w=[b_gsc])
    out_ops = []
    groups = []
    ci = 0
    while ci < nch:
        g2 = min(ci + 2, nch)
        groups.append(list(range(ci, g2)))
        ci = g2
    tcount = 0
    vcount = 0
    for gi, grp in enumerate(groups):
        ng = len(grp)
        gt_ = ng * 128
        hp = gi % 2
        xpar = {}
        for li, ch in enumerate(grp):
            par = tcount % 2
            tcount += 1
            xpar[ch] = par
            v = 1 if (ctx_last and ch == nch - 1) else 0
            off = ch * 128
            S.dma("sync", xt[par][:], xin[off:off + 128, :], w=[b_xt[par]])
            ss = sm[par][:, 0:1]; rstd = sm[par][:, 1:2]
            S.op("scalar", lambda e, par=par, ss=ss: e.activation(xs[par][:], xt[par][:], AF.Square, accum_out=ss),
                 r=[b_xt[par]], w=[b_xs[par], b_sm[par]])
            S.op("vector", lambda e, ss=ss, rstd=rstd: e.tensor_scalar(rstd, ss, 1.0 / D, 1e-6, ALU.mult, ALU.add),
                 r=[b_sm[par]], w=[b_sm[par]])
            S.op("scalar", lambda e, rstd=rstd: e.activation(rstd, rstd, AF.Sqrt), r=[b_sm[par]], w=[b_sm[par]])
            S.op("vector", lambda e, rstd=rstd: e.reciprocal(rstd, rstd), r=[b_sm[par]], w=[b_sm[par]])
            S.op("vector", lambda e, par=par, rstd=rstd: e.tensor_scalar(xs[par][:], xt[par][:], rstd, None, ALU.mult),
                 r=[b_xt[par], b_sm[par]], w=[b_xs[par]])
            for c in range(8):
                pb = c // 4
                S.op("tensor", lambda e, par=par, c=c, pb=pb: e.transpose(PS[pb][:, (c % 4) * 128:(c % 4) * 128 + 128],
                                                                          xs[par][:, c * 128:(c + 1) * 128], ident[:]),
                     r=[b_xs[par], b_id], w=[b_ps[pb]])
            for c in range(8):
                pb = c // 4
                S.op("scalar", lambda e, hp=hp, c=c, pb=pb, v=v, li=li: e.activation(
                    hT[hp][:, c, li * 128:(li + 1) * 128], PS[pb][:, (c % 4) * 128:(c % 4) * 128 + 128], AF.Identity,
                    bias=mc[:, 2 + 2 * v, c:c + 1], scale=gsc[:, v, c:c + 1]),
                    r=[b_ps[pb], b_mc, b_gsc], w=[b_hT[hp]])
        for m in range(16):
            pb = 2 + m % 2
            for c in range(8):
                S.op("tensor", lambda e, pb=pb, c=c, m=m, hp=hp, gt_=gt_: e.matmul(
                    PS[pb][:, 0:gt_], win[:, c, m * 128:(m + 1) * 128], hT[hp][:, c, 0:gt_], start=(c == 0), stop=(c == 7)),
                    r=[b_win, b_hT[hp]], w=[b_ps[pb]])
            S.op("scalar", lambda e, pb=pb, m=m, gt_=gt_: e.activation(uT[:, m, 0:gt_], PS[pb][:, 0:gt_], AF.Gelu,
                                                                       bias=bu[:, m:m + 1], scale=1.0),
                 r=[b_ps[pb], b_bu], w=[b_uT[m]])
        vps = {}
        for li, ch in enumerate(grp):
            vp = vcount % 2
            vcount += 1
            vps[ch] = vp
            v = 1 if (ctx_last and ch == nch - 1) else 0
            par = xpar[ch]
            off = ch * 128
            tk = slice(li * 128, (li + 1) * 128)
            for n in range(4):
                pb = 4 + n % 2
                for c in range(8):
                    S.op("tensor", lambda e, pb=pb, c=c, n=n, hp=hp, tk=tk: e.matmul(
                        PS[pb][:, :], hT[hp][:, c, tk], win[:, c, GW + n * 512:GW + (n + 1) * 512], start=(c == 0), stop=False),
                        r=[b_win, b_hT[hp]], w=[b_ps[pb]])
                S.op("tensor", lambda e, pb=pb, n=n: e.matmul(PS[pb][:, :], ones[0:1, :], bvr[0:1, n * 512:(n + 1) * 512],
                                                              start=False, stop=True),
                     r=[b_ones, b_bv], w=[b_ps[pb]])
                S.op("scalar", lambda e, pb=pb, n=n, vp=vp: e.activation(vv[vp][:, n * 512:(n + 1) * 512], PS[pb][:, :], AF.Gelu),
                     r=[b_ps[pb]], w=[b_vv[vp]])
            s1 = sm[par][:, 2:3]; nm = sm[par][:, 3:4]; sq = sm[par][:, 4:5]; r2 = sm[par][:, 5:6]
            S.op("vector", lambda e, s1=s1, vp=vp: e.reduce_sum(s1, vv[vp][:], AX.X), r=[b_vv[vp]], w=[b_sm[par]])
            S.op("vector", lambda e, s1=s1, nm=nm: e.tensor_scalar(nm, s1, -1.0 / GW, None, ALU.mult), r=[b_sm[par]], w=[b_sm[par]])
            S.op("scalar", lambda e, vp=vp, nm=nm, sq=sq: e.activation(vn[vp][:], vv[vp][:], AF.Square, bias=nm, scale=1.0, accum_out=sq),
                 r=[b_vv[vp], b_sm[par]], w=[b_vn[vp], b_sm[par]])
            S.op("vector", lambda e, sq=sq, r2=r2: e.tensor_scalar(r2, sq, 1.0 / GW, 1e-5, ALU.mult, ALU.add), r=[b_sm[par]], w=[b_sm[par]])
            S.op("scalar", lambda e, r2=r2: e.activation(r2, r2, AF.Sqrt), r=[b_sm[par]], w=[b_sm[par]])
            S.op("vector", lambda e, r2=r2: e.reciprocal(r2, r2), r=[b_sm[par]], w=[b_sm[par]])
            S.op("vector", lambda e, vp=vp, nm=nm, r2=r2: e.tensor_scalar(vv[vp][:], vv[vp][:], nm, r2, ALU.add, ALU.mult),
                 r=[b_sm[par]], w=[b_vv[vp]])
            S.op("vector", lambda e, vp=vp: e.tensor_tensor(vv[vp][:], vv[vp][:], lgb[:], ALU.mult), r=[b_lg], w=[b_vv[vp]])
            S.op("gpsimd", lambda e, vp=vp: e.tensor_tensor(vn[vp][:], vv[vp][:], lbb[:], ALU.add), r=[b_vv[vp], b_lb], w=[b_vn[vp]])
        for li, ch in enumerate(grp):
            vp = vps[ch]
            v = 1 if (ctx_last and ch == nch - 1) else 0
            par = xpar[ch]
            off = ch * 128
            tk = slice(li * 128, (li + 1) * 128)
            for g4 in range(4):
                pb = 6 + g4 % 2
                for gg in range(4):
                    g = g4 * 4 + gg
                    S.op("tensor", lambda e, pb=pb, gg=gg, g=g, vp=vp: e.matmul(
                        PS[pb][:, gg * 128:(gg + 1) * 128], vn[vp][:, g * 128:(g + 1) * 128], wsT[:, g, :], start=True, stop=False),
                        r=[b_vn[vp], b_ws], w=[b_ps[pb]])
                    S.op("tensor", lambda e, pb=pb, gg=gg, g=g: e.matmul(
                        PS[pb][:, gg * 128:(gg + 1) * 128], ones[0:1, :], bsr[0:1, g, :], start=False, stop=True),
                        r=[b_ones, b_bs], w=[b_ps[pb]])
                S.op("vector", lambda e, pb=pb, g4=g4, tk=tk: e.tensor_tensor(
                    prod[:, g4 * 4:(g4 + 1) * 4, tk], PS[pb][:, :].rearrange("p (g t) -> p g t", g=4), uT[:, g4 * 4:(g4 + 1) * 4, tk], ALU.mult),
                    r=[b_ps[pb]] + b_uT[g4 * 4:(g4 + 1) * 4], w=[b_prod[li][g4]])
            op_ = tcount % 2
            for n in range(2):
                pb = n
                for k in range(16):
                    S.op("tensor", lambda e, pb=pb, k=k, n=n, tk=tk: e.matmul(
                        PS[pb][:, :], prod[:, k, tk], wo[:, k, n * 512:(n + 1) * 512], start=(k == 0), stop=(k == 15)),
                        r=b_prod[li] + [b_wo], w=[b_ps[pb]])
                sl = slice(n * 512, (n + 1) * 512)
                S.op("vector", lambda e, pb=pb, par=par, sl=sl: e.tensor_tensor(ot[par][:, sl], PS[pb][:, :], bob[:, sl], ALU.add),
                     r=[b_ps[pb], b_bo], w=[b_ot[par]])
            S.op("gpsimd", lambda e, par=par, v=v: e.tensor_tensor(ot[par][:], ot[par][:], gtb[:, v, :], ALU.mult),
                 r=[b_gt], w=[b_ot[par]])
            S.op("gpsimd", lambda e, par=par: e.tensor_tensor(ot[par][:], ot[par][:], xt[par][:], ALU.add),
                 r=[b_xt[par]], w=[b_ot[par]])
            out_ops.append(S.dma("sync", xout[off:off + 128, :], ot[par][:], r=[b_ot[par]]))
    S.emit(out_ops)
    return nc


_GMLP_NC = {}


def run_gmlp(xs_per_core, i, mods, inp, nch=33, trace=False):
    j = i // 2
    key = nch
    if key not in _GMLP_NC:
        _GMLP_NC[key] = build_gmlp(nch=nch)
    b_in = inp["ga_b_in"][j]
    wsT = np.ascontiguousarray(inp["ga_w_s"][j].transpose(2, 0, 1))
    base = dict(boutb=bcast(inp["ga_b_out"][j]), w_in=np.ascontiguousarray(inp["ga_w_in"][j]),
                b_ucol=np.ascontiguousarray(b_in[:GW].reshape(16, 128).T), b_vrow=np.ascontiguousarray(b_in[GW:].reshape(1, GW)),
                lngb=bcast(inp["ga_ln_g"][j]), lnbb=bcast(inp["ga_ln_b"][j]), wsT=wsT,
                bsrow=np.ascontiguousarray(inp["ga_b_s"][j].reshape(1, 16, 128)), w_out=np.ascontiguousarray(inp["ga_w_out"][j]),
                ident=np.eye(128, dtype=np.float32))
    maps = []
    for k in range(NCORES):
        b = k // 4
        ml = mods[i, b]; mcx = mods[i, 2]
        mc = np.stack([cols(inp["norm1_g"][i]), cols(ml[1 * D:2 * D]), cols(ml[0:D]), cols(mcx[1 * D:2 * D]), cols(mcx[0:D])], axis=1)
        gtb = np.stack([bcast(ml[2 * D:3 * D]), bcast(mcx[2 * D:3 * D])], axis=1)
        m = dict(base)
        m.update(xin=np.ascontiguousarray(xs_per_core[k]), mcols=np.ascontiguousarray(mc), gtb=np.ascontiguousarray(gtb))
        maps.append(m)
    res = run_bass_kernel_spmd(_GMLP_NC[key], maps, core_ids=list(range(NCORES)), trace=trace)
    if trace:
        print("gmlp exec_time_ns", res.exec_time_ns)
    return [r["xout"] for r in res.results]


PBLK = 256
NEG_E05 = -0.6065306597126334


def build_prep(nlat=NLAT, with_ctx=True, stop=None):
    nc = new_nc()
    nrow = nlat + 128 + (256 if with_ctx else 0)
    TP = nlat + (256 if with_ctx else 0)
    xin = dram_in(nc, "xin", [nrow, D])
    edge_d = dram_in(nc, "edge", [128, 2])
    mc_d = dram_in(nc, "mcols", [128, 5, 8])
    mu_d = dram_in(nc, "mu", [128, 6, 8])
    vc_d = dram_in(nc, "vcols", [128, 8, 8])
    wr_d = dram_in(nc, "wr", [D, D]); wk_d = dram_in(nc, "wk", [D, D]); wv_d = dram_in(nc, "wv", [D, D])
    w1_d = dram_in(nc, "w1", [2, D, 64]); w2_d = dram_in(nc, "w2", [2, 64, D])
    a1_d = dram_in(nc, "a1", [2, D, 64]); a2_d = dram_in(nc, "a2", [2, 64, D])
    g1_d = dram_in(nc, "g1", [D, 160]); g2_d = dram_in(nc, "g2", [160, D])
    id_d = dram_in(nc, "ident", [128, 128]); blk_d = dram_in(nc, "blk", [128, 128]); cm_d = dram_in(nc, "cmask", [128, PBLK])
    ob = dram_out(nc, "ob", [9, 8, 128, TP], BF16)
    of = dram_out(nc, "of", [2, 8, 128, TP])
    pc = dram_out(nc, "pc", [2, 8, 128, TP // 64])
    S = Sched(nc)
    ident = S.sb("ident", [128, 128], F32); b_id = Buf()
    blk = S.sb("blk", [128, 128], BF16); b_blk = Buf()
    cmask = S.sb("cmask", [128, PBLK], F32); b_cm = Buf()
    edge = S.sb("edge", [128, 2], F32); b_edge = Buf()
    mc = S.sb("mc", [128, 5, 8], F32); b_mc = Buf()
    gsc = S.sb("gsc", [128, 2, 8], F32); b_gsc = Buf()
    mu = S.sb("mu", [128, 6, 8], F32); b_mu = Buf()
    vc = S.sb("vc", [128, 8, 8], F32); b_vc = Buf()
    epsc = S.sb("epsc", [128, 1], F32); b_eps = Buf()
    wr = S.sb("wr", [128, 8, D], BF16); wk = S.sb("wk", [128, 8, D], BF16); wv = S.sb("wv", [128, 8, D], BF16)
    b_wr = Buf(); b_wk = Buf(); b_wv = Buf()
    w1 = S.sb("w1", [128, 2, 8, 64], BF16); a1 = S.sb("a1", [128, 2, 8, 64], BF16); b_w1 = Buf(); b_a1 = Buf()
    w2 = S.sb("w2", [64, 2, D], BF16); a2 = S.sb("a2", [64, 2, D], BF16); b_w2 = Buf(); b_a2 = Buf()
    g1 = S.sb("g1", [128, 8, 160], BF16); b_g1 = Buf()
    g2a = S.sb("g2a", [128, D], BF16); g2b = S.sb("g2b", [32, D], BF16); b_g2 = Buf()
    HL = S.sb("HL", [128, 8, PBLK + 128], F32); b_HL = [Buf() for _ in range(8)]
    XX = S.sb("XX", [128, 8, PBLK], F32); b_XX = [Buf() for _ in range(8)]
    XM = [S.sb("XM%d" % j, [128, 8, PBLK], BF16) for j in range(6)]; b_XM = [[Buf() for _ in range(8)] for _ in range(6)]
    R32 = S.sb("R32", [128, 8, PBLK], F32); K32 = S.sb("K32", [128, 8, PBLK], F32); V32 = S.sb("V32", [128, 8, PBLK], F32)
    b_R = [Buf() for _ in range(8)]; b_K = [Buf() for _ in range(8)]; b_V = [Buf() for _ in range(8)]
    TW = [S.sb("TW%d" % z, [64, PBLK], BF16) for z in range(2)]; b_TW = [Buf() for _ in range(2)]
    AL = [S.sb("AL%d" % z, [64, PBLK], BF16) for z in range(2)]; b_AL = [Buf() for _ in range(2)]
    SGa = S.sb("SGa", [128, PBLK], BF16); SGb = S.sb("SGb", [32, PBLK], BF16); b_SG = Buf()
    xt = [S.sb("xt%d" % i, [128, D], F32) for i in range(2)]; b_xt = [Buf() for _ in range(2)]
    xs = S.sb("xs", [128, D], F32); b_xs = Buf()
    sm = [S.sb("sm%d" % i, [128, 4], F32) for i in range(2)]; b_sm = [Buf() for _ in range(2)]

    def tmp(name, dt=F32):
        return S.sb("t_" + name, [128, PBLK], dt), Buf()
    LW2 = [[tmp("LW%d_%d" % (z, i)) for z in range(2)] for i in range(2)]; AZ2 = [[tmp("AZ%d_%d" % (z, i)) for z in range(2)] for i in range(2)]
    FF = [tmp("FF%d" % z) for z in range(2)]; CE = [tmp("CE%d" % z) for z in range(2)]
    CC = [tmp("CC%d" % z) for z in range(2)]
    Ec2 = [[tmp("Ec%d_%d" % (z, i)) for z in range(2)] for i in range(2)]; Ee2 = [[tmp("Ee%d_%d" % (z, i)) for z in range(2)] for i in range(2)]
    En2 = [[tmp("En%d_%d" % (z, i)) for z in range(2)] for i in range(2)]
    KR = tmp("KR"); SQ = tmp("SQ", BF16); RN = tmp("RN"); KK2 = [tmp("KK_%d" % i) for i in range(2)]; T1 = tmp("T1")
    KM2 = [[tmp("KM%d_%d" % (z, i)) for z in range(2)] for i in range(2)]; BT = tmp("BT"); KS = tmp("KS"); PB = tmp("PB", BF16)
    GO = [tmp("GO%d" % i) for i in range(2)]; BV = [tmp("BV%d" % i) for i in range(2)]
    OB = [[tmp("OB%d_%d" % (k, i), BF16) for k in range(9)] for i in range(2)]
    PCt = [S.sb("PCt%d" % i, [128, 2, PBLK // 64], F32) for i in range(2)]; b_PC = [Buf() for _ in range(2)]
    PS = [S.ps("psb%d" % i, [128, 512], F32) for i in range(8)]
    _bb = [Buf() for _ in range(8)]
    b_ps = [[b, b] for b in _bb]

    for t_, d_, b_ in [(ident, id_d, b_id), (cmask, cm_d, b_cm), (edge, edge_d, b_edge), (mc, mc_d, b_mc), (mu, mu_d, b_mu), (vc, vc_d, b_vc)]:
        S.dma("sync", t_[:], d_, w=[b_])
    S.dma("gpsimd", blk[:], blk_d, w=[b_blk])
    for t_, d_, b_ in [(wr, wr_d, b_wr), (wk, wk_d, b_wk), (wv, wv_d, b_wv)]:
        S.dma("gpsimd", t_[:], d_.rearrange("(c p) n -> p c n", p=128), w=[b_])
    for z in range(2):
        S.dma("gpsimd", w1[:, z, :, :], w1_d[z].rearrange("(c p) n -> p c n", p=128), w=[b_w1])
        S.dma("gpsimd", a1[:, z, :, :], a1_d[z].rearrange("(c p) n -> p c n", p=128), w=[b_a1])
        S.dma("gpsimd", w2[:, z, :], w2_d[z], w=[b_w2])
        S.dma("gpsimd", a2[:, z, :], a2_d[z], w=[b_a2])
    S.dma("gpsimd", g1[:], g1_d.rearrange("(c p) n -> p c n", p=128), w=[b_g1])
    S.dma("gpsimd", g2a[:], g2_d[0:128, :], w=[b_g2])
    S.dma("gpsimd", g2b[:], g2_d[128:160, :], w=[b_g2])
    S.op("vector", lambda e: e.memset(epsc[:], 1e-12), w=[b_eps])
    for v in range(2):
        S.op("vector", lambda e, v=v: e.scalar_tensor_tensor(gsc[:, v, :], mc[:, 1 + 2 * v, :], 1.0, mc[:, 0, :],
                                                              ALU.add, ALU.mult), r=[b_mc], w=[b_gsc])
    out_ops = []
    def E_body(co, op_, tok0, isctx, LW, AZ, Ec, Ee, En, KM, KK):
        cs = slice(co * 128, (co + 1) * 128)
        for z in range(2):
            reg = PS[4][:, z * 256:z * 256 + PBLK]
            S.op("tensor", lambda e, reg=reg, z=z, cs=cs: e.matmul(reg, w2[0:64, z, cs], TW[z][0:64, :], start=True, stop=True),
                 r=[b_w2, b_TW[z]], w=[b_ps[4][z]])
            S.op("scalar", lambda e, reg=reg, z=z, co=co: e.activation(LW[z][0][:], reg, AF.Sigmoid, bias=vc[:, z, co:co + 1], scale=1.0),
                 r=[b_ps[4][z], b_vc], w=[LW[z][1]])
            S.op("vector", lambda e, z=z: e.tensor_scalar(LW[z][0][:], LW[z][0][:], NEG_E05, None, ALU.mult), r=[], w=[LW[z][1]])
            reg2 = PS[5][:, z * 256:z * 256 + PBLK]
            S.op("tensor", lambda e, reg2=reg2, z=z, cs=cs: e.matmul(reg2, a2[0:64, z, cs], AL[z][0:64, :], start=True, stop=True),
                 r=[b_a2, b_AL[z]], w=[b_ps[5][z]])
            S.op("scalar", lambda e, reg2=reg2, z=z, co=co: e.activation(AZ[z][0][:], reg2, AF.Sigmoid, bias=vc[:, 2 + z, co:co + 1], scale=1.0),
                 r=[b_ps[5][z], b_vc], w=[AZ[z][1]])
        regg = PS[6][:, 0:PBLK]
        S.op("tensor", lambda e, cs=cs: e.matmul(regg, g2a[:, cs], SGa[:], start=True, stop=False), r=[b_g2, b_SG], w=[b_ps[6][0]])
        S.op("tensor", lambda e, cs=cs: e.matmul(regg, g2b[0:32, cs], SGb[0:32, :], start=False, stop=True), r=[b_g2, b_SG], w=[b_ps[6][0]])
        S.op("scalar", lambda e, op_=op_: e.copy(GO[op_][0][:], regg), r=[b_ps[6][0]], w=[GO[op_][1]])
        out_ops.append(S.dma("sync", of[1, co, :, tok0:tok0 + PBLK], GO[op_][0][:], r=[GO[op_][1]]))
        if stop == 'E1':
            return
        yield
        for z in range(2):
            S.op("vector", lambda e, z=z: e.tensor_tensor_scan(FF[z][0][:], cmask[:], LW[z][0][:], 0.0, ALU.mult, ALU.add),
                 r=[b_cm, LW[z][1]], w=[FF[z][1]])
        S.op("vector", lambda e: e.tensor_tensor(CE[0][0][:], FF[0][0][:], LW[0][0][:], ALU.subtract), r=[FF[0][1], LW[0][1]], w=[CE[0][1]])
        f3 = FF[1][0][:].rearrange("p (a b) -> p a b", b=64)
        S.op("vector", lambda e, f3=f3: e.tensor_tensor(CE[1][0][:].rearrange("p (a b) -> p a b", b=64),
                                                        f3[:, :, 63:64].to_broadcast([128, PBLK // 64, 64]), f3, ALU.subtract),
             r=[FF[1][1]], w=[CE[1][1]])
        S.op("vector", lambda e: e.tensor_tensor(CC[1][0][:], CE[1][0][:], LW[1][0][:], ALU.add), r=[CE[1][1], LW[1][1]], w=[CC[1][1]])
        csrc = [FF[0], CC[1]]
        for z in range(2):
            S.op("scalar", lambda e, z=z: e.activation(Ec[z][0][:], csrc[z][0][:], AF.Exp), r=[csrc[z][1]], w=[Ec[z][1]])
            S.op("scalar", lambda e, z=z: e.activation(En[z][0][:], csrc[z][0][:], AF.Exp, scale=-1.0), r=[csrc[z][1]], w=[En[z][1]])
            S.op("scalar", lambda e, z=z: e.activation(Ee[z][0][:], CE[z][0][:], AF.Exp), r=[CE[z][1]], w=[Ee[z][1]])
        S.op("vector", lambda e, op_=op_: e.tensor_copy(PCt[op_][:, 0, :], Ec[0][0][:].rearrange("p (a b) -> p a b", b=64)[:, :, 63]),
             r=[Ec[0][1]], w=[b_PC[op_]])
        S.op("vector", lambda e, op_=op_: e.tensor_copy(PCt[op_][:, 1, :], Ec[1][0][:].rearrange("p (a b) -> p a b", b=64)[:, :, 0]),
             r=[Ec[1][1]], w=[b_PC[op_]])
        ch0 = tok0 // 64
        for z in range(2):
            out_ops.append(S.dma("sync", pc[z, co, :, ch0:ch0 + PBLK // 64], PCt[op_][:, z, :], r=[b_PC[op_]]))
        if stop == 'E2':
            return
        yield
        S.op("vector", lambda e, co=co: e.tensor_scalar(KR[0][:], K32[:, co, :], vc[:, 4, co:co + 1], None, ALU.mult),
             r=[b_K[co], b_vc], w=[KR[1]])
        S.op("scalar", lambda e: e.activation(SQ[0][:], KR[0][:], AF.Square), r=[KR[1]], w=[SQ[1]])
        regk = PS[6][:, 256:256 + PBLK]
        S.op("tensor", lambda e: e.matmul(regk, blk[:], SQ[0][:], start=True, stop=True), r=[b_blk, SQ[1]], w=[b_ps[6][1]])
        S.op("scalar", lambda e: e.activation(RN[0][:], regk, AF.Sqrt, bias=epsc[:, 0:1], scale=1.0), r=[b_ps[6][1], b_eps], w=[RN[1]])
        S.op("vector", lambda e: e.reciprocal(RN[0][:], RN[0][:]), r=[RN[1]], w=[RN[1]])
        S.op("vector", lambda e: e.tensor_tensor(KK[0][:], KR[0][:], RN[0][:], ALU.mult), r=[KR[1], RN[1]], w=[KK[1]])
        if stop == 'E3':
            return
        yield
        for z in range(2):
            S.op("vector", lambda e, z=z, co=co: e.tensor_scalar(T1[0][:], AZ[z][0][:], -1.0, vc[:, 5, co:co + 1], ALU.add, ALU.mult),
                 r=[AZ[z][1], b_vc], w=[T1[1]])
            S.op("vector", lambda e, z=z, co=co: e.scalar_tensor_tensor(KM[z][0][:], T1[0][:], 1.0, K32[:, co, :], ALU.add, ALU.mult),
                 r=[T1[1], b_K[co]], w=[KM[z][1]])
        yield
        O = OB[op_]
        for z in range(2):
            S.op("vector", lambda e, z=z, O=O: e.scalar_tensor_tensor(O[z][0][:], KK[0][:], -1.0, Ee[z][0][:], ALU.mult, ALU.mult),
                 r=[KK[1], Ee[z][1]], w=[O[z][1]])
            S.op("gpsimd", lambda e, z=z, O=O, co=co: e.tensor_tensor(O[2 + z][0][:], R32[:, co, :], Ec[z][0][:], ALU.mult),
                 r=[b_R[co], Ec[z][1]], w=[O[2 + z][1]])
            S.op("gpsimd", lambda e, z=z: e.tensor_tensor(BT[0][:], KK[0][:], AZ[z][0][:], ALU.mult), r=[KK[1], AZ[z][1]], w=[BT[1]])
            S.op("gpsimd", lambda e, z=z, O=O: e.tensor_tensor(O[4 + z][0][:], BT[0][:], En[z][0][:], ALU.mult),
                 r=[BT[1], En[z][1]], w=[O[4 + z][1]])
            S.op("gpsimd", lambda e, z=z, O=O: e.tensor_tensor(O[6 + z][0][:], KM[z][0][:], En[z][0][:], ALU.mult),
                 r=[KM[z][1], En[z][1]], w=[O[6 + z][1]])
        S.op("scalar", lambda e, O=O, co=co: e.copy(O[8][0][:], V32[:, co, :]), r=[b_V[co]], w=[O[8][1]])
        for k in range(9):
            out_ops.append(S.dma("sync", ob[k, co, :, tok0:tok0 + PBLK], O[k][0][:], r=[O[k][1]]))
        if stop == 'E4':
            return
        yield
        S.op("gpsimd", lambda e: e.tensor_tensor(KS[0][:], KM[0][0][:], KM[1][0][:], ALU.add), r=[KM[0][1], KM[1][1]], w=[KS[1]])
        S.op("vector", lambda e, co=co: e.scalar_tensor_tensor(PB[0][:], R32[:, co, :], vc[:, 6, co:co + 1], KS[0][:], ALU.mult, ALU.mult),
             r=[b_R[co], b_vc, KS[1]], w=[PB[1]])
        regbv = PS[7][:, 0:PBLK]
        S.op("tensor", lambda e: e.matmul(regbv, blk[:], PB[0][:], start=True, stop=True), r=[b_blk, PB[1]], w=[b_ps[7][0]])
        S.op("vector", lambda e, op_=op_, co=co: e.tensor_tensor(BV[op_][0][:], regbv, V32[:, co, :], ALU.mult),
             r=[b_ps[7][0], b_V[co]], w=[BV[op_][1]])
        out_ops.append(S.dma("sync", of[0, co, :, tok0:tok0 + PBLK], BV[op_][0][:], r=[BV[op_][1]]))

    nblk_lat = nlat // PBLK
    blocks = [("lat", bi) for bi in range(nblk_lat)] + ([("ctx", 0)] if with_ctx else [])
    tcount = 0
    ocount = 0
    C0 = 64
    for kind, bi in blocks:
        isctx = kind == "ctx"
        v = 1 if isctx else 0
        if isctx:
            row0 = nlat + 128
            tl = [(row0, C0), (row0 + 128, C0 + 128)]
            tok0 = nlat
        else:
            row0 = bi * PBLK
            tl = [(row0, 0), (row0 + 128, 128), (row0 + 256, 256)]
            tok0 = bi * PBLK
        for (rw, col) in tl:
            par = tcount % 2
            tcount += 1
            S.dma("sync", xt[par][:], xin[rw:rw + 128, :], w=[b_xt[par]])
            ss = sm[par][:, 0:1]; rstd = sm[par][:, 1:2]
            S.op("scalar", lambda e, par=par, ss=ss: e.activation(xs[:], xt[par][:], AF.Square, accum_out=ss),
                 r=[b_xt[par]], w=[b_xs, b_sm[par]])
            S.op("vector", lambda e, ss=ss, rstd=rstd: e.tensor_scalar(rstd, ss, 1.0 / D, 1e-6, ALU.mult, ALU.add),
                 r=[b_sm[par]], w=[b_sm[par]])
            S.op("scalar", lambda e, rstd=rstd: e.activation(rstd, rstd, AF.Sqrt), r=[b_sm[par]], w=[b_sm[par]])
            S.op("vector", lambda e, rstd=rstd: e.reciprocal(rstd, rstd), r=[b_sm[par]], w=[b_sm[par]])
            S.op("vector", lambda e, par=par, rstd=rstd: e.tensor_scalar(xs[:], xt[par][:], rstd, None, ALU.mult),
                 r=[b_xt[par], b_sm[par]], w=[b_xs])
            for c in range(8):
                pb = c // 4
                S.op("tensor", lambda e, c=c, pb=pb: e.transpose(PS[pb][:, (c % 4) * 128:(c % 4) * 128 + 128],
                                                                 xs[:, c * 128:(c + 1) * 128], ident[:]),
                     r=[b_xs, b_id], w=b_ps[pb])
            for c in range(8):
                pb = c // 4
                S.op("scalar", lambda e, c=c, pb=pb, v=v, col=col: e.activation(
                    HL[:, c, col:col + 128], PS[pb][:, (c % 4) * 128:(c % 4) * 128 + 128], AF.Identity,
                    bias=mc[:, 2 + 2 * v, c:c + 1], scale=gsc[:, v, c:c + 1]),
                    r=b_ps[pb] + [b_mc, b_gsc], w=[b_HL[c]])
        if stop == 'A':
            continue
        for c in range(8):
            ctr = HL[:, c, C0:C0 + PBLK]
            if not isctx:
                c4 = ctr.rearrange("p (a b) -> p a b", b=64)
                x4 = XX[:, c, :].rearrange("p (a b) -> p a b", b=64)
                if c < 2:
                    S.op("vector", lambda e, x4=x4, c4=c4: e.tensor_tensor(x4[:, :, 1:64], c4[:, :, 0:63], c4[:, :, 1:64], ALU.subtract),
                         r=[b_HL[c]], w=[b_XX[c]])
                    S.op("vector", lambda e, x4=x4, c4=c4: e.tensor_scalar(x4[:, :, 0:1], c4[:, :, 0:1], -1.0, None, ALU.mult),
                         r=[b_HL[c]], w=[b_XX[c]])
                elif c < 4:
                    S.op("vector", lambda e, x4=x4, c4=c4: e.tensor_tensor(x4[:, :, 0:63], c4[:, :, 1:64], c4[:, :, 0:63], ALU.subtract),
                         r=[b_HL[c]], w=[b_XX[c]])
                    S.op("vector", lambda e, x4=x4, c4=c4: e.tensor_scalar(x4[:, :, 63:64], c4[:, :, 63:64], -1.0, None, ALU.mult),
                         r=[b_HL[c]], w=[b_XX[c]])
                elif c < 6:
                    S.op("vector", lambda e, c=c, ctr=ctr: e.tensor_tensor(XX[:, c, :], HL[:, c, 0:PBLK], ctr, ALU.subtract),
                         r=[b_HL[c]], w=[b_XX[c]])
                    if bi == 0:
                        S.op("vector", lambda e, c=c: e.scalar_tensor_tensor(XX[:, c, 0:64], HL[:, c, 0:64], edge[:, 0:1], HL[:, c, 64:128],
                                                                             ALU.mult, ALU.subtract),
                             r=[b_HL[c], b_edge], w=[b_XX[c]])
                else:
                    S.op("vector", lambda e, c=c, ctr=ctr: e.tensor_tensor(XX[:, c, :], HL[:, c, 128:128 + PBLK], ctr, ALU.subtract),
                         r=[b_HL[c]], w=[b_XX[c]])
                    if bi == nblk_lat - 1:
                        S.op("vector", lambda e, c=c: e.scalar_tensor_tensor(XX[:, c, PBLK - 64:PBLK], HL[:, c, PBLK + 64:PBLK + 128], edge[:, 1:2],
                                                                             HL[:, c, PBLK:PBLK + 64], ALU.mult, ALU.subtract),
                             r=[b_HL[c], b_edge], w=[b_XX[c]])
            else:
                if c < 4:
                    S.op("vector", lambda e, c=c: e.tensor_tensor(XX[:, c, 1:PBLK], HL[:, c, C0:C0 + PBLK - 1], HL[:, c, C0 + 1:C0 + PBLK], ALU.subtract),
                         r=[b_HL[c]], w=[b_XX[c]])
                    S.op("vector", lambda e, c=c: e.tensor_scalar(XX[:, c, 0:1], HL[:, c, C0:C0 + 1], -1.0, None, ALU.mult),
                         r=[b_HL[c]], w=[b_XX[c]])
                else:
                    S.op("vector", lambda e, c=c: e.tensor_tensor(XX[:, c, 0:PBLK - 1], HL[:, c, C0 + 1:C0 + PBLK], HL[:, c, C0:C0 + PBLK - 1], ALU.subtract),
                         r=[b_HL[c]], w=[b_XX[c]])
                    S.op("vector", lambda e, c=c: e.tensor_scalar(XX[:, c, PBLK - 1:PBLK], HL[:, c, C0 + PBLK - 1:C0 + PBLK], -1.0, None, ALU.mult),
                         r=[b_HL[c]], w=[b_XX[c]])
        if stop == 'B':
            continue
        for j in range(6):
            eng = "vector"
            for c in range(8):
                S.op(eng, lambda e, j=j, c=c: e.scalar_tensor_tensor(XM[j][:, c, :], XX[:, c, :], mu[:, j, c:c + 1], HL[:, c, C0:C0 + PBLK],
                                                                     ALU.mult, ALU.add),
                     r=[b_XX[c], b_HL[c], b_mu], w=[b_XM[j][c]])
        if stop == 'C':
            continue
        pk = [0]

        def proj(wsb, bw, j, dst, bdst, extra=None):
            for co in range(8):
                pb = 2 + pk[0] % 2; hb = (pk[0] // 2) % 2
                pk[0] += 1
                reg = PS[pb][:, hb * 256:hb * 256 + PBLK]
                for c in range(8):
                    S.op("tensor", lambda e, reg=reg, c=c, co=co, wsb=wsb, j=j: e.matmul(
                        reg, wsb[:, c, co * 128:(co + 1) * 128], XM[j][:, c, :], start=(c == 0), stop=(c == 7)),
                        r=[bw] + b_XM[j], w=[b_ps[pb][hb]])
                S.op("scalar", lambda e, reg=reg, co=co, dst=dst: e.copy(dst[:, co, :], reg), r=[b_ps[pb][hb]], w=[bdst[co]])
        proj(wr, b_wr, 0, R32, b_R)
        proj(wk, b_wk, 2, K32, b_K)
        proj(wv, b_wv, 3, V32, b_V)
        for z in range(2):
            reg = PS[4][0:64, z * 256:z * 256 + PBLK]
            for c in range(8):
                S.op("tensor", lambda e, reg=reg, c=c, z=z: e.matmul(reg, w1[:, z, c, :], XM[1][:, c, :], start=(c == 0), stop=(c == 7)),
                     r=[b_w1] + b_XM[1], w=[b_ps[4][z]])
            S.op("scalar", lambda e, reg=reg, z=z: e.activation(TW[z][:], reg, AF.Tanh), r=[b_ps[4][z]], w=[b_TW[z]])
        for z in range(2):
            reg = PS[5][0:64, z * 256:z * 256 + PBLK]
            for c in range(8):
                S.op("tensor", lambda e, reg=reg, c=c, z=z: e.matmul(reg, a1[:, z, c, :], XM[4][:, c, :], start=(c == 0), stop=(c == 7)),
                     r=[b_a1] + b_XM[4], w=[b_ps[5][z]])
            S.op("scalar", lambda e, reg=reg, z=z: e.copy(AL[z][:], reg), r=[b_ps[5][z]], w=[b_AL[z]])
        rega = PS[6][:, 0:PBLK]; regb = PS[6][0:32, 256:256 + PBLK]
        for c in range(8):
            S.op("tensor", lambda e, c=c: e.matmul(rega, g1[:, c, 0:128], XM[5][:, c, :], start=(c == 0), stop=(c == 7)),
                 r=[b_g1] + b_XM[5], w=[b_ps[6][0]])
        for c in range(8):
            S.op("tensor", lambda e, c=c: e.matmul(regb, g1[:, c, 128:160], XM[5][:, c, :], start=(c == 0), stop=(c == 7)),
                 r=[b_g1] + b_XM[5], w=[b_ps[6][1]])
        S.op("scalar", lambda e: e.activation(SGa[:], rega, AF.Sigmoid), r=[b_ps[6][0]], w=[b_SG])
        S.op("scalar", lambda e: e.activation(SGb[:], regb, AF.Sigmoid), r=[b_ps[6][1]], w=[b_SG])
        if stop == 'D':
            continue
        for cp in range(4):
            gens = []
            for co in (2 * cp, 2 * cp + 1):
                op_ = ocount % 2
                ocount += 1
                gens.append(E_body(co, op_, tok0, isctx, LW2[op_], AZ2[op_], Ec2[op_], Ee2[op_], En2[op_], KM2[op_], KK2[op_]))
            alive = list(gens)
            while alive:
                for g_ in list(alive):
                    try:
                        next(g_)
                    except StopIteration:
                        alive.remove(g_)
    S.emit(out_ops)
    return nc


_PREP_NC = {}


def prep_consts():
    blk = np.zeros((128, 128), np.float32); blk[:64, :64] = 1; blk[64:, 64:] = 1
    cm = np.ones((128, PBLK), np.float32); cm[:, ::64] = 0
    return dict(ident=np.eye(128, dtype=np.float32), blk=blk, cmask=cm)


def colsk(vs):
    return np.ascontiguousarray(np.stack([cols(v) for v in vs], axis=1))


def run_prep(x, xc, i, mods, inp, nlat=NLAT, trace=False):
    j = i // 2
    key = nlat
    if key not in _PREP_NC:
        _PREP_NC[key] = build_prep(nlat=nlat)
    P = {k: inp["rw_" + k][j] for k in ["mu", "wr", "wk", "wv", "w0", "w1", "w2", "a0", "a1", "a2", "g1", "g2", "k_k", "k_a", "r_k"]}
    base = dict(prep_consts())
    base.update(mu=colsk([P["mu"][t] for t in range(6)]),
                vcols=colsk([P["w0"][0], P["w0"][1], P["a0"][0], P["a0"][1], P["k_k"], P["k_a"], P["r_k"].reshape(-1), np.zeros(D)]),
                wr=np.ascontiguousarray(P["wr"]), wk=np.ascontiguousarray(P["wk"]), wv=np.ascontiguousarray(P["wv"]),
                w1=np.ascontiguousarray(P["w1"]), w2=np.ascontiguousarray(P["w2"]), a1=np.ascontiguousarray(P["a1"]),
                a2=np.ascontiguousarray(P["a2"]), g1=np.ascontiguousarray(P["g1"]), g2=np.ascontiguousarray(P["g2"]))
    nq = x.shape[1] // nlat
    maps = []
    for k in range(NCORES):
        b, q = k // nq, k % nq
        lo = q * nlat
        z64 = np.zeros((64, D), np.float32)
        hb = x[b, lo - 64:lo] if q > 0 else z64
        ha = x[b, lo + nlat:lo + nlat + 64] if q < nq - 1 else z64
        xin = np.concatenate([hb, x[b, lo:lo + nlat], ha, xc[b]], axis=0)
        ml = mods[i, b]; mcx = mods[i, 2]
        mc = np.stack([cols(inp["norm1_g"][i]), cols(ml[1 * D:2 * D]), cols(ml[0:D]), cols(mcx[1 * D:2 * D]), cols(mcx[0:D])], axis=1)
        edge = np.zeros((128, 2), np.float32); edge[:, 0] = 1.0 if q > 0 else 0.0; edge[:, 1] = 1.0 if q < nq - 1 else 0.0
        m = dict(base)
        m.update(xin=np.ascontiguousarray(xin, dtype=np.float32), mcols=np.ascontiguousarray(mc), edge=edge)
        maps.append(m)
    res = run_bass_kernel_spmd(_PREP_NC[key], maps, core_ids=list(range(NCORES)), trace=trace)
    if trace:
        print("prep exec_time_ns", res.exec_time_ns)
    return res.results


SGS = 4


def build_scan(nch=260, mode=None):
    nc = new_nc()
    AR_d = dram_in(nc, "AR", [64, nch, 8, 128], BF16)
    BK_d = dram_in(nc, "BK", [64, nch, 8, 128], BF16)
    BKT_d = dram_in(nc, "BKT", [128, nch, 8, 64], BF16)
    VT_d = dram_in(nc, "VT", [64, nch, 8, 64], BF16)
    PC_d = dram_in(nc, "PCB", [64, nch, 8])
    mg_d = dram_in(nc, "maskG", [128, 8, 128])
    mn_d = dram_in(nc, "maskNT", [64, 8, 64])
    id_d = dram_in(nc, "ident8", [64, 8, 64])
    Y_d = dram_out(nc, "Y", [64, nch, 8, 64])
    S = Sched(nc)
    maskG = S.sb("maskG", [128, 8, 128], F32); maskNT = S.sb("maskNT", [64, 8, 64], F32); id8 = S.sb("id8", [64, 8, 64], F32)
    b_c = Buf()
    ARs = [S.sb("AR%d" % i, [64, SGS, 8, 128], BF16) for i in range(2)]; b_AR = [Buf() for _ in range(2)]
    BKs = [S.sb("BK%d" % i, [64, SGS, 8, 128], BF16) for i in range(2)]; b_BK = [Buf() for _ in range(2)]
    BTs = [S.sb("BT%d" % i, [64, SGS, 8, 64], BF16) for i in range(2)]; b_BKT = [Buf() for _ in range(2)]
    KTs = [S.sb("KT%d" % i, [64, SGS, 8, 64], BF16) for i in range(2)]
    VTs = [S.sb("VT%d" % i, [64, SGS, 8, 64], BF16) for i in range(2)]
    UTs = [S.sb("UT%d" % i, [64, SGS, 8, 64], BF16) for i in range(2)]
    b_UVv = [Buf() for _ in range(2)]
    b_UVu = [[Buf() for _ in range(SGS)] for _ in range(2)]
    PCs = [S.sb("PC%d" % i, [64, SGS, 8], F32) for i in range(2)]; b_PC = [Buf() for _ in range(2)]
    Yst = [S.sb("Yst%d" % i, [64, SGS, 8, 64], F32) for i in range(2)]; b_Y = [Buf() for _ in range(2)]
    Gm = [S.sb("Gm%d" % i, [64, 8, 128], BF16) for i in range(2)]; b_Gm = [Buf() for _ in range(2)]
    Gk = [S.sb("Gk%d" % i, [64, 8, 128], BF16) for i in range(2)]
    NTm = [S.sb("NTm%d" % i, [64, 8, 64], BF16) for i in range(2)]; b_NTm = [Buf() for _ in range(2)]
    Pb = [S.sb("Pb%d" % i, [64, 8, 64], BF16) for i in range(2)]; b_Pb = [Buf() for _ in range(2)]
    PTb = [S.sb("PTb%d" % i, [64, 8, 64], BF16) for i in range(2)]; b_PTb = [Buf() for _ in range(2)]
    T32 = [S.sb("T32_%d" % i, [64, 8, 64], F32) for i in range(2)]; b_T32 = [Buf() for _ in range(2)]
    Tb = [S.sb("Tb%d" % i, [64, 8, 64], BF16) for i in range(2)]; b_Tb = [Buf() for _ in range(2)]
    WTb = S.sb("WTb", [64, 8, 64], BF16); b_WT = Buf()
    S32 = S.sb("S32", [64, 8, 64], F32); b_S32 = Buf()
    Sb = S.sb("Sb", [64, 8, 64], BF16); b_Sb = Buf()
    PS = [S.ps("psb%d" % i, [128, 512], F32) for i in range(8)]; b_ps = [Buf() for _ in range(8)]

    S.dma("sync", maskG[:], mg_d, w=[b_c]); S.dma("sync", maskNT[:], mn_d, w=[b_c]); S.dma("sync", id8[:], id_d, w=[b_c])
    S.op("vector", lambda e: e.memset(S32[:], 0.0), w=[b_S32])
    S.op("vector", lambda e: e.memset(Sb[:], 0.0), w=[b_Sb])
    out_ops = []
    ngrp = (nch + SGS - 1) // SGS

    def load_group(gi):
        gp = gi % 2
        s0 = gi * SGS; n = min(SGS, nch - s0)
        S.dma("sync", ARs[gp][:, 0:n], AR_d[:, s0:s0 + n], w=[b_AR[gp]])
        S.dma("sync", BKs[gp][:, 0:n], BK_d[:, s0:s0 + n], w=[b_BK[gp]])
        S.dma("sync", BTs[gp][:, 0:n], BKT_d[0:64, s0:s0 + n], w=[b_BKT[gp]])
        S.dma("sync", KTs[gp][:, 0:n], BKT_d[64:128, s0:s0 + n], w=[b_BKT[gp]])
        S.dma("sync", VTs[gp][:, 0:n], VT_d[:, s0:s0 + n], w=[b_UVv[gp]])
        S.dma("sync", PCs[gp][:, 0:n], PC_d[:, s0:s0 + n], w=[b_PC[gp]])

    def prep_stages(j):
        gp = (j // SGS) % 2; g = j % SGS; sp = j % 2
        AR = ARs[gp]; BK = BKs[gp]
        st = []

        def s_G():
            for i in range(8):
                S.op("tensor", lambda e, i=i: e.matmul(PS[i // 4][0:64, (i % 4) * 128:(i % 4) * 128 + 128], BK[:, g, i, 0:64], AR[:, g, i, :],
                                                       start=True, stop=True), r=[b_BK[gp], b_AR[gp]], w=[b_ps[i // 4]])
            for i in range(8):
                S.op("tensor", lambda e, i=i: e.matmul(PS[3 + i // 4][0:64, (i % 4) * 128:(i % 4) * 128 + 128], BK[:, g, i, 64:128], AR[:, g, i, :],
                                                       start=True, stop=True), r=[b_BK[gp], b_AR[gp]], w=[b_ps[3 + i // 4]])
            for i in range(8):
                S.op("tensor", lambda e, i=i: e.matmul(PS[2][0:64, i * 64:(i + 1) * 64], AR[:, g, i, 0:64], BK[:, g, i, 0:64],
                                                       start=True, stop=True), r=[b_BK[gp], b_AR[gp]], w=[b_ps[2]])
            for h in range(2):
                S.op("vector", lambda e, h=h: e.tensor_tensor(Gm[sp][:, 4 * h:4 * h + 4, :], PS[h][0:64, :].rearrange("p (a b) -> p a b", a=4),
                                                              maskG[0:64, 4 * h:4 * h + 4, :], ALU.mult), r=[b_ps[h], b_c], w=[b_Gm[sp]])
                S.op("vector", lambda e, h=h: e.tensor_tensor(Gk[sp][:, 4 * h:4 * h + 4, :], PS[3 + h][0:64, :].rearrange("p (a b) -> p a b", a=4),
                                                              maskG[0:64, 4 * h:4 * h + 4, :], ALU.mult), r=[b_ps[3 + h], b_c], w=[b_Gm[sp]])
            S.op("vector", lambda e: e.tensor_tensor(NTm[sp][:], PS[2][0:64, :].rearrange("p (a b) -> p a b", a=8), maskNT[:], ALU.mult),
                 r=[b_ps[2], b_c], w=[b_NTm[sp]])
            S.op("vector", lambda e: e.tensor_tensor(Tb[sp][:], Gm[sp][0:64, :, 0:64], id8[:], ALU.add), r=[b_Gm[sp], b_c], w=[b_Tb[sp]])
        st.append(s_G)
        for l in range(1, 6):
            def s_sq(l=l):
                if l == 1:
                    Pp = lambda i: Gm[sp][0:64, i, 0:64]; PTp = lambda i: NTm[sp][:, i, :]
                    rP = [b_Gm[sp]]; rPT = [b_NTm[sp]]
                else:
                    q = (l - 1) % 2
                    Pp = lambda i, q=q: Pb[q][:, i, :]; PTp = lambda i, q=q: PTb[q][:, i, :]
                    rP = [b_Pb[q]]; rPT = [b_PTb[q]]
                qn = l % 2
                if l < 5:
                    for i in range(8):
                        S.op("tensor", lambda e, i=i: e.matmul(PS[3][0:64, i * 64:(i + 1) * 64], PTp(i), Pp(i), start=True, stop=True),
                             r=rP + rPT, w=[b_ps[3]])
                for i in range(8):
                    S.op("tensor", lambda e, i=i: e.matmul(PS[4][0:64, i * 64:(i + 1) * 64], Pp(i), PTp(i), start=True, stop=True),
                         r=rP + rPT, w=[b_ps[4]])
                if l < 5:
                    S.op("scalar", lambda e: e.copy(Pb[qn][:], PS[3][0:64, :].rearrange("p (a b) -> p a b", a=8)), r=[b_ps[3]], w=[b_Pb[qn]])
                S.op("scalar", lambda e: e.copy(PTb[qn][:], PS[4][0:64, :].rearrange("p (a b) -> p a b", a=8)), r=[b_ps[4]], w=[b_PTb[qn]])
            st.append(s_sq)

            def s_T(l=l):
                qn = l % 2
                for i in range(8):
                    S.op("tensor", lambda e, i=i: e.matmul(PS[2][0:64, i * 64:(i + 1) * 64], PTb[qn][:, i, :], Tb[sp][:, i, :], start=True, stop=True),
                         r=[b_PTb[qn], b_Tb[sp]], w=[b_ps[2]])
                S.op("vector", lambda e: e.tensor_tensor(Tb[sp][:], Tb[sp][:], PS[2][0:64, :].rearrange("p (a b) -> p a b", a=8), ALU.add),
                     r=[b_ps[2]], w=[b_Tb[sp]])
            st.append(s_T)
        sq = [st[1 + 2 * l] for l in range(5)]; tu = [st[2 + 2 * l] for l in range(5)]
        st = [st[0], sq[0], sq[1], tu[0], sq[2], tu[1], sq[3], tu[2], sq[4], tu[3], tu[4]]
        return st

    def seq_stages(j):
        gp = (j // SGS) % 2; g = j % SGS; sp = j % 2
        AR = ARs[gp]; UT = UTs[gp]; VT = VTs[gp]; BT = BTs[gp]; KT = KTs[gp]
        st = []

        def s_W():
            for i in range(8):
                reg = PS[5][0:64, i * 64:(i + 1) * 64]
                S.op("tensor", lambda e, i=i, reg=reg: e.matmul(reg, AR[:, g, i, 0:64], Sb[:, i, :], start=True, stop=False),
                     r=[b_AR[gp], b_Sb], w=[b_ps[5]])
                S.op("tensor", lambda e, i=i, reg=reg: e.matmul(reg, Gk[sp][:, i, 0:64], VT[:, g, i, :], start=False, stop=True),
                     r=[b_Gm[sp], b_UVv[gp]], w=[b_ps[5]])
            S.op("scalar", lambda e: e.copy(WTb[:], PS[5][0:64, :].rearrange("p (a b) -> p a b", a=8)), r=[b_ps[5]], w=[b_WT])
        st.append(s_W)

        def s_U():
            for i in range(8):
                S.op("tensor", lambda e, i=i: e.matmul(PS[6][0:64, i * 64:(i + 1) * 64], Tb[sp][:, i, :], WTb[:, i, :], start=True, stop=True),
                     r=[b_Tb[sp], b_WT], w=[b_ps[6]])
            S.op("scalar", lambda e: e.copy(UT[:, g, :, :], PS[6][0:64, :].rearrange("p (a b) -> p a b", a=8)),
                 r=[b_ps[6]], w=[b_UVu[gp][g]])
        st.append(s_U)

        def s_YS():
            for i in range(8):
                reg = PS[7][0:64, i * 64:(i + 1) * 64]
                S.op("tensor", lambda e, i=i, reg=reg: e.matmul(reg, AR[:, g, i, 64:128], Sb[:, i, :], start=True, stop=False),
                     r=[b_AR[gp], b_Sb], w=[b_ps[7]])
                S.op("tensor", lambda e, i=i, reg=reg: e.matmul(reg, Gm[sp][:, i, 64:128], UT[:, g, i, :], start=False, stop=False),
                     r=[b_Gm[sp], b_UVu[gp][g]], w=[b_ps[7]])
                S.op("tensor", lambda e, i=i, reg=reg: e.matmul(reg, Gk[sp][:, i, 64:128], VT[:, g, i, :], start=False, stop=True),
                     r=[b_Gm[sp], b_UVv[gp]], w=[b_ps[7]])
            for i in range(8):
                S.op("tensor", lambda e, i=i: e.matmul(PS[5][0:64, i * 64:(i + 1) * 64], BT[:, g, i, :], UT[:, g, i, :], start=True, stop=False),
                     r=[b_BKT[gp], b_UVu[gp][g]], w=[b_ps[5]])
                S.op("tensor", lambda e, i=i: e.matmul(PS[5][0:64, i * 64:(i + 1) * 64], KT[:, g, i, :], VT[:, g, i, :], start=False, stop=True),
                     r=[b_BKT[gp], b_UVv[gp]], w=[b_ps[5]])
            S.op("scalar", lambda e: e.copy(Yst[gp][:, g, :, :], PS[7][0:64, :].rearrange("p (a b) -> p a b", a=8)), r=[b_ps[7]], w=[b_Y[gp]])
            S.op("vector", lambda e: e.tensor_tensor(S32[:], S32[:], PS[5][0:64, :].rearrange("p (a b) -> p a b", a=8), ALU.add),
                 r=[b_ps[5]], w=[b_S32])
            S.op("vector", lambda e: e.tensor_tensor(S32[:], S32[:], PCs[gp][:, g, :].unsqueeze(2).to_broadcast([64, 8, 64]), ALU.mult),
                 r=[b_PC[gp]], w=[b_S32])
            S.op("scalar", lambda e: e.copy(Sb[:], S32[:]), r=[b_S32], w=[b_Sb])
        st.append(s_YS)
        return st

    load_group(0)
    if ngrp > 1:
        load_group(1)
    for f in prep_stages(0):
        f()
    for j in range(nch):
        seq = seq_stages(j)
        pre = prep_stages(j + 1) if j + 1 < nch else []
        pos = {1: 0, 4: 1, 7: 2}
        k = 0
        nseq = {None: 3, "prep": 0, "W": 1, "U": 2}[mode]
        for pi in range(max(len(pre), 8)):
            if pi < len(pre):
                pre[pi]()
            if pi in pos:
                if pos[pi] < nseq:
                    seq[pos[pi]]()
                k += 1
        for si in range(k, 3):
            if si < nseq:
                seq[si]()
        if (j + 1) % SGS == 0 or j == nch - 1:
            gi = j // SGS
            s0 = gi * SGS; n = min(SGS, nch - s0)
            out_ops.append(S.dma("sync", Y_d[:, s0:s0 + n], Yst[gi % 2][:, 0:n], r=[b_Y[gi % 2]]))
            if gi + 2 < ngrp:
                load_group(gi + 2)
    S.emit(out_ops)
    return nc


def scan_consts():
    s = np.arange(64)[:, None]; t = np.arange(64)[None, :]
    mg = np.zeros((128, 8, 128), np.float32); mn = np.zeros((64, 8, 64), np.float32); idm = np.zeros((64, 8, 64), np.float32)
    for i in range(8):
        z = i // 4
        strict = (s < t) if z == 0 else (s > t)
        incl = (s <= t) if z == 0 else (s >= t)
        blk = np.concatenate([strict, incl], axis=1).astype(np.float32)
        mg[:, i, :] = np.concatenate([blk, blk], axis=0)
        mn[:, i, :] = strict.T.astype(np.float32)
        idm[:, i, :] = np.eye(64, dtype=np.float32)
    return dict(maskG=mg, maskNT=mn, ident8=idm)


GN_EPS = 64e-5


def build_rout(ntok=NTOK):
    nc = new_nc()
    x_d = dram_in(nc, "xin", [ntok, D]); y0_d = dram_in(nc, "y0", [ntok, D]); y1_d = dram_in(nc, "y1", [ntok, D])
    bv_d = dram_in(nc, "bv", [ntok, D]); ga_d = dram_in(nc, "gate", [ntok, D])
    lg_d = dram_in(nc, "lngb", [128, D]); lb_d = dram_in(nc, "lnbb", [128, D]); gt_d = dram_in(nc, "gtb", [128, 2, D])
    wo_d = dram_in(nc, "wo", [D, D]); id_d = dram_in(nc, "ident", [128, 128])
    xout = dram_out(nc, "xout", [ntok, D])
    S = Sched(nc)
    ident = S.sb("ident", [128, 128], F32); b_id = Buf()
    lgb = S.sb("lgb", [128, D], F32); lbb = S.sb("lbb", [128, D], F32); gtb = S.sb("gtb", [128, 2, D], F32); b_c = Buf()
    wo = S.sb("wo", [128, 8, D], BF16); b_wo = Buf()
    names = ["x", "y0", "y1", "bv", "ga"]
    srcs = [x_d, y0_d, y1_d, bv_d, ga_d]
    tl = [[S.sb("%s%d" % (n, i), [128, D], F32) for n in names] for i in range(2)]
    b_tl = [[Buf() for _ in names] for _ in range(2)]
    yc = S.sb("yc", [128, D], F32); b_yc = Buf()
    sq = S.sb("sq", [128, D], F32); b_sq = Buf()
    zT = [S.sb("zT%d" % i, [128, 8, 128], BF16) for i in range(2)]; b_zT = [Buf() for _ in range(2)]
    ot = [S.sb("ot%d" % i, [128, D], F32) for i in range(2)]; b_ot = [Buf() for _ in range(2)]
    sm = [S.sb("sm%d" % i, [128, 4, 16], F32) for i in range(2)]; b_sm = [Buf() for _ in range(2)]
    PS = [S.ps("psb%d" % i, [128, 512], F32) for i in range(8)]; b_ps = [Buf() for _ in range(8)]
    S.dma("sync", ident[:], id_d, w=[b_id]); S.dma("sync", lgb[:], lg_d, w=[b_c]); S.dma("sync", lbb[:], lb_d, w=[b_c])
    S.dma("sync", gtb[:], gt_d, w=[b_c])
    S.dma("gpsimd", wo[:], wo_d.rearrange("(c p) n -> p c n", p=128), w=[b_wo])
    out_ops = []
    for ti, (off, P) in enumerate(token_tiles(ntok)):
        par = ti % 2
        v = 1 if off >= NLAT and ntok > NLAT else 0
        T = tl[par]; B = b_tl[par]
        for n in range(5):
            S.dma("sync" if n % 2 == 0 else "gpsimd", T[n][0:P, :], srcs[n][off:off + P, :], w=[B[n]])
        X, Y0, Y1, BVt, GA = [t[0:P, :] for t in T]
        m = sm[par]
        s1 = m[0:P, 0, :]; nm = m[0:P, 1, :]; s2 = m[0:P, 2, :]; rs = m[0:P, 3, :]
        v3 = lambda ap: ap.rearrange("p (h k) -> p h k", k=64)
        bc = lambda ap, P: ap.unsqueeze(2).to_broadcast([P, 16, 64])
        S.op("vector", lambda e, Y0=Y0, Y1=Y1: e.tensor_tensor(Y0, Y0, Y1, ALU.add), r=[B[2]], w=[B[1]])
        S.op("vector", lambda e, Y0=Y0, s1=s1: e.reduce_sum(s1, v3(Y0), AX.X), r=[B[1]], w=[b_sm[par]])
        S.op("vector", lambda e, s1=s1, nm=nm: e.tensor_scalar(nm, s1, -1.0 / 64, None, ALU.mult), r=[b_sm[par]], w=[b_sm[par]])
        S.op("vector", lambda e, Y0=Y0, nm=nm, P=P: e.tensor_tensor(v3(yc[0:P, :]), v3(Y0), bc(nm, P), ALU.add), r=[B[1], b_sm[par]], w=[b_yc])
        S.op("scalar", lambda e, P=P: e.activation(sq[0:P, :], yc[0:P, :], AF.Square), r=[b_yc], w=[b_sq])
        S.op("vector", lambda e, s2=s2, P=P: e.reduce_sum(s2, v3(sq[0:P, :]), AX.X), r=[b_sq], w=[b_sm[par]])
        S.op("vector", lambda e, s2=s2, rs=rs: e.tensor_scalar(rs, s2, 1.0 / 64, GN_EPS, ALU.mult, ALU.add), r=[b_sm[par]], w=[b_sm[par]])
        S.op("scalar", lambda e, rs=rs: e.activation(rs, rs, AF.Sqrt), r=[b_sm[par]], w=[b_sm[par]])
        S.op("vector", lambda e, rs=rs: e.reciprocal(rs, rs), r=[b_sm[par]], w=[b_sm[par]])
        S.op("vector", lambda e, rs=rs, P=P: e.tensor_tensor(v3(yc[0:P, :]), v3(yc[0:P, :]), bc(rs, P), ALU.mult), r=[b_sm[par]], w=[b_yc])
        S.op("vector", lambda e, P=P: e.tensor_tensor(yc[0:P, :], yc[0:P, :], lgb[0:P, :], ALU.mult), r=[b_c], w=[b_yc])
        S.op("gpsimd", lambda e, P=P: e.tensor_tensor(yc[0:P, :], yc[0:P, :], lbb[0:P, :], ALU.add), r=[b_c], w=[b_yc])
        S.op("gpsimd", lambda e, P=P, BVt=BVt: e.tensor_tensor(yc[0:P, :], yc[0:P, :], BVt, ALU.add), r=[B[3]], w=[b_yc])
        S.op("vector", lambda e, P=P, GA=GA: e.tensor_tensor(yc[0:P, :], yc[0:P, :], GA, ALU.mult), r=[B[4]], w=[b_yc])
        for c in range(8):
            pb = c // 4
            S.op("tensor", lambda e, P=P, c=c, pb=pb: e.transpose(PS[pb][:, (c % 4) * 128:(c % 4) * 128 + P], yc[0:P, c * 128:(c + 1) * 128], ident[0:P, 0:P]),
                 r=[b_yc, b_id], w=[b_ps[pb]])
        for pb in range(2):
            S.op("scalar", lambda e, P=P, pb=pb, par=par: e.copy(zT[par][:, 4 * pb:4 * pb + 4, 0:P],
                                                                  PS[pb][:, :].rearrange("p (a b) -> p a b", a=4)[:, :, 0:P]),
                 r=[b_ps[pb]], w=[b_zT[par]])
        for n in range(2):
            pb = 2 + n
            for c in range(8):
                S.op("tensor", lambda e, P=P, c=c, n=n, pb=pb, par=par: e.matmul(PS[pb][0:P, :], zT[par][:, c, 0:P], wo[:, c, n * 512:(n + 1) * 512],
                                                                                 start=(c == 0), stop=(c == 7)),
                     r=[b_zT[par], b_wo], w=[b_ps[pb]])
            sl = slice(n * 512, (n + 1) * 512)
            S.op("vector", lambda e, P=P, pb=pb, par=par, sl=sl, v=v: e.tensor_tensor(ot[par][0:P, sl], PS[pb][0:P, :], gtb[0:P, v, sl], ALU.mult),
                 r=[b_ps[pb], b_c], w=[b_ot[par]])
        S.op("gpsimd", lambda e, P=P, par=par, X=X: e.tensor_tensor(ot[par][0:P, :], ot[par][0:P, :], X, ALU.add), r=[B[0]], w=[b_ot[par]])
        out_ops.append(S.dma("sync", xout[off:off + P, :], ot[par][0:P, :], r=[b_ot[par]]))
    S.emit(out_ops)
    return nc


SEQ = 16384
CTX = 256
NCH_ALL = (SEQ + CTX) // 64
_SCAN_NC = {}
_ROUT_NC = {}
DEBUG_HOOK = None


def _dbg(name, arr):
    if DEBUG_HOOK is not None:
        DEBUG_HOOK(name, arr)


def run_rwkv_mixer(x, xc, i, mods, inp):
    j = i // 2
    pres = run_prep(x, xc, i, mods, inp)
    order = [np.arange(NCH_ALL), np.concatenate([np.arange(3, -1, -1), np.arange(NCH_ALL - 1, 3, -1)])]
    consts = scan_consts()
    maps = []
    for k in range(NCORES):
        b, hq = k // 4, k % 4
        if hq == 0:
            obs = [np.asarray(pres[b * 4 + q]["ob"]) for q in range(4)]
            pcs = [np.asarray(pres[b * 4 + q]["pc"]) for q in range(4)]
            F = np.concatenate([obs[0][:, :, :, NLAT:]] + [o[:, :, :, :NLAT] for o in obs], axis=3)
            F = F.reshape(9, 8, 128, NCH_ALL, 64)
            PCa = np.concatenate([pcs[0][:, :, :, NLAT // 64:]] + [p[:, :, :, :NLAT // 64] for p in pcs], axis=3)
        AR = np.empty((64, NCH_ALL, 8, 128), F.dtype); BK = np.empty((64, NCH_ALL, 8, 128), F.dtype)
        BKT = np.empty((128, NCH_ALL, 8, 64), F.dtype); VT = np.empty((64, NCH_ALL, 8, 64), F.dtype)
        PCB = np.empty((64, NCH_ALL, 8), np.float32)
        for ci in range(8):
            z, hl = ci // 4, ci % 4
            c = 2 * hq + hl // 2; p0 = (hl % 2) * 64
            od = order[z]
            A = F[0 + z, c, p0:p0 + 64][:, od]; R = F[2 + z, c, p0:p0 + 64][:, od]
            Bm = F[4 + z, c, p0:p0 + 64][:, od]; Km = F[6 + z, c, p0:p0 + 64][:, od]
            V = F[8, c, p0:p0 + 64][:, od]
            AR[:, :, ci, 0:64] = A; AR[:, :, ci, 64:128] = R
            BK[:, :, ci, 0:64] = Bm; BK[:, :, ci, 64:128] = Km
            BKT[0:64, :, ci, :] = Bm.transpose(2, 1, 0); BKT[64:128, :, ci, :] = Km.transpose(2, 1, 0)
            VT[:, :, ci, :] = V.transpose(2, 1, 0)
            PCB[:, :, ci] = PCa[z, c, p0:p0 + 64][:, od]
        m = dict(consts)
        m.update(AR=AR, BK=BK, BKT=BKT, VT=VT, PCB=PCB)
        maps.append(m)
    if NCH_ALL not in _SCAN_NC:
        _SCAN_NC[NCH_ALL] = build_scan(nch=NCH_ALL)
    sres = run_bass_kernel_spmd(_SCAN_NC[NCH_ALL], maps, core_ids=list(range(NCORES))).results
    yall = np.empty((2, 2, NCH_ALL, 64, 16, 64), np.float32)
    for k in range(NCORES):
        b, hq = k // 4, k % 4
        Y = np.asarray(sres[k]["Y"])
        for ci in range(8):
            z, hl = ci // 4, ci % 4
            yall[z, b, order[z], :, 4 * hq + hl, :] = Y[:, :, ci, :].transpose(1, 0, 2)
    yall = yall.reshape(2, 2, NCH_ALL * 64, D)
    _dbg("y_l%d" % i, yall)
    P = {kk: inp["rw_" + kk][j] for kk in ["wo", "ln_g", "ln_b"]}
    base = dict(lngb=bcast(P["ln_g"]), lnbb=bcast(P["ln_b"]), wo=np.ascontiguousarray(P["wo"]), ident=np.eye(128, dtype=np.float32))
    maps = []
    for k in range(NCORES):
        b, q = k // 4, k % 4
        lat = slice(CTX + q * NLAT, CTX + (q + 1) * NLAT); cx = slice(q * NCTX, (q + 1) * NCTX)
        of = np.asarray(pres[k]["of"])
        oft = of.transpose(0, 3, 1, 2).reshape(2, NLAT + CTX, D)
        tok = np.concatenate([np.arange(NLAT), NLAT + np.arange(q * NCTX, (q + 1) * NCTX)])
        ml = mods[i, b]; mcx = mods[i, 2]
        m = dict(base)
        m.update(xin=np.concatenate([x[b, q * NLAT:(q + 1) * NLAT], xc[b, cx]], axis=0),
                 y0=np.concatenate([yall[0, b, lat], yall[0, b, cx]], axis=0),
                 y1=np.concatenate([yall[1, b, lat], yall[1, b, cx]], axis=0),
                 bv=np.ascontiguousarray(oft[0][tok]), gate=np.ascontiguousarray(oft[1][tok]),
                 gtb=np.ascontiguousarray(np.stack([bcast(ml[2 * D:3 * D]), bcast(mcx[2 * D:3 * D])], axis=1)))
        maps.append(m)
    if NTOK not in _ROUT_NC:
        _ROUT_NC[NTOK] = build_rout(ntok=NTOK)
    ores = run_bass_kernel_spmd(_ROUT_NC[NTOK], maps, core_ids=list(range(NCORES))).results
    return [np.asarray(r["xout"]) for r in ores]


def kernel(**inp):
    inp = {k: np.asarray(v) for k, v in inp.items()}
    x = np.array(inp["x"], np.float32)
    xc = np.array(inp["ctx"], np.float32)
    mods = run_mod(inp["c"], inp["c_ctx"], inp["ada_w"], inp["ada_b"])
    for i in range(4):
        last = i == 3
        if i % 2 == 0:
            xs = []
            for k in range(NCORES):
                b, q = k // 4, k % 4
                xs.append(np.concatenate([x[b, q * NLAT:(q + 1) * NLAT], xc[b, (q // 2) * 128:(q // 2 + 1) * 128]], axis=0))
            outs = run_gmlp(xs, i, mods, inp)
            mix = []
            for k in range(NCORES):
                q = k % 4
                mix.append(np.concatenate([outs[k][:NLAT], outs[k][NLAT + (q % 2) * 64:NLAT + (q % 2) * 64 + 64]], axis=0))
        else:
            mix = run_rwkv_mixer(x, xc, i, mods, inp)
        _dbg("mix%d" % i, mix)
        outs = run_moe(mix, i, mods, inp, final=last)
        xn = np.empty_like(x); xcn = np.empty_like(xc)
        for k in range(NCORES):
            b, q = k // 4, k % 4
            xn[b, q * NLAT:(q + 1) * NLAT] = outs[k][:NLAT]
            xcn[b, q * NCTX:(q + 1) * NCTX] = outs[k][NLAT:]
        x, xc = xn, xcn
        _dbg("x%d" % i, x)
    return x
```
